# Optimizing a Trainium2 kernel written in Bass

```python
import math
import jax, jax.numpy as jnp
from jax import lax
import numpy as np

D_MODEL = 1024
BATCH = 2
SEQ = 8192
DEPTH = 1

CHUNK = 64

D_MIX = D_MODEL
SB_WIDTH = D_MIX // 2
SB_HEAD_DIM = 64
SB_HEADS = SB_WIDTH // SB_HEAD_DIM
Q_BLOCK = 128
SSM_WIDTH = D_MIX - SB_WIDTH
SSM_GROUP = 16
SSM_GROUPS = SSM_WIDTH // SSM_GROUP
SSM_STATE = 64
DT_MIN = 1e-3
DT_MAX = 1e-1
N_EXPERTS = 32
TOP_K = 4
D_FF = D_MODEL
SWIGLU_LIMIT = 7.0
SWIGLU_ALPHA = 1.702
EXPERT_BLOCK = 128
EPS = 1e-5

kernel_name = "hybrid_sb_s5_moe_block"


def rmsnorm(x, g):
    x32 = x.astype(jnp.float32)
    y = x32 * lax.rsqrt(jnp.mean(x32 * x32, axis=-1, keepdims=True) + EPS)
    return (y * g.astype(jnp.float32)).astype(x.dtype)


def stick_breaking_attention(q, k, v):
    bsz, seq, _ = q.shape
    def heads(a):
        return a.astype(jnp.float32).reshape(bsz, seq, SB_HEADS, SB_HEAD_DIM).transpose(0, 2, 1, 3)
    qh, kh, vh = heads(q), heads(k), heads(v)
    n_blk = seq // Q_BLOCK
    q_blocks = qh.reshape(bsz, SB_HEADS, n_blk, Q_BLOCK, SB_HEAD_DIM).transpose(2, 0, 1, 3, 4)
    starts = jnp.arange(n_blk, dtype=jnp.int32) * Q_BLOCK
    key_pos = jnp.arange(seq, dtype=jnp.int32)
    scale = 1.0 / math.sqrt(SB_HEAD_DIM)

    def one_block(args):
        qb, t0 = args
        z = jnp.einsum('bhqd,bhkd->bhqk', qb, kh) * scale
        t = t0 + jnp.arange(Q_BLOCK, dtype=jnp.int32)
        causal = key_pos[None, :] < t[:, None]
        log_keep = jnp.where(causal, -jax.nn.softplus(z), 0.0)
        between = lax.cumsum(log_keep, axis=3, reverse=True) - log_keep
        a = jnp.where(causal, jnp.exp(jax.nn.log_sigmoid(z) + between), 0.0)
        return jnp.einsum('bhqk,bhkd->bhqd', a, vh)

    out = lax.map(one_block, (q_blocks, starts))
    return out.transpose(1, 0, 3, 2, 4).reshape(bsz, seq, SB_WIDTH)


def s5_mixer(u, lam_re, lam_im, log_dt, b_re, b_im, c_re, c_im, d_skip, w_glu):
    bsz, seq, _ = u.shape
    u32 = u.astype(jnp.float32).reshape(bsz, seq, SSM_GROUPS, SSM_GROUP)
    lam = lax.complex(lam_re.astype(jnp.float32), lam_im.astype(jnp.float32))
    dt = jnp.exp(log_dt.astype(jnp.float32))[:, None]
    lam_bar = jnp.exp(lam * dt)
    b_mat = lax.complex(b_re.astype(jnp.float32), b_im.astype(jnp.float32))
    c_mat = lax.complex(c_re.astype(jnp.float32), c_im.astype(jnp.float32))
    b_bar = ((lam_bar - 1.0) / lam)[:, :, None] * b_mat
    bu = jnp.einsum('gpc,bsgc->bsgp', b_bar, u32.astype(jnp.complex64))
    a = jnp.broadcast_to(lam_bar, bu.shape)

    def combine(left, right):
        a_l, b_l = left
        a_r, b_r = right
        return a_r * a_l, a_r * b_l + b_r

    _, states = lax.associative_scan(combine, (a, bu), axis=1)
    y = jnp.real(jnp.einsum('gcp,bsgp->bsgc', c_mat, states))
    y = y + d_skip.astype(jnp.float32).reshape(SSM_GROUPS, SSM_GROUP) * u32
    y = jax.nn.gelu(y.reshape(bsz, seq, SSM_WIDTH))
    ab = y @ w_glu.astype(jnp.float32)
    y_a, y_b = jnp.split(ab, 2, axis=-1)
    return y_a * jax.nn.sigmoid(y_b)


def moe_ffn(xn, w_router, b_router, w_gate, b_gate, w_up, b_up, w_down, b_down):
    bsz, seq, d = xn.shape
    n_tok = bsz * seq
    xt = xn.reshape(n_tok, d)
    logits = (xt @ w_router + b_router).astype(jnp.float32)
    top_val, top_idx = lax.top_k(logits, TOP_K)
    gates = jax.nn.softmax(top_val, axis=-1)
    n_assign = n_tok * TOP_K
    e_flat = top_idx.reshape(n_assign)
    tok_flat = jnp.repeat(jnp.arange(n_tok, dtype=jnp.int32), TOP_K)
    g_flat = gates.reshape(n_assign)
    order = jnp.argsort(e_flat)
    e_sorted, tok_sorted, g_sorted = e_flat[order], tok_flat[order], g_flat[order]
    counts = jnp.bincount(e_flat, length=N_EXPERTS)
    starts = jnp.cumsum(counts) - counts
    padded = ((counts + EXPERT_BLOCK - 1) // EXPERT_BLOCK) * EXPERT_BLOCK
    pad_starts = jnp.cumsum(padded) - padded
    pad_ends = pad_starts + padded
    rank = jnp.arange(n_assign, dtype=jnp.int32) - starts[e_sorted]
    dest = pad_starts[e_sorted] + rank
    n_rows = n_assign + N_EXPERTS * EXPERT_BLOCK
    n_blocks = n_rows // EXPERT_BLOCK
    row_tok = jnp.full((n_rows,), n_tok, dtype=jnp.int32).at[dest].set(tok_sorted)
    x_pad = jnp.concatenate([xt, jnp.zeros((1, d), xt.dtype)], axis=0)
    x_rows = x_pad[row_tok].reshape(n_blocks, EXPERT_BLOCK, d)
    block_start = jnp.arange(n_blocks, dtype=jnp.int32) * EXPERT_BLOCK
    block_expert = jnp.minimum(jnp.searchsorted(pad_ends, block_start, side='right'), N_EXPERTS - 1)

    def expert_block(args):
        xb, e = args
        gate = xb @ w_gate[e] + b_gate[e]
        up = xb @ w_up[e] + b_up[e]
        gate = jnp.minimum(gate, SWIGLU_LIMIT)
        up = jnp.clip(up, -SWIGLU_LIMIT, SWIGLU_LIMIT)
        glu = gate * jax.nn.sigmoid(SWIGLU_ALPHA * gate)
        return ((up + 1.0) * glu) @ w_down[e] + b_down[e]

    y_rows = lax.map(expert_block, (x_rows, block_expert)).reshape(n_rows, d)
    y_assign = y_rows[dest].astype(jnp.float32) * g_sorted[:, None]
    out = jax.ops.segment_sum(y_assign, tok_sorted, num_segments=n_tok)
    return out.reshape(bsz, seq, d).astype(xn.dtype)


def setup_inputs(seed: int = 0) -> dict:
    key = jax.random.key(seed)
    ks = jax.random.split(key, 26)
    L, D, W, G, C, P, E, F = DEPTH, D_MODEL, SSM_WIDTH, SSM_GROUPS, SSM_GROUP, SSM_STATE, N_EXPERTS, D_FF
    n_in = 3 * SB_WIDTH + SSM_WIDTH
    nrm = lambda k, shape: jax.random.normal(k, shape, jnp.float32)
    return {
        "x": nrm(ks[0], (BATCH, SEQ, D)),
        "ln1_g": 1.0 + 0.01 * nrm(ks[1], (L, D)),
        "w_in": nrm(ks[2], (L, D, n_in)) * D ** -0.5,
        "lam_re": -0.5 + 0.01 * nrm(ks[3], (L, G, P)),
        "lam_im": jnp.pi * jnp.arange(P, dtype=jnp.float32) + 0.01 * nrm(ks[4], (L, G, P)),
        "log_dt": jax.random.uniform(ks[5], (L, G), jnp.float32, math.log(DT_MIN), math.log(DT_MAX)),
        "ssm_b_re": nrm(ks[6], (L, G, P, C)) * (2.0 * C) ** -0.5,
        "ssm_b_im": nrm(ks[7], (L, G, P, C)) * (2.0 * C) ** -0.5,
        "ssm_c_re": nrm(ks[8], (L, G, C, P)) * (2.0 * P) ** -0.5,
        "ssm_c_im": nrm(ks[9], (L, G, C, P)) * (2.0 * P) ** -0.5,
        "ssm_d": nrm(ks[10], (L, W)),
        "w_glu": nrm(ks[11], (L, W, 2 * W)) * W ** -0.5,
        "g_sb": 1.0 + 0.01 * nrm(ks[12], (L, SB_WIDTH)),
        "g_ssm": 1.0 + 0.01 * nrm(ks[13], (L, SSM_WIDTH)),
        "w_out": nrm(ks[14], (L, D_MIX, D)) * D_MIX ** -0.5,
        "ln2_g": 1.0 + 0.01 * nrm(ks[15], (L, D)),
        "w_router": nrm(ks[16], (L, D, E)) * D ** -0.5,
        "b_router": 0.01 * nrm(ks[17], (L, E)),
        "w_gate": nrm(ks[18], (L, E, D, F)) * D ** -0.5,
        "b_gate": 0.01 * nrm(ks[19], (L, E, F)),
        "w_up": nrm(ks[20], (L, E, D, F)) * D ** -0.5,
        "b_up": 0.01 * nrm(ks[21], (L, E, F)),
        "w_down": nrm(ks[22], (L, E, F, D)) * F ** -0.5,
        "b_down": 0.01 * nrm(ks[23], (L, E, D)),
        "ln_f_g": 1.0 + 0.01 * nrm(ks[24], (D,)),
    }


def reference(x, ln1_g, w_in, lam_re, lam_im, log_dt, ssm_b_re, ssm_b_im, ssm_c_re, ssm_c_im,
              ssm_d, w_glu, g_sb, g_ssm, w_out, ln2_g, w_router, b_router, w_gate, b_gate,
              w_up, b_up, w_down, b_down, ln_f_g):
    for l in range(DEPTH):
        h = rmsnorm(x, ln1_g[l])
        proj = h @ w_in[l]
        q = proj[..., 0:SB_WIDTH]
        k = proj[..., SB_WIDTH:2 * SB_WIDTH]
        v = proj[..., 2 * SB_WIDTH:3 * SB_WIDTH]
        u = proj[..., 3 * SB_WIDTH:]
        y_sb = stick_breaking_attention(q, k, v)
        y_ssm = s5_mixer(u, lam_re[l], lam_im[l], log_dt[l], ssm_b_re[l], ssm_b_im[l],
                         ssm_c_re[l], ssm_c_im[l], ssm_d[l], w_glu[l])
        mixed = jnp.concatenate([rmsnorm(y_sb, g_sb[l]), rmsnorm(y_ssm, g_ssm[l])], axis=-1)
        x = x + (mixed @ w_out[l].astype(jnp.float32)).astype(x.dtype)
        x = x + moe_ffn(rmsnorm(x, ln2_g[l]), w_router[l], b_router[l], w_gate[l], b_gate[l],
                        w_up[l], b_up[l], w_down[l], b_down[l])
    return rmsnorm(x, ln_f_g)
```

```python
import numpy as np
import ml_dtypes
import concourse.bass as bass
import concourse.mybir as mybir
from concourse.bass_utils import run_bass_kernel_spmd

F32 = mybir.dt.float32
BF16 = mybir.dt.bfloat16
AF = mybir.ActivationFunctionType
ALU = mybir.AluOpType
AX = mybir.AxisListType


class Buf:
    __slots__ = ("name", "w", "r")

    def __init__(self, name=""):
        self.name = name
        self.w = None
        self.r = {}


class _Rec:
    def __init__(self):
        self.call = None

    def __getattr__(self, name):
        def f(*a, **kw):
            self.call = (name, a, kw)
            return self
        return f


class Sched:
    ENGS = ("pe", "act", "dve", "pool", "sp")

    def __init__(self, nc, sems, dma_sems):
        self.nc = nc
        self.sem = dict(zip(self.ENGS, sems))
        self.cnt = {e: 0 for e in self.ENGS}
        self.ops = {e: [] for e in self.ENGS}
        self.waited = {e: {} for e in self.ENGS}
        self.dma_sems = dma_sems
        self.dma_cnt = [0] * len(dma_sems)
        self.dma_next = 0
        self.n_inst = 0

    def _semh(self, key):
        return self.sem[key] if isinstance(key, str) else self.dma_sems[key]

    def _need(self, eng, key, val):
        if key == eng and eng == "pe":
            return
        if self.waited[eng].get(key, 0) >= val:
            return
        self.waited[eng][key] = val
        h = self._semh(key)
        self.ops[eng].append(lambda e, h=h, val=val: e.wait_ge(h, val))

    def _deps(self, eng, reads, writes):
        for b in reads:
            if b.w is not None:
                self._need(eng, *b.w)
        for b in writes:
            if b.w is not None:
                self._need(eng, *b.w)
            for k, v in b.r.items():
                self._need(eng, k, v)

    def op(self, eng, fn, reads=(), writes=()):
        self._deps(eng, reads, writes)
        self.cnt[eng] += 1
        c = self.cnt[eng]
        h = self.sem[eng]
        rec = _Rec()
        fn(rec)
        m, a, kw = rec.call
        self.ops[eng].append(lambda e, m=m, a=a, kw=kw, h=h: getattr(e, m)(*a, **kw).then_inc(h, 1))
        for b in writes:
            b.w = (eng, c)
            b.r = {}
        for b in reads:
            if b.w is None or b.w != (eng, c):
                b.r[eng] = c
        self.n_inst += 1

    def dma(self, fn, reads=(), writes=(), eng="sp"):
        i = self.dma_next
        self.dma_next = (self.dma_next + 1) % len(self.dma_sems)
        if self.dma_cnt[i] > 0:
            self._need(eng, i, self.dma_cnt[i])
        self._deps(eng, reads, writes)
        self.dma_cnt[i] += 16
        v = self.dma_cnt[i]
        h = self.dma_sems[i]
        rec = _Rec()
        fn(rec)
        m, a, kw = rec.call
        self.ops[eng].append(lambda e, m=m, a=a, kw=kw, h=h: getattr(e, m)(*a, **kw).then_inc(h, 16))
        for b in writes:
            b.w = (i, v)
            b.r = {}
        for b in reads:
            b.r[i] = v
        self.n_inst += 1
        return (i, v)

    def finish(self, eng="sp"):
        for i, v in enumerate(self.dma_cnt):
            if v > 0:
                self._need(eng, i, v)

    def emit(self, block):
        ops = self.ops

        @block.tensor
        def _(e):
            for f in ops["pe"]:
                f(e)

        @block.scalar
        def _(e):
            for f in ops["act"]:
                f(e)

        @block.vector
        def _(e):
            for f in ops["dve"]:
                f(e)

        @block.gpsimd
        def _(e):
            for f in ops["pool"]:
                f(e)

        @block.sync
        def _(e):
            for f in ops["sp"]:
                f(e)

    def barrier(self):
        for eng in self.ENGS:
            for k in self.ENGS:
                if k != eng and self.cnt[k] > 0:
                    self._need(eng, k, self.cnt[k])
            for i, v in enumerate(self.dma_cnt):
                if v > 0:
                    self._need(eng, i, v)


class TT:
    def __init__(self, t, name, nbuf=1):
        self.t = t
        self.b = Buf(name)
        self.bs = [Buf(f"{name}{i}") for i in range(nbuf)] if nbuf > 1 else None

    def __getitem__(self, k):
        return self.t[k]


class View:
    def __init__(self, ap, name):
        self.ap = ap
        self.b = Buf(name)

    def __getitem__(self, k):
        return self.ap[k]


NTOK = 8192
NBLK = 64
NOWN = 16
EPS = 1e-5
GELU_C = 0.7978845608028654
EXPERTS = 32


def build_program(dbg=(), stop_after=None, n_experts=EXPERTS, upto=None):
    from contextlib import ExitStack
    nc = bass.Bass("TRN2", target_bir_lowering=False)

    def din(name, shape, dt=F32):
        return nc.dram_tensor(name, list(shape), dt, kind="ExternalInput").ap()

    xv = din("xv", [NTOK, 1024])
    w_in = din("w_in", [1024, 2048]); w_glu = din("w_glu", [512, 1024]); w_out = din("w_out", [1024, 1024])
    w_router = din("w_router", [1024, 32])
    w_gate = din("w_gate", [32, 1024, 1024]); w_up = din("w_up", [32, 1024, 1024]); w_down = din("w_down", [32, 1024, 1024])
    g1_d = din("g1", [128, 8]); gcat_d = din("gcat", [128, 8]); g2_d = din("g2", [128, 8])
    gf_d = din("gf", [1, 1024]); brow_d = din("brow", [1, 32])
    bgT_d = din("bgT", [128, 32, 8]); buT_d = din("buT", [128, 32, 8]); bd_d = din("bd", [32, 1024])
    lamre_d = din("lamre", [128, 16]); lamim_d = din("lamim", [128, 16]); logdt_d = din("logdt", [128, 16])
    bre_d = din("bre", [128, 16, 16]); bim_d = din("bim", [128, 16, 16])
    cre_d = din("creT", [128, 16, 16]); cim_d = din("cimT", [128, 16, 16]); dsk_d = din("dsk", [128, 4])
    ident_d = din("ident", [128, 128]); negtri_d = din("negtri", [128, 128]); negones_d = din("negones", [128, 128])
    strict_d = din("strict", [128, 128]); ramp_d = din("ramp", [128, 128]); ramp2_d = din("ramp2", [128, 128])
    out_d = nc.dram_tensor("out", [NOWN * 128, 1024], F32, kind="ExternalOutput").ap()
    dbg_d = {}

    top = ExitStack()
    with top:
        sems = [top.enter_context(nc.semaphore(f"s_{e}")) for e in Sched.ENGS]
        dsems = [top.enter_context(nc.semaphore(f"d_{i}")) for i in range(24)]
        S = Sched(nc, sems, dsems)

        used = {}

        def uniq(n):
            used[n] = used.get(n, 0) + 1
            return n if used[n] == 1 else f"{n}_v{used[n]}"

        def sb(es, name, shape, dt=F32, nbuf=1):
            return TT(es.enter_context(nc.sbuf_tensor(uniq("sb_" + name), list(shape), dt)), name, nbuf)

        def ps(es, name, shape=(128, 512), dt=F32):
            return TT(es.enter_context(nc.psum_tensor(uniq("ps_" + name), list(shape), dt)), name)

        def dump(name, ap, buf, shape):
            if name not in dbg:
                return
            d = nc.dram_tensor("dbg_" + name, list(shape), F32, kind="ExternalOutput").ap()
            dbg_d[name] = d
            S.dma(lambda e: e.dma_start(out=d, in_=ap), reads=[buf])

        def load(dst, dst_ap, src_ap, eng="sp"):
            S.dma(lambda e: e.dma_start(out=dst_ap, in_=src_ap), writes=[dst.b], eng=eng)

        ident32 = sb(top, "ident32", [128, 128]); ident16 = sb(top, "ident16", [128, 128], BF16)
        negtri16 = sb(top, "negtri16", [128, 128], BF16); negones16 = sb(top, "negones16", [128, 128], BF16)
        strict16 = sb(top, "strict16", [128, 128], BF16)
        shared = sb(top, "shared", [128, 16384], BF16)
        mssm16 = View(shared[:, 0:8192].rearrange("p (a b) -> p a b", a=NOWN), "mssm16")
        ysb16 = View(shared[:, 8192:16384].rearrange("p (a b) -> p a b", a=NOWN), "ysb16")
        ss_sb = sb(top, "ss_sb", [128, NOWN, 2])
        cstg = sb(top, "cstg", [128, 128])
        load(ident32, ident32[:], ident_d)
        S.op("dve", lambda e: e.tensor_copy(out=ident16[:], in_=ident32[:]), reads=[ident32.b], writes=[ident16.b])
        for dst, src in ((negtri16, negtri_d), (negones16, negones_d), (strict16, strict_d)):
            load(cstg, cstg[:], src)
            S.op("dve", lambda e, dst=dst: e.tensor_copy(out=dst[:], in_=cstg[:]), reads=[cstg.b], writes=[dst.b])

        def make_prologue(es, evac_eng, stat_eng="act", extra_pT=()):
            P = {}
            P["xc"] = sb(es, "xc", [128, 2, 1024]); P["xs16"] = sb(es, "xs16", [128, 4, 1024], BF16)
            P["junk"] = sb(es, "junk", [128, 1024], BF16); P["ss4"] = sb(es, "ss4", [128, 4]); P["rstd4"] = sb(es, "rstd4", [128, 4])
            P["hT"] = [sb(es, "hT0", [128, 8, 512], BF16)] * 2
            pTt = ps(es, "pTt", [128, 1024], BF16)

            class _PV:
                def __init__(s_): s_.b = pTt.b
                def __getitem__(s_, k): return pTt.t[:, 0:512][k]
            class _PVx:
                def __init__(s_, tt): s_.tt = tt; s_.b = tt.b
                def __getitem__(s_, k): return s_.tt.t[:].bitcast(BF16)[:, 0:512][k]
            P["pT"] = [_PV()] + [_PVx(t_) for t_ in extra_pT]
            P["n"] = 0
            P["defer"] = None

            def pro_a(c):
                xc, xs16, junk, ss4, rstd4 = P["xc"], P["xs16"], P["junk"], P["ss4"], P["rstd4"]
                for hf in range(2):
                    load(xc, xc[:], xv[c * 512 + hf * 256:c * 512 + (hf + 1) * 256, :].rearrange("(b p) d -> p b d", p=128))
                    for i2 in range(2):
                        i = 2 * hf + i2
                        if stat_eng == "act":
                            S.op("act", lambda e: e.activation(out=junk[:], in_=xc[:, i2, :], func=AF.Square, accum_out=ss4[:, i:i + 1]),
                                 reads=[xc.b], writes=[junk.b, ss4.b])
                        else:
                            S.op("dve", lambda e: e.scalar_tensor_tensor(out=junk[:], in0=xc[:, i2, :], scalar=1.0, in1=xc[:, i2, :], op0=ALU.mult, op1=ALU.mult, accum_out=ss4[:, i:i + 1]),
                                 reads=[xc.b], writes=[junk.b, ss4.b])
                    S.op("act", lambda e: e.activation(out=rstd4[:, 2 * hf:2 * hf + 2], in_=ss4[:, 2 * hf:2 * hf + 2], func=AF.Ln, scale=1.0 / 1024, bias=EPS), reads=[ss4.b], writes=[rstd4.b])
                    S.op("act", lambda e: e.activation(out=rstd4[:, 2 * hf:2 * hf + 2], in_=rstd4[:, 2 * hf:2 * hf + 2], func=AF.Exp, scale=-0.5), reads=[rstd4.b], writes=[rstd4.b])
                    for i2 in range(2):
                        i = 2 * hf + i2
                        if stat_eng == "act":
                            S.op("act", lambda e: e.activation(out=xs16[:, i, :], in_=xc[:, i2, :], func=AF.Copy, scale=rstd4[:, i:i + 1]),
                                 reads=[xc.b, rstd4.b], writes=[xs16.b])
                        else:
                            S.op("dve", lambda e: e.tensor_scalar(out=xs16[:, i, :], in0=xc[:, i2, :], scalar1=rstd4[:, i:i + 1], scalar2=None, op0=ALU.mult),
                                 reads=[xc.b, rstd4.b], writes=[xs16.b])

            def pro_b_units(c):
                xs16 = P["xs16"]
                hT = P["hT"][c % 2]
                units = []
                for dt in range(8):
                    def unit(dt=dt):
                        pT = P["pT"][dt % len(P["pT"])]
                        for i in range(4):
                            S.op("pe", lambda e: e.transpose(pT[:, i * 128:(i + 1) * 128], xs16[:, i, dt * 128:(dt + 1) * 128], ident16[:]),
                                 reads=[xs16.b, ident16.b], writes=[pT.b])
                        eng = evac_eng if isinstance(evac_eng, str) else evac_eng[dt % len(evac_eng)]

                        def evac(pT=pT, dt=dt, eng=eng):
                            if eng == "act":
                                S.op("act", lambda e: e.activation(out=hT[:, dt, :], in_=pT[:], func=AF.Copy), reads=[pT.b], writes=[hT.b])
                            else:
                                S.op(eng, lambda e: e.tensor_copy(out=hT[:, dt, :], in_=pT[:]), reads=[pT.b], writes=[hT.b])
                        if P.get("defer") is not None:
                            P["defer"].append(evac)
                        else:
                            evac()
                    units.append(unit)
                return hT, units
            return pro_a, pro_b_units, P

        def load_win(es, name, colspecs, stgs):
            tot = sum(n for _, n, _ in colspecs)
            W = sb(es, name, [128, 8, tot], BF16)
            k = 0
            for dt in range(8):
                o = 0
                for (c0, n, sc) in colspecs:
                    stg = stgs[k % len(stgs)]
                    k += 1
                    load(stg, stg[:, 0:n], w_in[dt * 128:(dt + 1) * 128, c0:c0 + n])
                    gx = g1 if sc == 1.0 else g1q
                    S.op("act", lambda e: e.activation(out=W[:, dt, o:o + n], in_=stg[:, 0:n], func=AF.Copy, scale=gx[:, dt:dt + 1]), reads=[stg.b, gx.b], writes=[W.b])
                    o += n
            return W

        g1 = sb(top, "g1", [128, 8]); load(g1, g1[:], g1_d)
        g1q = sb(top, "g1q", [128, 8])
        S.op("dve", lambda e: e.tensor_scalar(out=g1q[:], in0=g1[:], scalar1=0.125, scalar2=None, op0=ALU.mult), reads=[g1.b], writes=[g1q.b])

        def phase_S():
            es = ExitStack()
            with es:
                stgs = [sb(es, f"stgS{i}", [128, 1024]) for i in range(2)]
                stg = stgs[0]
                Wu16 = load_win(es, "Wu16", [(1536, 512, 1.0)], stgs)
                Wglu16 = sb(es, "Wglu16", [128, 4, 1024], BF16)
                for ct in range(4):
                    stg = stgs[ct % 2]
                    load(stg, stg[:], w_glu[ct * 128:(ct + 1) * 128, :])
                    S.op("act", lambda e: e.activation(out=Wglu16[:, ct, :], in_=stg[:], func=AF.Copy), reads=[stg.b], writes=[Wglu16.b])
                dsk = sb(es, "dsk", [128, 4]); load(dsk, dsk[:], dsk_d)
                cosT = sb(es, "cosT", [128, 16, 128]); sinT = sb(es, "sinT", [128, 16, 128]); Rt = sb(es, "Rt", [128, 16, 128])
                Bre16 = sb(es, "Bre16", [128, 16, 128], BF16); Bim16 = sb(es, "Bim16", [128, 16, 128], BF16)
                Cre16 = sb(es, "Cre16", [128, 16, 128], BF16); Cim16 = sb(es, "Cim16", [128, 16, 128], BF16)
                rr2 = sb(es, "rr2", [128, 2, 16]); init = sb(es, "init", [128, 2, 16])
                Apr16 = sb(es, "Apr16", [128, 16, 128], BF16); Api16 = sb(es, "Api16", [128, 16, 128], BF16)
                Bpre = sb(es, "Bpre", [128, 16, 32]); Bpim = sb(es, "Bpim", [128, 16, 32]); a128 = sb(es, "a128", [128, 2, 16])
                pbr = [ps(es, f"pbr{i}") for i in range(2)]; pbi = [ps(es, f"pbi{i}") for i in range(2)]
                py = ps(es, "py")
                pin = [ps(es, f"pinS{i}") for i in range(2)]

                su = ExitStack()
                with su:
                    def small(name, shape=(128, 16)):
                        return sb(su, name, shape)
                    lamre = small("lamre"); lamim = small("lamim"); logdt = small("logdt")
                    bre = small("bre", [128, 16, 16]); bim = small("bim", [128, 16, 16])
                    creT = small("creTs", [128, 16, 16]); cimT = small("cimTs", [128, 16, 16])
                    ramp = small("ramp", [128, 128])
                    for t_, d_ in ((lamre, lamre_d), (lamim, lamim_d), (logdt, logdt_d), (bre, bre_d), (bim, bim_d), (creT, cre_d), (cimT, cim_d), (ramp, ramp_d)):
                        load(t_, t_[:], d_)
                    dtv = small("dtv"); ar = small("ar"); th = small("th"); rr = small("rr")
                    S.op("act", lambda e: e.activation(out=dtv[:], in_=logdt[:], func=AF.Exp), reads=[logdt.b], writes=[dtv.b])
                    S.op("dve", lambda e: e.tensor_tensor(out=ar[:], in0=lamre[:], in1=dtv[:], op=ALU.mult), reads=[lamre.b, dtv.b], writes=[ar.b])
                    S.op("dve", lambda e: e.tensor_tensor(out=th[:], in0=lamim[:], in1=dtv[:], op=ALU.mult), reads=[lamim.b, dtv.b], writes=[th.b])
                    S.op("act", lambda e: e.activation(out=rr[:], in_=ar[:], func=AF.Exp), reads=[ar.b], writes=[rr.b])

                    sc_tmp = {}

                    def sincos(name, ang, n, cos_out, sin_out, cb, sbuf_):
                        if n not in sc_tmp:
                            sc_tmp[n] = (sb(su, name + "_tq", [128, n]), sb(su, name + "_ti", [128, n], mybir.dt.int32), sb(su, name + "_tf", [128, n]))
                        tq, ti, tf = sc_tmp[n]
                        for off, outap, ob in ((0.0, sin_out, sbuf_), (0.25, cos_out, cb)):
                            S.op("dve", lambda e, off=off: e.tensor_scalar(out=tq[:], in0=ang[:], scalar1=1.0 / (2 * np.pi), scalar2=off, op0=ALU.mult, op1=ALU.add),
                                 reads=[ang.b], writes=[tq.b])
                            S.op("dve", lambda e: e.tensor_copy(out=ti[:], in_=tq[:]), reads=[tq.b], writes=[ti.b])
                            S.op("dve", lambda e: e.tensor_copy(out=tf[:], in_=ti[:]), reads=[ti.b], writes=[tf.b])
                            S.op("dve", lambda e: e.tensor_tensor(out=tq[:], in0=tq[:], in1=tf[:], op=ALU.subtract), reads=[tq.b, tf.b], writes=[tq.b])
                            S.op("act", lambda e, outap=outap: e.activation(out=outap, in_=tq[:], func=AF.Sin, scale=6.28318), reads=[tq.b], writes=[ob])

                    cth = small("cth"); sth = small("sth")
                    sincos("a", th, 16, cth[:], sth[:], cth.b, sth.b)
                    a_re = small("a_re"); a_im = small("a_im")
                    S.op("dve", lambda e: e.tensor_tensor(out=a_re[:], in0=rr[:], in1=cth[:], op=ALU.mult), reads=[rr.b, cth.b], writes=[a_re.b])
                    S.op("dve", lambda e: e.tensor_tensor(out=a_im[:], in0=rr[:], in1=sth[:], op=ALU.mult), reads=[rr.b, sth.b], writes=[a_im.b])
                    nre = small("nre"); den = small("den"); t0 = small("t0"); t1 = small("t1"); cf_re = small("cf_re"); cf_im = small("cf_im"); ncf_im = small("ncf_im")
                    S.op("dve", lambda e: e.tensor_scalar(out=nre[:], in0=a_re[:], scalar1=-1.0, scalar2=None, op0=ALU.add), reads=[a_re.b], writes=[nre.b])
                    S.op("dve", lambda e: e.tensor_tensor(out=t0[:], in0=lamre[:], in1=lamre[:], op=ALU.mult), reads=[lamre.b], writes=[t0.b])
                    S.op("dve", lambda e: e.tensor_tensor(out=t1[:], in0=lamim[:], in1=lamim[:], op=ALU.mult), reads=[lamim.b], writes=[t1.b])
                    S.op("dve", lambda e: e.tensor_tensor(out=den[:], in0=t0[:], in1=t1[:], op=ALU.add), reads=[t0.b, t1.b], writes=[den.b])
                    S.op("dve", lambda e: e.reciprocal(out=den[:], in_=den[:]), reads=[den.b], writes=[den.b])
                    S.op("dve", lambda e: e.tensor_tensor(out=t0[:], in0=nre[:], in1=lamre[:], op=ALU.mult), reads=[nre.b, lamre.b], writes=[t0.b])
                    S.op("dve", lambda e: e.tensor_tensor(out=t1[:], in0=a_im[:], in1=lamim[:], op=ALU.mult), reads=[a_im.b, lamim.b], writes=[t1.b])
                    S.op("dve", lambda e: e.tensor_tensor(out=t0[:], in0=t0[:], in1=t1[:], op=ALU.add), reads=[t0.b, t1.b], writes=[t0.b])
                    S.op("dve", lambda e: e.tensor_tensor(out=cf_re[:], in0=t0[:], in1=den[:], op=ALU.mult), reads=[t0.b, den.b], writes=[cf_re.b])
                    S.op("dve", lambda e: e.tensor_tensor(out=t0[:], in0=a_im[:], in1=lamre[:], op=ALU.mult), reads=[a_im.b, lamre.b], writes=[t0.b])
                    S.op("dve", lambda e: e.tensor_tensor(out=t1[:], in0=nre[:], in1=lamim[:], op=ALU.mult), reads=[nre.b, lamim.b], writes=[t1.b])
                    S.op("dve", lambda e: e.tensor_tensor(out=t0[:], in0=t0[:], in1=t1[:], op=ALU.subtract), reads=[t0.b, t1.b], writes=[t0.b])
                    S.op("dve", lambda e: e.tensor_tensor(out=cf_im[:], in0=t0[:], in1=den[:], op=ALU.mult), reads=[t0.b, den.b], writes=[cf_im.b])
                    S.op("dve", lambda e: e.tensor_scalar(out=ncf_im[:], in0=cf_im[:], scalar1=-1.0, scalar2=None, op0=ALU.mult), reads=[cf_im.b], writes=[ncf_im.b])
                    Mre = sb(su, "Mre", [128, 16, 128]); Mim = sb(su, "Mim", [128, 16, 128]); tb = sb(su, "tb", [128, 16])
                    S.op("pool", lambda e: e.memset(Mre[:], 0.0), writes=[Mre.b])
                    S.op("pool", lambda e: e.memset(Mim[:], 0.0), writes=[Mim.b])
                    S.op("pool", lambda e: e.memset(Cre16[:], 0.0), writes=[Cre16.b])
                    S.op("pool", lambda e: e.memset(Cim16[:], 0.0), writes=[Cim16.b])
                    for j in range(16):
                        jj = j % 4
                        for g2 in range(2):
                            p0, p1 = 64 * g2, 64 * g2 + 64
                            c0 = 32 * jj + 16 * g2
                            S.op("dve", lambda e, j=j, p0=p0, p1=p1: e.tensor_scalar(out=tb[p0:p1, :], in0=bre[p0:p1, j, :], scalar1=cf_re[p0:p1, j:j + 1], scalar2=None, op0=ALU.mult),
                                 reads=[bre.b, cf_re.b], writes=[tb.b])
                            S.op("dve", lambda e, j=j, p0=p0, p1=p1, c0=c0: e.scalar_tensor_tensor(out=Mre[p0:p1, j, c0:c0 + 16], in0=bim[p0:p1, j, :], scalar=ncf_im[p0:p1, j:j + 1], in1=tb[p0:p1, :], op0=ALU.mult, op1=ALU.add),
                                 reads=[bim.b, ncf_im.b, tb.b], writes=[Mre.b])
                            S.op("dve", lambda e, j=j, p0=p0, p1=p1: e.tensor_scalar(out=tb[p0:p1, :], in0=bim[p0:p1, j, :], scalar1=cf_re[p0:p1, j:j + 1], scalar2=None, op0=ALU.mult),
                                 reads=[bim.b, cf_re.b], writes=[tb.b])
                            S.op("dve", lambda e, j=j, p0=p0, p1=p1, c0=c0: e.scalar_tensor_tensor(out=Mim[p0:p1, j, c0:c0 + 16], in0=bre[p0:p1, j, :], scalar=cf_im[p0:p1, j:j + 1], in1=tb[p0:p1, :], op0=ALU.mult, op1=ALU.add),
                                 reads=[bre.b, cf_im.b, tb.b], writes=[Mim.b])
                            S.op("pool", lambda e, j=j, p0=p0, p1=p1, c0=c0: e.tensor_copy(out=Cre16[p0:p1, j, c0:c0 + 16], in_=creT[p0:p1, j, :]), reads=[creT.b], writes=[Cre16.b])
                            S.op("pool", lambda e, j=j, p0=p0, p1=p1, c0=c0: e.tensor_scalar(out=Cim16[p0:p1, j, c0:c0 + 16], in0=cimT[p0:p1, j, :], scalar1=-1.0, scalar2=None, op0=ALU.mult), reads=[cimT.b], writes=[Cim16.b])
                    for j in range(16):
                        for (M_, B_) in ((Mre, Bre16), (Mim, Bim16)):
                            S.op("pe", lambda e, j=j, M_=M_: e.transpose(py[:, 0:128], M_[:, j, :], ident32[:]), reads=[M_.b, ident32.b], writes=[py.b])
                            S.op("dve", lambda e, j=j, B_=B_: e.tensor_copy(out=B_[:, j, :], in_=py[:, 0:128]), reads=[py.b], writes=[B_.b])
                    ang = sb(su, "ang", [128, 16, 128])
                    for j in range(16):
                        S.op("dve", lambda e, j=j: e.tensor_scalar(out=ang[:, j, :], in0=ramp[:], scalar1=th[:, j:j + 1], scalar2=None, op0=ALU.mult), reads=[ramp.b, th.b], writes=[ang.b])
                        S.op("pool", lambda e, j=j: e.tensor_scalar(out=Rt[:, j, :], in0=ramp[:], scalar1=0.0, scalar2=rr[:, j:j + 1], op0=ALU.mult, op1=ALU.add), reads=[ramp.b, rr.b], writes=[Rt.b])
                    angf = TT(ang.t, "angf"); angf.b = ang.b
                    class _V:
                        def __init__(s_, t, b): s_.t = t; s_.b = b
                        def __getitem__(s_, k): return s_.t[:].rearrange("p a b -> p (a b)")
                    sincos("tab", _V(ang.t, ang.b), 2048, cosT[:].rearrange("p a b -> p (a b)"), sinT[:].rearrange("p a b -> p (a b)"), cosT.b, sinT.b)
                    S.op("pool", lambda e: e.memset(Rt[:, :, 0:1], 0.0), writes=[Rt.b])
                    S.op("dve", lambda e: e.tensor_copy(out=rr2[:, 0, :], in_=rr[:]), reads=[rr.b], writes=[rr2.b])
                    S.op("dve", lambda e: e.tensor_copy(out=rr2[:, 1, :], in_=rr[:]), reads=[rr.b], writes=[rr2.b])
                    S.op("dve", lambda e: e.memset(init[:], 0.0), writes=[init.b])
                    ramp2 = small("ramp2", [128, 128]); load(ramp2, ramp2[:], ramp2_d)
                    ang2 = ang; rpow = sb(su, "rpow", [128, 16, 128])
                    c2 = sb(su, "c2", [128, 16, 128]); s2 = sb(su, "s2", [128, 16, 128])
                    for j in range(16):
                        S.op("dve", lambda e: e.tensor_scalar(out=ang2[:, j, :], in0=ramp2[:], scalar1=th[:, j:j + 1], scalar2=None, op0=ALU.mult), reads=[ramp2.b, th.b], writes=[ang2.b])
                    sincos("tab2", _V(ang2.t, ang2.b), 2048, c2[:].rearrange("p a b -> p (a b)"), s2[:].rearrange("p a b -> p (a b)"), c2.b, s2.b)
                    for j in range(16):
                        S.op("act", lambda e: e.activation(out=rpow[:, j, :], in_=ramp2[:], func=AF.Exp, scale=ar[:, j:j + 1]), reads=[ramp2.b, ar.b], writes=[rpow.b])
                    S.op("dve", lambda e: e.tensor_tensor(out=c2[:], in0=c2[:], in1=rpow[:], op=ALU.mult), reads=[c2.b, rpow.b], writes=[c2.b])
                    S.op("dve", lambda e: e.tensor_tensor(out=s2[:], in0=s2[:], in1=rpow[:], op=ALU.mult), reads=[s2.b, rpow.b], writes=[s2.b])
                    for j in range(16):
                        for (src_, dst_) in ((c2, Apr16), (s2, Api16)):
                            S.op("pe", lambda e: e.transpose(py[:, 0:128], src_[:, j, :], ident32[:]), reads=[src_.b, ident32.b], writes=[py.b])
                            S.op("dve", lambda e: e.tensor_copy(out=dst_[:, j, :], in_=py[:, 0:128]), reads=[py.b], writes=[dst_.b])
                        jj = j % 4
                        S.op("dve", lambda e: e.tensor_copy(out=Bpre[:, j, :], in_=Mre[:, j, 32 * jj:32 * jj + 32]), reads=[Mre.b], writes=[Bpre.b])
                        S.op("dve", lambda e: e.tensor_copy(out=Bpim[:, j, :], in_=Mim[:, j, 32 * jj:32 * jj + 32]), reads=[Mim.b], writes=[Bpim.b])
                    r128 = small("r128")
                    S.op("act", lambda e: e.activation(out=r128[:], in_=ar[:], func=AF.Exp, scale=128.0), reads=[ar.b], writes=[r128.b])
                    S.op("dve", lambda e: e.tensor_tensor(out=a128[:, 0, :], in0=r128[:], in1=cosT[:, :, 127], op=ALU.mult), reads=[r128.b, cosT.b], writes=[a128.b])
                    S.op("dve", lambda e: e.tensor_tensor(out=a128[:, 1, :], in0=r128[:], in1=sinT[:, :, 127], op=ALU.mult), reads=[r128.b, sinT.b], writes=[a128.b])
                    dump("cosT", cosT[:].rearrange("p a b -> p (a b)"), cosT.b, [128, 2048])
                    dump("Rt", Rt[:].rearrange("p a b -> p (a b)"), Rt.b, [128, 2048])
                    S.barrier()
                pro_a, pro_b_units, _P = make_prologue(es, "act")
                pend = []
                uT16 = [sb(es, f"uT16_{i}", [128, 4, 128], BF16) for i in range(2)]
                utok16 = [sb(es, f"utok16_{i}", [128, 3, 512], BF16) for i in range(2)]
                qri = sb(es, "qri", [128, 2, 16]); m8 = [sb(es, f"m8_{i}", [128, 16]) for i in range(4)]
                u32 = [sb(es, f"u32_{i}", [128, 4, 128]) for i in range(2)]
                tmpA = [sb(es, f"tmpA{i}", [128, 512]) for i in range(4)]
                fS = shared[:, 8192:16384].bitcast(F32)
                tmpB = [View(fS[:, i * 512:(i + 1) * 512], f"tmpB{i}") for i in range(4)]
                bts = [sb(es, "bt0", [128, 2, 16, 128])] * 2; Wt = sb(es, "Wt", [128, 2, 16, 128])
                xri16 = sb(es, "xri16", [128, 2, 16, 128], BF16)
                m6 = [sb(es, f"m6_{i}", [128, 16]) for i in range(4)]
                yv = View(fS[:, 2048:2560], "yv"); g1t = View(fS[:, 2560:3072], "g1t"); g2t = View(fS[:, 3072:3584], "g2t"); gl16 = sb(es, "gl16", [128, 4, 128], BF16)
                ysm = View(fS[:, 3584:4096], "ysm"); sss = sb(es, "sss", [128, 1]); rss = sb(es, "rss", [128, 1])
                junkS = sb(es, "junkS", [128, 512], BF16)
                nq = [0]

                def inproj_units(c, hT):
                    return [lambda i=i: inproj_tok(c, hT, i) for i in range(3)] + [lambda ct=ct: inproj_u1(c, hT, ct) for ct in range(4)]

                def inproj_tok(c, hT, i):
                    pn = pin[i % 2]
                    for dt in range(8):
                        S.op("pe", lambda e: e.matmul(pn[:], lhsT=hT[:, dt, i * 128:(i + 1) * 128], rhs=Wu16[:, dt, :], start=(dt == 0), stop=(dt == 7)), reads=[Wu16.b, hT.b], writes=[pn.b])
                    S.op("act", lambda e: e.activation(out=utok16[c % 2][:, i, :], in_=pn[:], func=AF.Copy), reads=[pn.b], writes=[utok16[c % 2].b])

                def inproj_u1(c, hT, ct):
                    pn = pin[ct % 2]
                    for dt in range(8):
                        S.op("pe", lambda e: e.matmul(pn[:, 0:128], lhsT=Wu16[:, dt, ct * 128:(ct + 1) * 128], rhs=hT[:, dt, 384:512], start=(dt == 0), stop=(dt == 7)), reads=[Wu16.b, hT.b], writes=[pn.b])
                    S.op("act", lambda e: e.activation(out=uT16[c % 2][:, ct, :], in_=pn[:, 0:128], func=AF.Copy), reads=[pn.b], writes=[uT16[c % 2].b])
                    S.op("act", lambda e: e.activation(out=u32[c % 2][:, ct, :], in_=pn[:, 0:128], func=AF.Copy), reads=[pn.b], writes=[u32[c % 2].b])

                def skip_block(c, i):
                    if i == 0:
                        assert not pend
                        if c + 1 < NOWN:
                            pro_a(c + 1)
                            hTn, un = pro_b_units(c + 1)
                            pend.extend(un + inproj_units(c + 1, hTn))
                    sl = nq[0] % 2
                    nq[0] += 1
                    pr, pi_ = pbr[sl], pbi[sl]
                    ut = utok16[c % 2]
                    for j in range(16):
                        S.op("pe", lambda e: e.matmul(pr[:, 32 * j:32 * j + 32], lhsT=Apr16[:, j, :], rhs=ut[:, i, 32 * j:32 * j + 32], start=(j == 0), stop=(j == 15), skip_group_check=True), reads=[Apr16.b, ut.b], writes=[pr.b])
                        S.op("pe", lambda e: e.matmul(pi_[:, 32 * j:32 * j + 32], lhsT=Api16[:, j, :], rhs=ut[:, i, 32 * j:32 * j + 32], start=(j == 0), stop=(j == 15), skip_group_check=True), reads=[Api16.b, ut.b], writes=[pi_.b])
                    for _ in range(3):
                        if pend:
                            pend.pop(0)()
                    ta = tmpA if sl == 0 else tmpB
                    Bre_f = Bpre[:].rearrange("p a b -> p (a b)"); Bim_f = Bpim[:].rearrange("p a b -> p (a b)")
                    S.op("dve", lambda e: e.tensor_tensor(out=ta[0][:], in0=pr[:], in1=Bre_f, op=ALU.mult), reads=[pr.b, Bpre.b], writes=[ta[0].b])
                    S.op("dve", lambda e: e.tensor_tensor(out=ta[1][:], in0=pi_[:], in1=Bim_f, op=ALU.mult), reads=[pi_.b, Bpim.b], writes=[ta[1].b])
                    S.op("dve", lambda e: e.tensor_tensor(out=ta[2][:], in0=pi_[:], in1=Bre_f, op=ALU.mult), reads=[pi_.b, Bpre.b], writes=[ta[2].b])
                    S.op("dve", lambda e: e.tensor_tensor(out=ta[3][:], in0=pr[:], in1=Bim_f, op=ALU.mult), reads=[pr.b, Bpim.b], writes=[ta[3].b])
                    S.op("dve", lambda e: e.tensor_tensor(out=ta[0][:], in0=ta[0][:], in1=ta[1][:], op=ALU.subtract), reads=[ta[0].b, ta[1].b], writes=[ta[0].b])
                    S.op("dve", lambda e: e.tensor_tensor(out=ta[2][:], in0=ta[2][:], in1=ta[3][:], op=ALU.add), reads=[ta[2].b, ta[3].b], writes=[ta[2].b])
                    S.op("dve", lambda e: e.tensor_reduce(out=qri[:, 0, :], in_=ta[0][:].rearrange("p (a b) -> p a b", a=16), axis=AX.X, op=ALU.add), reads=[ta[0].b], writes=[qri.b])
                    S.op("dve", lambda e: e.tensor_reduce(out=qri[:, 1, :], in_=ta[2][:].rearrange("p (a b) -> p a b", a=16), axis=AX.X, op=ALU.add), reads=[ta[2].b], writes=[qri.b])
                    S.op("dve", lambda e: e.tensor_tensor(out=m8[0][:], in0=init[:, 0, :], in1=a128[:, 0, :], op=ALU.mult), reads=[init.b, a128.b], writes=[m8[0].b])
                    S.op("dve", lambda e: e.tensor_tensor(out=m8[1][:], in0=init[:, 1, :], in1=a128[:, 1, :], op=ALU.mult), reads=[init.b, a128.b], writes=[m8[1].b])
                    S.op("dve", lambda e: e.tensor_tensor(out=m8[2][:], in0=init[:, 1, :], in1=a128[:, 0, :], op=ALU.mult), reads=[init.b, a128.b], writes=[m8[2].b])
                    S.op("dve", lambda e: e.tensor_tensor(out=m8[3][:], in0=init[:, 0, :], in1=a128[:, 1, :], op=ALU.mult), reads=[init.b, a128.b], writes=[m8[3].b])
                    S.op("dve", lambda e: e.tensor_tensor(out=m8[0][:], in0=m8[0][:], in1=m8[1][:], op=ALU.subtract), reads=[m8[0].b, m8[1].b], writes=[m8[0].b])
                    S.op("dve", lambda e: e.tensor_tensor(out=m8[2][:], in0=m8[2][:], in1=m8[3][:], op=ALU.add), reads=[m8[2].b, m8[3].b], writes=[m8[2].b])
                    S.op("dve", lambda e: e.tensor_tensor(out=init[:, 0, :], in0=m8[0][:], in1=qri[:, 0, :], op=ALU.add), reads=[m8[0].b, qri.b], writes=[init.b])
                    S.op("dve", lambda e: e.tensor_tensor(out=init[:, 1, :], in0=m8[2][:], in1=qri[:, 1, :], op=ALU.add), reads=[m8[2].b, qri.b], writes=[init.b])

                def demod(g):
                    c, i = g // 4, g % 4
                    bt = bts[g % 2]
                    uT = uT16[c % 2]
                    for qd in range(4):
                        sl = nq[0] % 2
                        nq[0] += 1
                        pr, pi_ = pbr[sl], pbi[sl]
                        for jj in range(4):
                            j = 4 * qd + jj
                            S.op("pe", lambda e, j=j, jj=jj, pr=pr: e.matmul(pr[:, jj * 128:(jj + 1) * 128], lhsT=Bre16[:, j, :], rhs=uT[:, qd, :], start=(jj == 0), stop=(jj == 3), skip_group_check=True),
                                 reads=[Bre16.b, uT.b], writes=[pr.b])
                            S.op("pe", lambda e, j=j, jj=jj, pi_=pi_: e.matmul(pi_[:, jj * 128:(jj + 1) * 128], lhsT=Bim16[:, j, :], rhs=uT[:, qd, :], start=(jj == 0), stop=(jj == 3), skip_group_check=True),
                                 reads=[Bim16.b, uT.b], writes=[pi_.b])
                        for _ in range(2):
                            if pend:
                                pend.pop(0)()
                        cs = cosT[:, 4 * qd:4 * qd + 4, :].rearrange("p a b -> p (a b)")
                        sn = sinT[:, 4 * qd:4 * qd + 4, :].rearrange("p a b -> p (a b)")
                        ta = tmpA if sl == 0 else tmpB
                        S.op("dve", lambda e, pr=pr, cs=cs, ta=ta: e.tensor_tensor(out=ta[0][:], in0=pr[:], in1=cs, op=ALU.mult), reads=[pr.b, cosT.b], writes=[ta[0].b])
                        S.op("dve", lambda e, pi_=pi_, sn=sn, ta=ta: e.tensor_tensor(out=ta[1][:], in0=pi_[:], in1=sn, op=ALU.mult), reads=[pi_.b, sinT.b], writes=[ta[1].b])
                        S.op("dve", lambda e, pi_=pi_, cs=cs, ta=ta: e.tensor_tensor(out=ta[2][:], in0=pi_[:], in1=cs, op=ALU.mult), reads=[pi_.b, cosT.b], writes=[ta[2].b])
                        S.op("dve", lambda e, pr=pr, sn=sn, ta=ta: e.tensor_tensor(out=ta[3][:], in0=pr[:], in1=sn, op=ALU.mult), reads=[pr.b, sinT.b], writes=[ta[3].b])
                        S.op("pool", lambda e, qd=qd, ta=ta: e.tensor_tensor(out=bt[:, 0, 4 * qd:4 * qd + 4, :].rearrange("p a b -> p (a b)"), in0=ta[0][:], in1=ta[1][:], op=ALU.add),
                             reads=[ta[0].b, ta[1].b], writes=[bt.b])
                        S.op("pool", lambda e, qd=qd, ta=ta: e.tensor_tensor(out=bt[:, 1, 4 * qd:4 * qd + 4, :].rearrange("p a b -> p (a b)"), in0=ta[2][:], in1=ta[3][:], op=ALU.subtract),
                             reads=[ta[2].b, ta[3].b], writes=[bt.b])

                def tail(g):
                    bt = bts[g % 2]
                    S.op("dve", lambda e: e.tensor_tensor(out=m6[0][:, 0:16], in0=init[:, 0, :], in1=rr2[:, 0, :], op=ALU.mult), reads=[init.b, rr2.b], writes=[m6[0].b])
                    S.op("dve", lambda e: e.tensor_tensor(out=m6[1][:, 0:16], in0=init[:, 1, :], in1=rr2[:, 1, :], op=ALU.mult), reads=[init.b, rr2.b], writes=[m6[1].b])
                    S.op("dve", lambda e: e.tensor_tensor(out=bt[:, 0, :, 0], in0=bt[:, 0, :, 0], in1=m6[0][:], op=ALU.add), reads=[bt.b, m6[0].b], writes=[bt.b])
                    S.op("dve", lambda e: e.tensor_tensor(out=bt[:, 1, :, 0], in0=bt[:, 1, :, 0], in1=m6[1][:], op=ALU.add), reads=[bt.b, m6[1].b], writes=[bt.b])
                    Rf = Rt[:].rearrange("p a b -> p (a b)")
                    for ri in range(2):
                        S.op("dve", lambda e, ri=ri: e.tensor_tensor_scan(out=Wt[:, ri, :, :].rearrange("p a b -> p (a b)"), data0=Rf, data1=bt[:, ri, :, :].rearrange("p a b -> p (a b)"), initial=0.0, op0=ALU.mult, op1=ALU.add),
                             reads=[Rt.b, bt.b], writes=[Wt.b])
                    wr, wi = Wt[:, 0, :, 127], Wt[:, 1, :, 127]
                    c128, s128 = cosT[:, :, 127], sinT[:, :, 127]
                    S.op("dve", lambda e: e.tensor_tensor(out=m6[0][:], in0=wr, in1=c128, op=ALU.mult), reads=[Wt.b, cosT.b], writes=[m6[0].b])
                    S.op("dve", lambda e: e.tensor_tensor(out=m6[1][:], in0=wi, in1=s128, op=ALU.mult), reads=[Wt.b, sinT.b], writes=[m6[1].b])
                    S.op("dve", lambda e: e.tensor_tensor(out=m6[2][:], in0=wi, in1=c128, op=ALU.mult), reads=[Wt.b, cosT.b], writes=[m6[2].b])
                    S.op("dve", lambda e: e.tensor_tensor(out=m6[3][:], in0=wr, in1=s128, op=ALU.mult), reads=[Wt.b, sinT.b], writes=[m6[3].b])
                    S.op("dve", lambda e: e.tensor_tensor(out=init[:, 0, :], in0=m6[0][:], in1=m6[1][:], op=ALU.subtract), reads=[m6[0].b, m6[1].b], writes=[init.b])
                    S.op("dve", lambda e: e.tensor_tensor(out=init[:, 1, :], in0=m6[2][:], in1=m6[3][:], op=ALU.add), reads=[m6[2].b, m6[3].b], writes=[init.b])

                def own_block(c):
                    ob = c
                    for qd in range(4):
                        cs = cosT[:, 4 * qd:4 * qd + 4, :].rearrange("p a b -> p (a b)")
                        sn = sinT[:, 4 * qd:4 * qd + 4, :].rearrange("p a b -> p (a b)")
                        wrq = Wt[:, 0, 4 * qd:4 * qd + 4, :].rearrange("p a b -> p (a b)")
                        wiq = Wt[:, 1, 4 * qd:4 * qd + 4, :].rearrange("p a b -> p (a b)")
                        ta = tmpA if qd % 2 == 0 else tmpB
                        S.op("dve", lambda e, ta=ta, wrq=wrq, cs=cs: e.tensor_tensor(out=ta[0][:], in0=wrq, in1=cs, op=ALU.mult), reads=[Wt.b, cosT.b], writes=[ta[0].b])
                        S.op("dve", lambda e, ta=ta, wiq=wiq, sn=sn: e.tensor_tensor(out=ta[1][:], in0=wiq, in1=sn, op=ALU.mult), reads=[Wt.b, sinT.b], writes=[ta[1].b])
                        S.op("dve", lambda e, ta=ta, wiq=wiq, cs=cs: e.tensor_tensor(out=ta[2][:], in0=wiq, in1=cs, op=ALU.mult), reads=[Wt.b, cosT.b], writes=[ta[2].b])
                        S.op("dve", lambda e, ta=ta, wrq=wrq, sn=sn: e.tensor_tensor(out=ta[3][:], in0=wrq, in1=sn, op=ALU.mult), reads=[Wt.b, sinT.b], writes=[ta[3].b])
                        S.op("dve", lambda e, ta=ta, qd=qd: e.tensor_tensor(out=xri16[:, 0, 4 * qd:4 * qd + 4, :].rearrange("p a b -> p (a b)"), in0=ta[0][:], in1=ta[1][:], op=ALU.subtract),
                             reads=[ta[0].b, ta[1].b], writes=[xri16.b])
                        S.op("dve", lambda e, ta=ta, qd=qd: e.tensor_tensor(out=xri16[:, 1, 4 * qd:4 * qd + 4, :].rearrange("p a b -> p (a b)"), in0=ta[2][:], in1=ta[3][:], op=ALU.add),
                             reads=[ta[2].b, ta[3].b], writes=[xri16.b])
                    for ct in range(4):
                        n = 0
                        for jj in range(4):
                            j = 4 * ct + jj
                            for (C_, ri) in ((Cre16, 0), (Cim16, 1)):
                                S.op("pe", lambda e, ct=ct, j=j, C_=C_, ri=ri, n=n: e.matmul(py[:, ct * 128:(ct + 1) * 128], lhsT=C_[:, j, :], rhs=xri16[:, ri, j, :], start=(n == 0 and ct == 0), stop=(n == 7 and ct == 3), skip_group_check=True),
                                     reads=[C_.b, xri16.b], writes=[py.b])
                                n += 1
                    for ct in range(4):
                        S.op("dve", lambda e, ct=ct: e.scalar_tensor_tensor(out=yv[:, ct * 128:(ct + 1) * 128], in0=u32[c % 2][:, ct, :], scalar=dsk[:, ct:ct + 1], in1=py[:, ct * 128:(ct + 1) * 128], op0=ALU.mult, op1=ALU.add),
                             reads=[u32[c % 2].b, dsk.b, py.b], writes=[yv.b])
                    dump(f"ypre{ob}", yv[:], yv.b, [128, 512])

                def own_B(c):
                    ob = c
                    S.op("dve", lambda e: e.tensor_tensor(out=g1t[:], in0=yv[:], in1=yv[:], op=ALU.mult), reads=[yv.b], writes=[g1t.b])
                    S.op("dve", lambda e: e.tensor_scalar(out=g1t[:], in0=g1t[:], scalar1=0.044715, scalar2=1.0, op0=ALU.mult, op1=ALU.add), reads=[g1t.b], writes=[g1t.b])
                    S.op("dve", lambda e: e.tensor_tensor(out=g1t[:], in0=g1t[:], in1=yv[:], op=ALU.mult), reads=[g1t.b, yv.b], writes=[g1t.b])
                    S.op("act", lambda e: e.activation(out=g2t[:], in_=g1t[:], func=AF.Sigmoid, scale=2.0 * GELU_C), reads=[g1t.b], writes=[g2t.b])
                    S.op("dve", lambda e: e.tensor_tensor(out=gl16[:].rearrange("p a b -> p (a b)"), in0=yv[:], in1=g2t[:], op=ALU.mult), reads=[yv.b, g2t.b], writes=[gl16.b])
                    for half in range(2):
                        for ct in range(4):
                            S.op("pe", lambda e, half=half, ct=ct: e.matmul(pin[half][:], lhsT=gl16[:, ct, :], rhs=Wglu16[:, ct, half * 512:(half + 1) * 512], start=(ct == 0), stop=(ct == 3)),
                                 reads=[gl16.b, Wglu16.b], writes=[pin[half].b])

                def own_B2(c):
                    ob = c
                    S.op("act", lambda e: e.activation(out=g1t[:], in_=pin[1][:], func=AF.Sigmoid), reads=[pin[1].b], writes=[g1t.b])
                    S.op("dve", lambda e: e.tensor_tensor(out=ysm[:], in0=pin[0][:], in1=g1t[:], op=ALU.mult), reads=[pin[0].b, g1t.b], writes=[ysm.b])
                    dump(f"yssm{ob}", ysm[:], ysm.b, [128, 512])
                    S.op("act", lambda e: e.activation(out=junkS[:], in_=ysm[:], func=AF.Square, accum_out=sss[:, 0:1]), reads=[ysm.b], writes=[junkS.b, sss.b])
                    S.op("act", lambda e: e.activation(out=rss[:], in_=sss[:], func=AF.Ln, scale=1.0 / 512, bias=EPS), reads=[sss.b], writes=[rss.b])
                    S.op("act", lambda e: e.activation(out=rss[:], in_=rss[:], func=AF.Exp, scale=-0.5), reads=[rss.b], writes=[rss.b])
                    S.op("act", lambda e: e.activation(out=mssm16[:, ob, :], in_=ysm[:], func=AF.Copy, scale=rss[:, 0:1]), reads=[ysm.b, rss.b], writes=[mssm16.b])

                if upto == "setup":
                    return
                pro_a(0)
                hT0, un = pro_b_units(0)
                for u_ in un + inproj_units(0, hT0):
                    u_()
                nch_ = NOWN if upto is None else 2
                done_ = set()

                def do_skip(c, i):
                    if (c, i) not in done_:
                        done_.add((c, i))
                        skip_block(c, i)
                for c in range(nch_):
                    for i in range(3):
                        do_skip(c, i)
                    demod(4 * c + 3)
                    tail(4 * c + 3)
                    while pend:
                        pend.pop(0)()
                    own_block(c)
                    if c + 1 < nch_:
                        do_skip(c + 1, 0)
                    own_B(c)
                    if c + 1 < nch_:
                        do_skip(c + 1, 1)
                    own_B2(c)
                S.barrier()

        def phase_A(hs, pidx):
            es = ExitStack()
            with es:
                stgs = [sb(es, f"stgA{pidx}_{i}", [128, 256]) for i in range(3)]
                W16 = load_win(es, f"Wqkv16_{pidx}", [(64 * hs, 256, 0.125), (512 + 64 * hs, 256, 1.0), (1024 + 64 * hs, 256, 1.0)], stgs)
                kT = sb(es, f"kT{pidx}", [128, 2, NTOK], BF16)
                vS = sb(es, f"vS{pidx}", [128, NBLK, 256], BF16)
                kb_ = [Buf(f"kT_c{c}") for c in range(NOWN)]
                vb_ = [Buf(f"vS_c{c}") for c in range(NOWN)]
                qT = [sb(es, f"qT{pidx}_{i}", [128, 2, 128], BF16) for i in range(2)]
                pin = [ps(es, f"pinA{pidx}_{i}") for i in range(2)]
                pA = [ps(es, f"pA{pidx}_{i}") for i in range(4)]
                pO = ps(es, f"pO{pidx}")
                E32 = [sb(es, f"E32_{pidx}_{i}", [128, 512]) for i in range(4)]
                Lp = [sb(es, f"Lp{pidx}_{i}", [128, 512], BF16) for i in range(4)]
                AT = [sb(es, f"AT{pidx}_{i}", [128, 512], BF16) for i in range(4)]
                Gs = [sb(es, f"Gs{pidx}_{i}", [128, 128], BF16) for i in range(4)]
                Ls = [sb(es, f"Ls{pidx}_{i}", [128, 512], BF16) for i in range(4)]
                gt1 = sb(es, f"gt1_{pidx}", [128, 256], BF16); gt2 = sb(es, f"gt2_{pidx}", [128, 128], BF16)
                ysraw = sb(es, f"ysraw{pidx}", [128, 256]); junkA = sb(es, f"junkA{pidx}", [128, 256])
                pro_a, pro_b_units, PP = make_prologue(es, "dve", "dve", extra_pT=pin)
                deferred = []
                PP["defer"] = deferred
                pend = []

                def inproj_units(c, hT):
                    return [lambda ft=ft, h=h: inproj_k(c, hT, ft, h) for ft in range(2) for h in range(2)] + [lambda i=i, h=h: inproj_v(c, hT, i, h) for i in range(4) for h in range(2)] + [lambda: inproj_q(c, hT, 0), lambda: inproj_q(c, hT, 1)]

                def inproj_k(c, hT, ft, h):
                    if True:
                        pn = pin[ft]
                        for dt in range(4 * h, 4 * h + 4):
                            S.op("pe", lambda e: e.matmul(pn[:], lhsT=W16[:, dt, 256 + ft * 128:256 + (ft + 1) * 128], rhs=hT[:, dt, :], start=(dt == 0), stop=(dt == 7)),
                                 reads=[W16.b, hT.b], writes=[pn.b])
                        if h == 1:
                            deferred.append(lambda: S.op("dve", lambda e: e.tensor_copy(out=kT[:, ft, c * 512:(c + 1) * 512], in_=pn[:]), reads=[pn.b], writes=[kb_[c]]))

                def inproj_v(c, hT, i, h):
                    if True:
                        pn = pin[i % 2]
                        for dt in range(4 * h, 4 * h + 4):
                            S.op("pe", lambda e: e.matmul(pn[:, 0:256], lhsT=hT[:, dt, i * 128:(i + 1) * 128], rhs=W16[:, dt, 512:768], start=(dt == 0), stop=(dt == 7)),
                                 reads=[W16.b, hT.b], writes=[pn.b])
                        if h == 1:
                            deferred.append(lambda: S.op("dve", lambda e: e.tensor_copy(out=vS[:, 4 * c + i, :], in_=pn[:, 0:256]), reads=[pn.b], writes=[vb_[c]]))

                def inproj_q(c, hT, ft):
                    q = qT[c % 2]
                    if True:
                        pn = pin[ft]
                        for dt in range(8):
                            S.op("pe", lambda e: e.matmul(pn[:, 0:128], lhsT=W16[:, dt, ft * 128:(ft + 1) * 128], rhs=hT[:, dt, 384:512], start=(dt == 0), stop=(dt == 7)),
                                 reads=[W16.b, hT.b], writes=[pn.b])
                        deferred.append(lambda: S.op("dve", lambda e: e.tensor_copy(out=q[:, ft, :], in_=pn[:, 0:128]), reads=[pn.b], writes=[q.b]))

                def attention(c):
                    q = qT[c % 2]
                    tasks = [(hl, m) for hl in range(4) for m in range(c, -1, -1)]
                    st = {"n": 0}

                    def stage1(n):
                        hl, m = tasks[n]
                        sl = n % 4
                        p0 = 64 * (hl % 2)
                        ft = hl // 2
                        first = (m == c)
                        for i in range(4):
                            kb = 4 * m + i
                            S.op("pe", lambda e: e.matmul(pA[sl][:, i * 128:(i + 1) * 128], lhsT=kT[p0:p0 + 64, ft, kb * 128:(kb + 1) * 128], rhs=q[p0:p0 + 64, ft, :], start=(i == 0), stop=False, skip_group_check=True),
                                 reads=[kb_[m], q.b], writes=[pA[sl].b])
                        S.op("act", lambda e: e.activation(out=E32[sl][:], in_=pA[sl][:], func=AF.Exp), reads=[pA[sl].b], writes=[E32[sl].b])

                    def stage1b(n):
                        hl, m = tasks[n]
                        sl = n % 4
                        first = (m == c)
                        S.op("act", lambda e: e.activation(out=Lp[sl][:], in_=E32[sl][:], func=AF.Ln, bias=1.0, scale=1.0), reads=[E32[sl].b], writes=[Lp[sl].b])
                        if first:
                            S.op("dve", lambda e: e.tensor_tensor(out=Lp[sl][:, 384:512], in0=Lp[sl][:, 384:512], in1=strict16[:], op=ALU.mult), reads=[Lp[sl].b, strict16.b], writes=[Lp[sl].b])
                        L_ = Ls[sl]
                        Ln_ = Ls[(n + 1) % 4]
                        if first:
                            S.op("dve", lambda e: e.tensor_copy(out=L_[:, 256:384], in_=Lp[sl][:, 384:512]), reads=[Lp[sl].b], writes=[L_.b])
                        else:
                            S.op("dve", lambda e: e.tensor_tensor(out=L_[:, 256:384], in0=Lp[sl][:, 384:512], in1=L_[:, 384:512], op=ALU.add), reads=[Lp[sl].b, L_.b], writes=[L_.b])
                        S.op("dve", lambda e: e.tensor_tensor(out=L_[:, 128:256], in0=L_[:, 256:384], in1=Lp[sl][:, 256:384], op=ALU.add), reads=[Lp[sl].b, L_.b], writes=[L_.b])
                        S.op("dve", lambda e: e.tensor_tensor(out=L_[:, 0:128], in0=L_[:, 128:256], in1=Lp[sl][:, 128:256], op=ALU.add), reads=[Lp[sl].b, L_.b], writes=[L_.b])
                        if m > 0:
                            S.op("dve", lambda e: e.tensor_tensor(out=Ln_[:, 384:512], in0=L_[:, 0:128], in1=Lp[sl][:, 0:128], op=ALU.add), reads=[Lp[sl].b, L_.b], writes=[Ln_.b])

                    def stage2(n):
                        hl, m = tasks[n]
                        sl = n % 4
                        first = (m == c)
                        nmm = 2
                        cnt = [0]

                        def mm(out_ap, lhsT_ap, rhs_ap, rbufs):
                            cnt[0] += 1
                            S.op("pe", lambda e: e.matmul(out_ap, lhsT=lhsT_ap, rhs=rhs_ap, start=False, stop=(cnt[0] == nmm), skip_group_check=True), reads=rbufs, writes=[pA[sl].b])
                        mm(pA[sl][:], negtri16[:], Lp[sl][:], [negtri16.b, Lp[sl].b])
                        if first:
                            mm(pA[sl][:, 0:384], negones16[:], Ls[sl][:, 0:384], [negones16.b, Ls[sl].b])
                        else:
                            mm(pA[sl][:], negones16[:], Ls[sl][:], [negones16.b, Ls[sl].b])
                        S.op("act", lambda e: e.activation(out=AT[sl][:], in_=pA[sl][:], func=AF.Exp), reads=[pA[sl].b], writes=[AT[sl].b])
                        if first:
                            S.op("dve", lambda e: e.tensor_tensor(out=AT[sl][:, 384:512], in0=AT[sl][:, 384:512], in1=strict16[:], op=ALU.mult), reads=[AT[sl].b, strict16.b], writes=[AT[sl].b])

                    def stage3(n):
                        hl, m = tasks[n]
                        sl = n % 4
                        first = (m == c)
                        for i in range(4):
                            kb = 4 * m + i
                            S.op("pe", lambda e: e.matmul(pO[:, hl * 64:(hl + 1) * 64], lhsT=AT[sl][:, i * 128:(i + 1) * 128], rhs=vS[:, kb, hl * 64:(hl + 1) * 64], start=(first and i == 0 and hl == 0), stop=(m == 0 and i == 3 and hl == 3), skip_group_check=True),
                                 reads=[AT[sl].b, vb_[m]], writes=[pO.b])

                    NT = len(tasks)
                    for it in range(NT + 3):
                        while deferred:
                            deferred.pop(0)()
                        if it < NT:
                            stage1(it)
                        if pend:
                            pend.pop(0)()
                        if 0 <= it - 2 < NT:
                            stage2(it - 2)
                        if it < NT:
                            stage1b(it)
                        if 0 <= it - 3 < NT:
                            stage3(it - 3)
                    S.op("dve", lambda e: e.tensor_copy(out=ysraw[:], in_=pO[:, 0:256]), reads=[pO.b], writes=[ysraw.b])
                    dump(f"ysb{pidx}_{c}", ysraw[:], ysraw.b, [128, 256])
                    S.op("dve", lambda e: e.scalar_tensor_tensor(out=junkA[:], in0=ysraw[:], scalar=1.0, in1=ysraw[:], op0=ALU.mult, op1=ALU.mult, accum_out=ss_sb[:, c, pidx:pidx + 1]),
                         reads=[ysraw.b], writes=[junkA.b, ss_sb.b])
                    S.op("pool", lambda e: e.tensor_copy(out=ysb16[:, c, 64 * hs:64 * hs + 256], in_=ysraw[:]), reads=[ysraw.b], writes=[ysb16.b])

                nch = NOWN if upto is None else int(upto)
                pro_a(0)
                hT0, un = pro_b_units(0)
                for u_ in un + inproj_units(0, hT0):
                    u_()
                    while deferred:
                        deferred.pop(0)()
                for c in range(nch):
                    if c + 1 < nch:
                        pro_a(c + 1)
                        hTn, un = pro_b_units(c + 1)
                        pend.extend(un + inproj_units(c + 1, hTn))
                    attention(c)
                    while pend:
                        pend.pop(0)()
                        while deferred:
                            deferred.pop(0)()
                    while deferred:
                        deferred.pop(0)()
                S.barrier()

        def phase_F(R):
            es = ExitStack()
            with es:
                acc, xnT16, Wgt = R["acc"], R["xnT16"], R["Wgt"]
                stgF = [sb(es, f"stgF{i}", [128, 1024]) for i in range(2)]
                gcat = sb(es, "gcat", [128, 8]); load(gcat, gcat[:], gcat_d)
                g2 = R["g2"]
                Wout16 = sb(es, "Wout16", [128, 8, 1024], BF16)
                for k in range(8):
                    stg = stgF[k % 2]
                    load(stg, stg[:], w_out[k * 128:(k + 1) * 128, :])
                    S.op("act", lambda e: e.activation(out=Wout16[:, k, :], in_=stg[:], func=AF.Copy, scale=gcat[:, k:k + 1]), reads=[stg.b, gcat.b], writes=[Wout16.b])
                Wr16 = sb(es, "Wr16", [128, 8, 32], BF16)
                for k in range(8):
                    stg = stgF[k % 2]
                    load(stg, stg[:, 0:32], w_router[k * 128:(k + 1) * 128, :])
                    S.op("act", lambda e: e.activation(out=Wr16[:, k, :], in_=stg[:, 0:32], func=AF.Copy, scale=g2[:, k:k + 1]), reads=[stg.b, g2.b], writes=[Wr16.b])
                browb = sb(es, "browb", [128, 32]); load(browb, browb[:], brow_d.partition_broadcast(128))
                bd32 = sb(es, "bd32", [32, 1024]); load(bd32, bd32[:], bd_d)
                def two(name, shape, dt=F32):
                    return [sb(es, f"{name}{i}", shape, dt) for i in range(2)]
                xb_ = two("xbF", [128, 1024]); mix16_ = two("mix16", [128, 512], BF16); mixT_ = two("mixT", [128, 8, 128], BF16)
                sst_ = two("sstF", [128, 1]); rst_ = two("rstF", [128, 1]); junk = sb(es, "junkF", [128, 1024], BF16)
                xn16_ = two("xn16", [128, 1024], BF16)
                lg_ = two("lg", [128, 32]); mx8_ = two("mx8", [128, 8]); msk_ = two("msk", [128, 32]); nmx_ = two("nmx", [128, 1])
                ex_ = two("exF", [128, 32]); den_ = two("denF", [128, 1]); WgtT_ = two("WgtT", [32, 128])
                pT_ = [ps(es, f"pTF{i}", [128, 1024], BF16) for i in range(2)]
                pd = [ps(es, f"pdF{i}") for i in range(2)]
                pl = ps(es, "plF"); pw = ps(es, "pwF")
                pbs = [ps(es, f"pbF{i}") for i in range(2)]
                for ob in range(NOWN):
                    z_ = ob % 2
                    xb, mix16, mixT, sst, rst, xn16 = xb_[z_], mix16_[z_], mixT_[z_], sst_[z_], rst_[z_], xn16_[z_]
                    lg, mx8, msk, nmx, ex, den, WgtT, pT = lg_[z_], mx8_[z_], msk_[z_], nmx_[z_], ex_[z_], den_[z_], WgtT_[z_], pT_[z_]
                    load(xb, xb[:], xv[(4 * ob + 3) * 128:(4 * ob + 4) * 128, :])
                    S.op("dve", lambda e: e.tensor_tensor(out=sst[:], in0=ss_sb[:, ob, 0:1], in1=ss_sb[:, ob, 1:2], op=ALU.add), reads=[ss_sb.b], writes=[sst.b])
                    S.op("act", lambda e: e.activation(out=rst[:], in_=sst[:], func=AF.Ln, scale=1.0 / 512, bias=EPS), reads=[sst.b], writes=[rst.b])
                    S.op("act", lambda e: e.activation(out=rst[:], in_=rst[:], func=AF.Exp, scale=-0.5), reads=[rst.b], writes=[rst.b])
                    S.op("act", lambda e: e.activation(out=mix16[:], in_=ysb16[:, ob, :], func=AF.Copy, scale=rst[:, 0:1]), reads=[ysb16.b, rst.b], writes=[mix16.b])
                    for k in range(8):
                        src = mix16[:, k * 128:(k + 1) * 128] if k < 4 else mssm16[:, ob, (k - 4) * 128:(k - 3) * 128]
                        S.op("pe", lambda e: e.transpose(pT[:, k * 128:(k + 1) * 128], src, ident16[:]), reads=[mix16.b, mssm16.b, ident16.b], writes=[pT.b])
                    S.op("dve", lambda e: e.tensor_copy(out=mixT[:].rearrange("p a b -> p (a b)"), in_=pT[:]), reads=[pT.b], writes=[mixT.b])
                    for half in range(2):
                        for k in range(8):
                            S.op("pe", lambda e: e.matmul(pd[half][:], lhsT=mixT[:, k, :], rhs=Wout16[:, k, half * 512:(half + 1) * 512], start=(k == 0), stop=(k == 7)),
                                 reads=[mixT.b, Wout16.b], writes=[pd[half].b])
                        S.op("dve", lambda e: e.tensor_tensor(out=acc[:, ob, half * 512:(half + 1) * 512], in0=pd[half][:], in1=xb[:, half * 512:(half + 1) * 512], op=ALU.add),
                             reads=[pd[half].b, xb.b], writes=[acc.b])
                    dump(f"x1_{ob}", acc[:, ob, :], acc.b, [128, 1024])
                    S.op("act", lambda e: e.activation(out=junk[:], in_=acc[:, ob, :], func=AF.Square, accum_out=sst[:, 0:1]), reads=[acc.b], writes=[junk.b, sst.b])
                    S.op("act", lambda e: e.activation(out=rst[:], in_=sst[:], func=AF.Ln, scale=1.0 / 1024, bias=EPS), reads=[sst.b], writes=[rst.b])
                    S.op("act", lambda e: e.activation(out=rst[:], in_=rst[:], func=AF.Exp, scale=-0.5), reads=[rst.b], writes=[rst.b])
                    S.op("act", lambda e: e.activation(out=xn16[:], in_=acc[:, ob, :], func=AF.Copy, scale=rst[:, 0:1]), reads=[acc.b, rst.b], writes=[xn16.b])
                    for k in range(8):
                        S.op("pe", lambda e: e.transpose(pT[:, k * 128:(k + 1) * 128], xn16[:, k * 128:(k + 1) * 128], ident16[:]), reads=[xn16.b, ident16.b], writes=[pT.b])
                    S.op("dve", lambda e: e.tensor_copy(out=xnT16[:, :, ob * 128:(ob + 1) * 128], in_=pT[:].rearrange("p (a b) -> p a b", a=8)), reads=[pT.b], writes=[xnT16.b])
                    for k in range(8):
                        S.op("pe", lambda e: e.matmul(pl[:, 0:32], lhsT=xnT16[:, k, ob * 128:(ob + 1) * 128], rhs=Wr16[:, k, :], start=(k == 0), stop=(k == 7)), reads=[xnT16.b, Wr16.b], writes=[pl.b])
                    S.op("dve", lambda e: e.tensor_tensor(out=lg[:], in0=pl[:, 0:32], in1=browb[:], op=ALU.add), reads=[pl.b, browb.b], writes=[lg.b])
                    dump(f"lg_{ob}", lg[:], lg.b, [128, 32])
                    S.op("dve", lambda e: e.max(out=mx8[:], in_=lg[:]), reads=[lg.b], writes=[mx8.b])
                    S.op("dve", lambda e: e.tensor_scalar(out=msk[:], in0=lg[:], scalar1=mx8[:, 3:4], scalar2=None, op0=ALU.is_ge), reads=[lg.b, mx8.b], writes=[msk.b])
                    S.op("dve", lambda e: e.tensor_scalar(out=nmx[:], in0=mx8[:, 0:1], scalar1=-1.0, scalar2=None, op0=ALU.mult), reads=[mx8.b], writes=[nmx.b])
                    S.op("act", lambda e: e.activation(out=ex[:], in_=lg[:], func=AF.Exp, bias=nmx[:, 0:1], scale=1.0), reads=[lg.b, nmx.b], writes=[ex.b])
                    S.op("dve", lambda e: e.scalar_tensor_tensor(out=ex[:], in0=ex[:], scalar=1.0, in1=msk[:], op0=ALU.mult, op1=ALU.mult, accum_out=den[:, 0:1]), reads=[ex.b, msk.b], writes=[ex.b, den.b])
                    S.op("dve", lambda e: e.reciprocal(out=den[:], in_=den[:]), reads=[den.b], writes=[den.b])
                    S.op("dve", lambda e: e.tensor_scalar(out=Wgt[:, ob, :], in0=ex[:], scalar1=den[:, 0:1], scalar2=None, op0=ALU.mult), reads=[ex.b, den.b], writes=[Wgt.b])
                    S.op("pe", lambda e: e.transpose(pw[0:32, 0:128], Wgt[:, ob, :], ident32[:]), reads=[Wgt.b, ident32.b], writes=[pw.b])
                    S.op("dve", lambda e: e.tensor_copy(out=WgtT[:], in_=pw[0:32, 0:128]), reads=[pw.b], writes=[WgtT.b])
                    for half in range(2):
                        S.op("pe", lambda e: e.matmul(pbs[half][:], lhsT=WgtT[:], rhs=bd32[:, half * 512:(half + 1) * 512], start=True, stop=True), reads=[WgtT.b, bd32.b], writes=[pbs[half].b])
                        S.op("dve", lambda e: e.tensor_tensor(out=acc[:, ob, half * 512:(half + 1) * 512], in0=pbs[half][:], in1=acc[:, ob, half * 512:(half + 1) * 512], op=ALU.add),
                             reads=[pbs[half].b, acc.b], writes=[acc.b])
                S.barrier()

        def phase_M(R):
            es = ExitStack()
            with es:
                acc, xnT16, Wgt, g2 = R["acc"], R["xnT16"], R["Wgt"], R["g2"]
                bgT = sb(es, "bgT", [128, 32, 8]); load(bgT, bgT[:], bgT_d)
                buT = sb(es, "buT", [128, 32, 8]); load(buT, buT[:], buT_d)
                Wg16 = sb(es, "Wg16", [128, 8, 1024], BF16); Wu16 = sb(es, "Wu16m", [128, 8, 1024], BF16); Wd16 = sb(es, "Wd16", [128, 8, 1024], BF16)
                stg = [sb(es, f"stgM{i}", [128, 1024]) for i in range(2)]
                hT = View(shared[:, 0:16384].rearrange("p (a b) -> p a b", a=8), "hTm")
                hb = [Buf(f"hTm_t{i}") for i in range(4)]
                g32 = [sb(es, f"g32_{i}", [128, 512]) for i in range(2)]; s32 = [sb(es, f"s32_{i}", [128, 512]) for i in range(2)]
                u32 = [sb(es, f"u32m_{i}", [128, 512]) for i in range(2)]
                pg = [ps(es, f"pg{i}") for i in range(3)]; pu = [ps(es, f"pu{i}") for i in range(3)]; pdn = [ps(es, f"pdn{i}") for i in range(2)]
                nst = [0]
                K17 = 1.0 / 1.702
                g2s = sb(es, "g2s", [128, 8])
                S.op("dve", lambda e: e.tensor_scalar(out=g2s[:], in0=g2[:], scalar1=K17, scalar2=None, op0=ALU.mult), reads=[g2.b], writes=[g2s.b])
                S.op("dve", lambda e: e.tensor_scalar(out=buT[:], in0=buT[:], scalar1=K17, scalar2=None, op0=ALU.mult), reads=[buT.b], writes=[buT.b])

                def w_pieces(dst, src_e, scaled):
                    def piece(dt):
                        st = stg[nst[0] % 2]
                        nst[0] += 1
                        load(st, st[:], src_e[dt * 128:(dt + 1) * 128, :])
                        if scaled is not None:
                            S.op("act", lambda e: e.activation(out=dst[:, dt, :], in_=st[:], func=AF.Copy, scale=scaled[:, dt:dt + 1]), reads=[st.b, scaled.b], writes=[dst.b])
                        else:
                            S.op("act", lambda e: e.activation(out=dst[:, dt, :], in_=st[:], func=AF.Copy), reads=[st.b], writes=[dst.b])
                    return [lambda dt=dt: piece(dt) for dt in range(8)]

                ngu = [0]
                ndn = [0]
                pendw = []

                def gate_up(e_):
                    for tt in range(4):
                        t0 = tt * 512
                        for ft in range(8):
                            sl = ngu[0] % 3
                            s2 = ngu[0] % 2
                            ngu[0] += 1
                            for dt in range(8):
                                S.op("pe", lambda e: e.matmul(pg[sl][:], lhsT=Wg16[:, dt, ft * 128:(ft + 1) * 128], rhs=xnT16[:, dt, t0:t0 + 512], start=(dt == 0), stop=(dt == 7)), reads=[Wg16.b, xnT16.b], writes=[pg[sl].b])
                            for dt in range(8):
                                S.op("pe", lambda e: e.matmul(pu[sl][:], lhsT=Wu16[:, dt, ft * 128:(ft + 1) * 128], rhs=xnT16[:, dt, t0:t0 + 512], start=(dt == 0), stop=(dt == 7)), reads=[Wu16.b, xnT16.b], writes=[pu[sl].b])
                            S.op("dve", lambda e: e.tensor_scalar(out=g32[s2][:], in0=pg[sl][:], scalar1=bgT[:, e_, ft:ft + 1], scalar2=7.0, op0=ALU.add, op1=ALU.min), reads=[pg[sl].b, bgT.b], writes=[g32[s2].b])
                            S.op("act", lambda e: e.activation(out=s32[s2][:], in_=g32[s2][:], func=AF.Silu, scale=1.702), reads=[g32[s2].b], writes=[s32[s2].b])
                            S.op("dve", lambda e: e.tensor_scalar(out=u32[s2][:], in0=pu[sl][:], scalar1=buT[:, e_, ft:ft + 1], scalar2=7.0 * K17, op0=ALU.add, op1=ALU.min), reads=[pu[sl].b, buT.b], writes=[u32[s2].b])
                            S.op("dve", lambda e: e.tensor_scalar(out=u32[s2][:], in0=u32[s2][:], scalar1=-7.0 * K17, scalar2=K17, op0=ALU.max, op1=ALU.add), reads=[u32[s2].b], writes=[u32[s2].b])
                            S.op("dve", lambda e: e.tensor_tensor(out=hT[:, ft, t0:t0 + 512], in0=s32[s2][:], in1=u32[s2][:], op=ALU.mult), reads=[s32[s2].b, u32[s2].b], writes=[hb[tt]])
                            if pendw and (ngu[0] % 2 == 0):
                                pendw.pop(0)()

                def down(e_):
                    for ob in range(NOWN):
                        for hf in range(2):
                            sl = ndn[0] % 2
                            ndn[0] += 1
                            for ft in range(8):
                                S.op("pe", lambda e: e.matmul(pdn[sl][:], lhsT=hT[:, ft, ob * 128:(ob + 1) * 128], rhs=Wd16[:, ft, hf * 512:(hf + 1) * 512], start=(ft == 0), stop=(ft == 7)), reads=[hb[ob // 4], Wd16.b], writes=[pdn[sl].b])
                            S.op("dve", lambda e: e.scalar_tensor_tensor(out=acc[:, ob, hf * 512:(hf + 1) * 512], in0=pdn[sl][:], scalar=Wgt[:, ob, e_:e_ + 1], in1=acc[:, ob, hf * 512:(hf + 1) * 512], op0=ALU.mult, op1=ALU.add),
                                 reads=[pdn[sl].b, Wgt.b, acc.b], writes=[acc.b])

                for p_ in w_pieces(Wg16, w_gate[0], g2) + w_pieces(Wu16, w_up[0], g2s):
                    p_()
                for e_ in range(n_experts):
                    pendw.extend(w_pieces(Wd16, w_down[e_], None))
                    gate_up(e_)
                    while pendw:
                        pendw.pop(0)()
                    if e_ + 1 < n_experts:
                        for p_ in w_pieces(Wg16, w_gate[e_ + 1], g2) + w_pieces(Wu16, w_up[e_ + 1], g2s):
                            p_()
                    down(e_)
                S.barrier()

        def phase_out(R):
            es = ExitStack()
            with es:
                acc = R["acc"]
                gfb = sb(es, "gfb", [128, 1024]); load(gfb, gfb[:], gf_d.partition_broadcast(128))
                junk = sb(es, "junkO", [128, 1024], BF16); sst = sb(es, "sstO", [128, 1]); rst = sb(es, "rstO", [128, 1])
                ob_t = [sb(es, f"obuf{i}", [128, 1024]) for i in range(2)]
                for ob in range(NOWN):
                    o = ob_t[ob % 2]
                    dump(f"x2_{ob}", acc[:, ob, :], acc.b, [128, 1024])
                    S.op("act", lambda e: e.activation(out=junk[:], in_=acc[:, ob, :], func=AF.Square, accum_out=sst[:, 0:1]), reads=[acc.b], writes=[junk.b, sst.b])
                    S.op("act", lambda e: e.activation(out=rst[:], in_=sst[:], func=AF.Ln, scale=1.0 / 1024, bias=EPS), reads=[sst.b], writes=[rst.b])
                    S.op("act", lambda e: e.activation(out=rst[:], in_=rst[:], func=AF.Exp, scale=-0.5), reads=[rst.b], writes=[rst.b])
                    S.op("dve", lambda e: e.scalar_tensor_tensor(out=o[:], in0=acc[:, ob, :], scalar=rst[:, 0:1], in1=gfb[:], op0=ALU.mult, op1=ALU.mult), reads=[acc.b, rst.b, gfb.b], writes=[o.b])
                    S.dma(lambda e: e.dma_start(out=out_d[ob * 128:(ob + 1) * 128, :], in_=o[:]), reads=[o.b])

        def finish():
            S.finish()
            with nc.Block() as block:
                S.emit(block)
            return nc, dbg_d

        phases = stop_after or "SABFMO"
        if "S" in phases:
            phase_S()
        if "A" in phases:
            phase_A(0, 0)
        if "B" in phases:
            phase_A(4, 1)
        R = {}
        R["g2"] = sb(top, "g2", [128, 8]); load(R["g2"], R["g2"][:], g2_d)
        R["acc"] = sb(top, "acc", [128, NOWN, 1024])
        R["xnT16"] = sb(top, "xnT16", [128, 8, NOWN * 128], BF16)
        R["Wgt"] = sb(top, "Wgt", [128, NOWN, 32])
        if "F" in phases:
            phase_F(R)
        if "M" in phases:
            phase_M(R)
        if "O" in phases:
            phase_out(R)
        return finish()


def host_inputs(x, ln1_g, w_in, lam_re, lam_im, log_dt, ssm_b_re, ssm_b_im, ssm_c_re, ssm_c_im,
                ssm_d, w_glu, g_sb, g_ssm, w_out, ln2_g, w_router, b_router, w_gate, b_gate,
                w_up, b_up, w_down, b_down, ln_f_g, cores=range(8)):
    f = np.float32
    c_ = np.ascontiguousarray
    x = np.asarray(x, f)
    shared = {
        "w_in": c_(np.asarray(w_in, f)[0]), "w_glu": c_(np.asarray(w_glu, f)[0]), "w_out": c_(np.asarray(w_out, f)[0]),
        "w_router": c_(np.asarray(w_router, f)[0]),
        "w_gate": c_(np.asarray(w_gate, f)[0]), "w_up": c_(np.asarray(w_up, f)[0]), "w_down": c_(np.asarray(w_down, f)[0]),
        "g1": c_(np.asarray(ln1_g, f)[0].reshape(8, 128).T),
        "gcat": c_(np.concatenate([np.asarray(g_sb, f)[0], np.asarray(g_ssm, f)[0]]).reshape(8, 128).T),
        "g2": c_(np.asarray(ln2_g, f)[0].reshape(8, 128).T),
        "gf": c_(np.asarray(ln_f_g, f).reshape(1, 1024)), "brow": c_(np.asarray(b_router, f)[0].reshape(1, 32)),
        "bgT": c_(np.asarray(b_gate, f)[0].reshape(32, 8, 128).transpose(2, 0, 1)),
        "buT": c_(np.asarray(b_up, f)[0].reshape(32, 8, 128).transpose(2, 0, 1)),
        "bd": c_(np.asarray(b_down, f)[0]),
        "lamre": c_(np.asarray(lam_re, f)[0].reshape(16, 128).T), "lamim": c_(np.asarray(lam_im, f)[0].reshape(16, 128).T),
        "logdt": c_(np.repeat(np.asarray(log_dt, f)[0].reshape(16, 2, 1), 64, axis=2).reshape(16, 128).T),
        "bre": c_(np.asarray(ssm_b_re, f)[0].reshape(16, 128, 16).transpose(1, 0, 2)),
        "bim": c_(np.asarray(ssm_b_im, f)[0].reshape(16, 128, 16).transpose(1, 0, 2)),
        "creT": c_(np.asarray(ssm_c_re, f)[0].reshape(16, 2, 16, 64).transpose(1, 3, 0, 2).reshape(128, 16, 16)),
        "cimT": c_(np.asarray(ssm_c_im, f)[0].reshape(16, 2, 16, 64).transpose(1, 3, 0, 2).reshape(128, 16, 16)),
        "dsk": c_(np.asarray(ssm_d, f)[0].reshape(4, 128).T),
        "ident": np.eye(128, dtype=f), "negtri": c_(-np.tril(np.ones((128, 128), f))), "negones": -np.ones((128, 128), f),
        "strict": c_(np.triu(np.ones((128, 128), f), 1)),
        "ramp": c_(np.broadcast_to(np.arange(1, 129, dtype=f)[None, :], (128, 128))),
        "ramp2": c_(np.broadcast_to(np.arange(127, -1, -1, dtype=f)[None, :], (128, 128))),
    }
    maps = []
    for c in cores:
        b, qt = c // 4, c % 4
        xp = np.concatenate([np.zeros((384, 1024), f), x[b]], axis=0)
        m = dict(shared)
        m["xv"] = c_(xp[qt * 128: qt * 128 + NTOK])
        maps.append(m)
    return maps


_NC_CACHE = {}


def kernel(**inputs):
    if "nc" not in _NC_CACHE:
        _NC_CACHE["nc"] = build_program()[0]
    nc = _NC_CACHE["nc"]
    maps = host_inputs(**inputs)
    res = run_bass_kernel_spmd(nc, maps, core_ids=list(range(8)))
    out = np.empty((2, 8192, 1024), np.float32)
    for c in range(8):
        b, qt = c // 4, c % 4
        o = np.asarray(res.results[c]["out"]).reshape(NOWN, 128, 1024)
        out[b].reshape(NBLK, 128, 1024)[qt::4] = o
    return out
```

```python
import numpy as np
import ml_dtypes
import concourse.bass as bass
import concourse.mybir as mybir
from concourse.bass_utils import run_bass_kernel_spmd

F32 = mybir.dt.float32
BF16 = mybir.dt.bfloat16
AF = mybir.ActivationFunctionType
ALU = mybir.AluOpType
AX = mybir.AxisListType


class Buf:
    __slots__ = ("name", "w", "r")

    def __init__(self, name=""):
        self.name = name
        self.w = None
        self.r = {}


class _Rec:
    def __init__(self):
        self.call = None

    def __getattr__(self, name):
        def f(*a, **kw):
            self.call = (name, a, kw)
            return self
        return f


class Sched:
    ENGS = ("pe", "act", "dve", "pool", "sp")

    def __init__(self, nc, sems, dma_sems):
        self.nc = nc
        self.sem = dict(zip(self.ENGS, sems))
        self.cnt = {e: 0 for e in self.ENGS}
        self.ops = {e: [] for e in self.ENGS}
        self.waited = {e: {} for e in self.ENGS}
        self.dma_sems = dma_sems
        self.dma_cnt = [0] * len(dma_sems)
        self.dma_next = 0
        self.n_inst = 0

    def _semh(self, key):
        return self.sem[key] if isinstance(key, str) else self.dma_sems[key]

    def _need(self, eng, key, val):
        if key == eng and eng == "pe":
            return
        if self.waited[eng].get(key, 0) >= val:
            return
        self.waited[eng][key] = val
        h = self._semh(key)
        self.ops[eng].append(lambda e, h=h, val=val: e.wait_ge(h, val))

    def _deps(self, eng, reads, writes):
        for b in reads:
            if b.w is not None:
                self._need(eng, *b.w)
        for b in writes:
            if b.w is not None:
                self._need(eng, *b.w)
            for k, v in b.r.items():
                self._need(eng, k, v)

    def op(self, eng, fn, reads=(), writes=()):
        self._deps(eng, reads, writes)
        self.cnt[eng] += 1
        c = self.cnt[eng]
        h = self.sem[eng]
        rec = _Rec()
        fn(rec)
        m, a, kw = rec.call
        self.ops[eng].append(lambda e, m=m, a=a, kw=kw, h=h: getattr(e, m)(*a, **kw).then_inc(h, 1))
        for b in writes:
            b.w = (eng, c)
            b.r = {}
        for b in reads:
            if b.w is None or b.w != (eng, c):
                b.r[eng] = c
        self.n_inst += 1

    def dma(self, fn, reads=(), writes=(), eng="sp"):
        i = self.dma_next
        self.dma_next = (self.dma_next + 1) % len(self.dma_sems)
        if self.dma_cnt[i] > 0:
            self._need(eng, i, self.dma_cnt[i])
        self._deps(eng, reads, writes)
        self.dma_cnt[i] += 16
        v = self.dma_cnt[i]
        h = self.dma_sems[i]
        rec = _Rec()
        fn(rec)
        m, a, kw = rec.call
        self.ops[eng].append(lambda e, m=m, a=a, kw=kw, h=h: getattr(e, m)(*a, **kw).then_inc(h, 16))
        for b in writes:
            b.w = (i, v)
            b.r = {}
        for b in reads:
            b.r[i] = v
        self.n_inst += 1
        return (i, v)

    def finish(self, eng="sp"):
        for i, v in enumerate(self.dma_cnt):
            if v > 0:
                self._need(eng, i, v)

    def emit(self, block):
        ops = self.ops

        @block.tensor
        def _(e):
            for f in ops["pe"]:
                f(e)

        @block.scalar
        def _(e):
            for f in ops["act"]:
                f(e)

        @block.vector
        def _(e):
            for f in ops["dve"]:
                f(e)

        @block.gpsimd
        def _(e):
            for f in ops["pool"]:
                f(e)

        @block.sync
        def _(e):
            for f in ops["sp"]:
                f(e)

    def barrier(self):
        for eng in self.ENGS:
            for k in self.ENGS:
                if k != eng and self.cnt[k] > 0:
                    self._need(eng, k, self.cnt[k])
            for i, v in enumerate(self.dma_cnt):
                if v > 0:
                    self._need(eng, i, v)


class TT:
    def __init__(self, t, name, nbuf=1):
        self.t = t
        self.b = Buf(name)
        self.bs = [Buf(f"{name}{i}") for i in range(nbuf)] if nbuf > 1 else None

    def __getitem__(self, k):
        return self.t[k]


class View:
    def __init__(self, ap, name):
        self.ap = ap
        self.b = Buf(name)

    def __getitem__(self, k):
        return self.ap[k]


NTOK = 8192
NBLK = 64
NOWN = 16
EPS = 1e-5
GELU_C = 0.7978845608028654
EXPERTS = 32


def build_program(dbg=(), stop_after=None, n_experts=EXPERTS, upto=None):
    from contextlib import ExitStack
    nc = bass.Bass("TRN2", target_bir_lowering=False)

    def din(name, shape, dt=F32):
        return nc.dram_tensor(name, list(shape), dt, kind="ExternalInput").ap()

    xv = din("xv", [NTOK, 1024])
    w_in = din("w_in", [1024, 2048]); w_glu = din("w_glu", [512, 1024]); w_out = din("w_out", [1024, 1024])
    w_router = din("w_router", [1024, 32])
    w_gate = din("w_gate", [32, 1024, 1024]); w_up = din("w_up", [32, 1024, 1024]); w_down = din("w_down", [32, 1024, 1024])
    g1_d = din("g1", [128, 8]); gcat_d = din("gcat", [128, 8]); g2_d = din("g2", [128, 8])
    gf_d = din("gf", [1, 1024]); brow_d = din("brow", [1, 32])
    bgT_d = din("bgT", [128, 32, 8]); buT_d = din("buT", [128, 32, 8]); bd_d = din("bd", [32, 1024])
    lamre_d = din("lamre", [128, 16]); lamim_d = din("lamim", [128, 16]); logdt_d = din("logdt", [128, 16])
    bre_d = din("bre", [128, 16, 16]); bim_d = din("bim", [128, 16, 16])
    cre_d = din("creT", [128, 16, 16]); cim_d = din("cimT", [128, 16, 16]); dsk_d = din("dsk", [128, 4])
    ident_d = din("ident", [128, 128]); negtri_d = din("negtri", [128, 128]); negones_d = din("negones", [128, 128])
    strict_d = din("strict", [128, 128]); ramp_d = din("ramp", [128, 128]); ramp2_d = din("ramp2", [128, 128])
    out_d = nc.dram_tensor("out", [NOWN * 128, 1024], F32, kind="ExternalOutput").ap()
    dbg_d = {}

    top = ExitStack()
    with top:
        sems = [top.enter_context(nc.semaphore(f"s_{e}")) for e in Sched.ENGS]
        dsems = [top.enter_context(nc.semaphore(f"d_{i}")) for i in range(24)]
        S = Sched(nc, sems, dsems)

        used = {}

        def uniq(n):
            used[n] = used.get(n, 0) + 1
            return n if used[n] == 1 else f"{n}_v{used[n]}"

        def sb(es, name, shape, dt=F32, nbuf=1):
            return TT(es.enter_context(nc.sbuf_tensor(uniq("sb_" + name), list(shape), dt)), name, nbuf)

        def ps(es, name, shape=(128, 512), dt=F32):
            return TT(es.enter_context(nc.psum_tensor(uniq("ps_" + name), list(shape), dt)), name)

        def dump(name, ap, buf, shape):
            if name not in dbg:
                return
            d = nc.dram_tensor("dbg_" + name, list(shape), F32, kind="ExternalOutput").ap()
            dbg_d[name] = d
            S.dma(lambda e: e.dma_start(out=d, in_=ap), reads=[buf])

        def load(dst, dst_ap, src_ap, eng="sp"):
            S.dma(lambda e: e.dma_start(out=dst_ap, in_=src_ap), writes=[dst.b], eng=eng)

        ident32 = sb(top, "ident32", [128, 128]); ident16 = sb(top, "ident16", [128, 128], BF16)
        negtri16 = sb(top, "negtri16", [128, 128], BF16); negones16 = sb(top, "negones16", [128, 128], BF16)
        strict16 = sb(top, "strict16", [128, 128], BF16)
        shared = sb(top, "shared", [128, 16384], BF16)
        mssm16 = View(shared[:, 0:8192].rearrange("p (a b) -> p a b", a=NOWN), "mssm16")
        ysb16 = View(shared[:, 8192:16384].rearrange("p (a b) -> p a b", a=NOWN), "ysb16")
        ss_sb = sb(top, "ss_sb", [128, NOWN, 2])
        cstg = sb(top, "cstg", [128, 128])
        load(ident32, ident32[:], ident_d)
        S.op("dve", lambda e: e.tensor_copy(out=ident16[:], in_=ident32[:]), reads=[ident32.b], writes=[ident16.b])
        for dst, src in ((negtri16, negtri_d), (negones16, negones_d), (strict16, strict_d)):
            load(cstg, cstg[:], src)
            S.op("dve", lambda e, dst=dst: e.tensor_copy(out=dst[:], in_=cstg[:]), reads=[cstg.b], writes=[dst.b])

        def make_prologue(es, evac_eng, stat_eng="act", extra_pT=()):
            P = {}
            P["xc"] = sb(es, "xc", [128, 2, 1024]); P["xs16"] = sb(es, "xs16", [128, 4, 1024], BF16)
            P["junk"] = sb(es, "junk", [128, 1024], BF16); P["ss4"] = sb(es, "ss4", [128, 4]); P["rstd4"] = sb(es, "rstd4", [128, 4])
            P["hT"] = [sb(es, "hT0", [128, 8, 512], BF16)] * 2
            pTt = ps(es, "pTt", [128, 1024], BF16)

            class _PV:
                def __init__(s_): s_.b = pTt.b
                def __getitem__(s_, k): return pTt.t[:, 0:512][k]
            class _PVx:
                def __init__(s_, tt): s_.tt = tt; s_.b = tt.b
                def __getitem__(s_, k): return s_.tt.t[:].bitcast(BF16)[:, 0:512][k]
            P["pT"] = [_PV()] + [_PVx(t_) for t_ in extra_pT]
            P["n"] = 0
            P["defer"] = None

            def pro_a(c):
                xc, xs16, junk, ss4, rstd4 = P["xc"], P["xs16"], P["junk"], P["ss4"], P["rstd4"]
                for hf in range(2):
                    load(xc, xc[:], xv[c * 512 + hf * 256:c * 512 + (hf + 1) * 256, :].rearrange("(b p) d -> p b d", p=128))
                    for i2 in range(2):
                        i = 2 * hf + i2
                        if stat_eng == "act":
                            S.op("act", lambda e: e.activation(out=junk[:], in_=xc[:, i2, :], func=AF.Square, accum_out=ss4[:, i:i + 1]),
                                 reads=[xc.b], writes=[junk.b, ss4.b])
                        else:
                            S.op("dve", lambda e: e.scalar_tensor_tensor(out=junk[:], in0=xc[:, i2, :], scalar=1.0, in1=xc[:, i2, :], op0=ALU.mult, op1=ALU.mult, accum_out=ss4[:, i:i + 1]),
                                 reads=[xc.b], writes=[junk.b, ss4.b])
                    S.op("act", lambda e: e.activation(out=rstd4[:, 2 * hf:2 * hf + 2], in_=ss4[:, 2 * hf:2 * hf + 2], func=AF.Ln, scale=1.0 / 1024, bias=EPS), reads=[ss4.b], writes=[rstd4.b])
                    S.op("act", lambda e: e.activation(out=rstd4[:, 2 * hf:2 * hf + 2], in_=rstd4[:, 2 * hf:2 * hf + 2], func=AF.Exp, scale=-0.5), reads=[rstd4.b], writes=[rstd4.b])
                    for i2 in range(2):
                        i = 2 * hf + i2
                        if stat_eng == "act":
                            S.op("act", lambda e: e.activation(out=xs16[:, i, :], in_=xc[:, i2, :], func=AF.Copy, scale=rstd4[:, i:i + 1]),
                                 reads=[xc.b, rstd4.b], writes=[xs16.b])
                        else:
                            S.op("dve", lambda e: e.tensor_scalar(out=xs16[:, i, :], in0=xc[:, i2, :], scalar1=rstd4[:, i:i + 1], scalar2=None, op0=ALU.mult),
                                 reads=[xc.b, rstd4.b], writes=[xs16.b])

            def pro_b_units(c):
                xs16 = P["xs16"]
                hT = P["hT"][c % 2]
                units = []
                for dt in range(8):
                    def unit(dt=dt):
                        pT = P["pT"][dt % len(P["pT"])]
                        for i in range(4):
                            S.op("pe", lambda e: e.transpose(pT[:, i * 128:(i + 1) * 128], xs16[:, i, dt * 128:(dt + 1) * 128], ident16[:]),
                                 reads=[xs16.b, ident16.b], writes=[pT.b])
                        eng = evac_eng if isinstance(evac_eng, str) else evac_eng[dt % len(evac_eng)]

                        def evac(pT=pT, dt=dt, eng=eng):
                            if eng == "act":
                                S.op("act", lambda e: e.activation(out=hT[:, dt, :], in_=pT[:], func=AF.Copy), reads=[pT.b], writes=[hT.b])
                            else:
                                S.op(eng, lambda e: e.tensor_copy(out=hT[:, dt, :], in_=pT[:]), reads=[pT.b], writes=[hT.b])
                        if P.get("defer") is not None:
                            P["defer"].append(evac)
                        else:
                            evac()
                    units.append(unit)
                return hT, units
            return pro_a, pro_b_units, P

        def load_win(es, name, colspecs, stgs):
            tot = sum(n for _, n, _ in colspecs)
            W = sb(es, name, [128, 8, tot], BF16)
            k = 0
            for dt in range(8):
                o = 0
                for (c0, n, sc) in colspecs:
                    stg = stgs[k % len(stgs)]
                    k += 1
                    load(stg, stg[:, 0:n], w_in[dt * 128:(dt + 1) * 128, c0:c0 + n])
                    gx = g1 if sc == 1.0 else g1q
                    S.op("act", lambda e: e.activation(out=W[:, dt, o:o + n], in_=stg[:, 0:n], func=AF.Copy, scale=gx[:, dt:dt + 1]), reads=[stg.b, gx.b], writes=[W.b])
                    o += n
            return W

        g1 = sb(top, "g1", [128, 8]); load(g1, g1[:], g1_d)
        g1q = sb(top, "g1q", [128, 8])
        S.op("dve", lambda e: e.tensor_scalar(out=g1q[:], in0=g1[:], scalar1=0.125, scalar2=None, op0=ALU.mult), reads=[g1.b], writes=[g1q.b])

        def phase_S():
            es = ExitStack()
            with es:
                stgs = [sb(es, f"stgS{i}", [128, 1024]) for i in range(2)]
                stg = stgs[0]
                Wu16 = load_win(es, "Wu16", [(1536, 512, 1.0)], stgs)
                Wglu16 = sb(es, "Wglu16", [128, 4, 1024], BF16)
                for ct in range(4):
                    stg = stgs[ct % 2]
                    load(stg, stg[:], w_glu[ct * 128:(ct + 1) * 128, :])
                    S.op("act", lambda e: e.activation(out=Wglu16[:, ct, :], in_=stg[:], func=AF.Copy), reads=[stg.b], writes=[Wglu16.b])
                dsk = sb(es, "dsk", [128, 4]); load(dsk, dsk[:], dsk_d)
                cosT = sb(es, "cosT", [128, 16, 128]); sinT = sb(es, "sinT", [128, 16, 128]); Rt = sb(es, "Rt", [128, 16, 128])
                Bre16 = sb(es, "Bre16", [128, 16, 128], BF16); Bim16 = sb(es, "Bim16", [128, 16, 128], BF16)
                Cre16 = sb(es, "Cre16", [128, 16, 128], BF16); Cim16 = sb(es, "Cim16", [128, 16, 128], BF16)
                rr2 = sb(es, "rr2", [128, 2, 16]); init = sb(es, "init", [128, 2, 16])
                Apr16 = sb(es, "Apr16", [128, 16, 128], BF16); Api16 = sb(es, "Api16", [128, 16, 128], BF16)
                Bpre = sb(es, "Bpre", [128, 16, 32]); Bpim = sb(es, "Bpim", [128, 16, 32]); a128 = sb(es, "a128", [128, 2, 16])
                pbr = [ps(es, f"pbr{i}") for i in range(2)]; pbi = [ps(es, f"pbi{i}") for i in range(2)]
                py = ps(es, "py")
                pin = [ps(es, f"pinS{i}") for i in range(2)]

                su = ExitStack()
                with su:
                    def small(name, shape=(128, 16)):
                        return sb(su, name, shape)
                    lamre = small("lamre"); lamim = small("lamim"); logdt = small("logdt")
                    bre = small("bre", [128, 16, 16]); bim = small("bim", [128, 16, 16])
                    creT = small("creTs", [128, 16, 16]); cimT = small("cimTs", [128, 16, 16])
                    ramp = small("ramp", [128, 128])
                    for t_, d_ in ((lamre, lamre_d), (lamim, lamim_d), (logdt, logdt_d), (bre, bre_d), (bim, bim_d), (creT, cre_d), (cimT, cim_d), (ramp, ramp_d)):
                        load(t_, t_[:], d_)
                    dtv = small("dtv"); ar = small("ar"); th = small("th"); rr = small("rr")
                    S.op("act", lambda e: e.activation(out=dtv[:], in_=logdt[:], func=AF.Exp), reads=[logdt.b], writes=[dtv.b])
                    S.op("dve", lambda e: e.tensor_tensor(out=ar[:], in0=lamre[:], in1=dtv[:], op=ALU.mult), reads=[lamre.b, dtv.b], writes=[ar.b])
                    S.op("dve", lambda e: e.tensor_tensor(out=th[:], in0=lamim[:], in1=dtv[:], op=ALU.mult), reads=[lamim.b, dtv.b], writes=[th.b])
                    S.op("act", lambda e: e.activation(out=rr[:], in_=ar[:], func=AF.Exp), reads=[ar.b], writes=[rr.b])

                    sc_tmp = {}

                    def sincos(name, ang, n, cos_out, sin_out, cb, sbuf_):
                        if n not in sc_tmp:
                            sc_tmp[n] = (sb(su, name + "_tq", [128, n]), sb(su, name + "_ti", [128, n], mybir.dt.int32), sb(su, name + "_tf", [128, n]))
                        tq, ti, tf = sc_tmp[n]
                        for off, outap, ob in ((0.0, sin_out, sbuf_), (0.25, cos_out, cb)):
                            S.op("dve", lambda e, off=off: e.tensor_scalar(out=tq[:], in0=ang[:], scalar1=1.0 / (2 * np.pi), scalar2=off, op0=ALU.mult, op1=ALU.add),
                                 reads=[ang.b], writes=[tq.b])
                            S.op("dve", lambda e: e.tensor_copy(out=ti[:], in_=tq[:]), reads=[tq.b], writes=[ti.b])
                            S.op("dve", lambda e: e.tensor_copy(out=tf[:], in_=ti[:]), reads=[ti.b], writes=[tf.b])
                            S.op("dve", lambda e: e.tensor_tensor(out=tq[:], in0=tq[:], in1=tf[:], op=ALU.subtract), reads=[tq.b, tf.b], writes=[tq.b])
                            S.op("act", lambda e, outap=outap: e.activation(out=outap, in_=tq[:], func=AF.Sin, scale=6.28318), reads=[tq.b], writes=[ob])

                    cth = small("cth"); sth = small("sth")
                    sincos("a", th, 16, cth[:], sth[:], cth.b, sth.b)
                    a_re = small("a_re"); a_im = small("a_im")
                    S.op("dve", lambda e: e.tensor_tensor(out=a_re[:], in0=rr[:], in1=cth[:], op=ALU.mult), reads=[rr.b, cth.b], writes=[a_re.b])
                    S.op("dve", lambda e: e.tensor_tensor(out=a_im[:], in0=rr[:], in1=sth[:], op=ALU.mult), reads=[rr.b, sth.b], writes=[a_im.b])
                    nre = small("nre"); den = small("den"); t0 = small("t0"); t1 = small("t1"); cf_re = small("cf_re"); cf_im = small("cf_im"); ncf_im = small("ncf_im")
                    S.op("dve", lambda e: e.tensor_scalar(out=nre[:], in0=a_re[:], scalar1=-1.0, scalar2=None, op0=ALU.add), reads=[a_re.b], writes=[nre.b])
                    S.op("dve", lambda e: e.tensor_tensor(out=t0[:], in0=lamre[:], in1=lamre[:], op=ALU.mult), reads=[lamre.b], writes=[t0.b])
                    S.op("dve", lambda e: e.tensor_tensor(out=t1[:], in0=lamim[:], in1=lamim[:], op=ALU.mult), reads=[lamim.b], writes=[t1.b])
                    S.op("dve", lambda e: e.tensor_tensor(out=den[:], in0=t0[:], in1=t1[:], op=ALU.add), reads=[t0.b, t1.b], writes=[den.b])
                    S.op("dve", lambda e: e.reciprocal(out=den[:], in_=den[:]), reads=[den.b], writes=[den.b])
                    S.op("dve", lambda e: e.tensor_tensor(out=t0[:], in0=nre[:], in1=lamre[:], op=ALU.mult), reads=[nre.b, lamre.b], writes=[t0.b])
                    S.op("dve", lambda e: e.tensor_tensor(out=t1[:], in0=a_im[:], in1=lamim[:], op=ALU.mult), reads=[a_im.b, lamim.b], writes=[t1.b])
                    S.op("dve", lambda e: e.tensor_tensor(out=t0[:], in0=t0[:], in1=t1[:], op=ALU.add), reads=[t0.b, t1.b], writes=[t0.b])
                    S.op("dve", lambda e: e.tensor_tensor(out=cf_re[:], in0=t0[:], in1=den[:], op=ALU.mult), reads=[t0.b, den.b], writes=[cf_re.b])
                    S.op("dve", lambda e: e.tensor_tensor(out=t0[:], in0=a_im[:], in1=lamre[:], op=ALU.mult), reads=[a_im.b, lamre.b], writes=[t0.b])
                    S.op("dve", lambda e: e.tensor_tensor(out=t1[:], in0=nre[:], in1=lamim[:], op=ALU.mult), reads=[nre.b, lamim.b], writes=[t1.b])
                    S.op("dve", lambda e: e.tensor_tensor(out=t0[:], in0=t0[:], in1=t1[:], op=ALU.subtract), reads=[t0.b, t1.b], writes=[t0.b])
                    S.op("dve", lambda e: e.tensor_tensor(out=cf_im[:], in0=t0[:], in1=den[:], op=ALU.mult), reads=[t0.b, den.b], writes=[cf_im.b])
                    S.op("dve", lambda e: e.tensor_scalar(out=ncf_im[:], in0=cf_im[:], scalar1=-1.0, scalar2=None, op0=ALU.mult), reads=[cf_im.b], writes=[ncf_im.b])
                    Mre = sb(su, "Mre", [128, 16, 128]); Mim = sb(su, "Mim", [128, 16, 128]); tb = sb(su, "tb", [128, 16])
                    S.op("pool", lambda e: e.memset(Mre[:], 0.0), writes=[Mre.b])
                    S.op("pool", lambda e: e.memset(Mim[:], 0.0), writes=[Mim.b])
                    S.op("pool", lambda e: e.memset(Cre16[:], 0.0), writes=[Cre16.b])
                    S.op("pool", lambda e: e.memset(Cim16[:], 0.0), writes=[Cim16.b])
                    for j in range(16):
                        jj = j % 4
                        for g2 in range(2):
                            p0, p1 = 64 * g2, 64 * g2 + 64
                            c0 = 32 * jj + 16 * g2
                            S.op("dve", lambda e, j=j, p0=p0, p1=p1: e.tensor_scalar(out=tb[p0:p1, :], in0=bre[p0:p1, j, :], scalar1=cf_re[p0:p1, j:j + 1], scalar2=None, op0=ALU.mult),
                                 reads=[bre.b, cf_re.b], writes=[tb.b])
                            S.op("dve", lambda e, j=j, p0=p0, p1=p1, c0=c0: e.scalar_tensor_tensor(out=Mre[p0:p1, j, c0:c0 + 16], in0=bim[p0:p1, j, :], scalar=ncf_im[p0:p1, j:j + 1], in1=tb[p0:p1, :], op0=ALU.mult, op1=ALU.add),
                                 reads=[bim.b, ncf_im.b, tb.b], writes=[Mre.b])
                            S.op("dve", lambda e, j=j, p0=p0, p1=p1: e.tensor_scalar(out=tb[p0:p1, :], in0=bim[p0:p1, j, :], scalar1=cf_re[p0:p1, j:j + 1], scalar2=None, op0=ALU.mult),
                                 reads=[bim.b, cf_re.b], writes=[tb.b])
                            S.op("dve", lambda e, j=j, p0=p0, p1=p1, c0=c0: e.scalar_tensor_tensor(out=Mim[p0:p1, j, c0:c0 + 16], in0=bre[p0:p1, j, :], scalar=cf_im[p0:p1, j:j + 1], in1=tb[p0:p1, :], op0=ALU.mult, op1=ALU.add),
                                 reads=[bre.b, cf_im.b, tb.b], writes=[Mim.b])
                            S.op("pool", lambda e, j=j, p0=p0, p1=p1, c0=c0: e.tensor_copy(out=Cre16[p0:p1, j, c0:c0 + 16], in_=creT[p0:p1, j, :]), reads=[creT.b], writes=[Cre16.b])
                            S.op("pool", lambda e, j=j, p0=p0, p1=p1, c0=c0: e.tensor_scalar(out=Cim16[p0:p1, j, c0:c0 + 16], in0=cimT[p0:p1, j, :], scalar1=-1.0, scalar2=None, op0=ALU.mult), reads=[cimT.b], writes=[Cim16.b])
                    for j in range(16):
                        for (M_, B_) in ((Mre, Bre16), (Mim, Bim16)):
                            S.op("pe", lambda e, j=j, M_=M_: e.transpose(py[:, 0:128], M_[:, j, :], ident32[:]), reads=[M_.b, ident32.b], writes=[py.b])
                            S.op("dve", lambda e, j=j, B_=B_: e.tensor_copy(out=B_[:, j, :], in_=py[:, 0:128]), reads=[py.b], writes=[B_.b])
                    ang = sb(su, "ang", [128, 16, 128])
                    for j in range(16):
                        S.op("dve", lambda e, j=j: e.tensor_scalar(out=ang[:, j, :], in0=ramp[:], scalar1=th[:, j:j + 1], scalar2=None, op0=ALU.mult), reads=[ramp.b, th.b], writes=[ang.b])
                        S.op("pool", lambda e, j=j: e.tensor_scalar(out=Rt[:, j, :], in0=ramp[:], scalar1=0.0, scalar2=rr[:, j:j + 1], op0=ALU.mult, op1=ALU.add), reads=[ramp.b, rr.b], writes=[Rt.b])
                    angf = TT(ang.t, "angf"); angf.b = ang.b
                    class _V:
                        def __init__(s_, t, b): s_.t = t; s_.b = b
                        def __getitem__(s_, k): return s_.t[:].rearrange("p a b -> p (a b)")
                    sincos("tab", _V(ang.t, ang.b), 2048, cosT[:].rearrange("p a b -> p (a b)"), sinT[:].rearrange("p a b -> p (a b)"), cosT.b, sinT.b)
                    S.op("pool", lambda e: e.memset(Rt[:, :, 0:1], 0.0), writes=[Rt.b])
                    S.op("dve", lambda e: e.tensor_copy(out=rr2[:, 0, :], in_=rr[:]), reads=[rr.b], writes=[rr2.b])
                    S.op("dve", lambda e: e.tensor_copy(out=rr2[:, 1, :], in_=rr[:]), reads=[rr.b], writes=[rr2.b])
                    S.op("dve", lambda e: e.memset(init[:], 0.0), writes=[init.b])
                    ramp2 = small("ramp2", [128, 128]); load(ramp2, ramp2[:], ramp2_d)
                    ang2 = ang; rpow = sb(su, "rpow", [128, 16, 128])
                    c2 = sb(su, "c2", [128, 16, 128]); s2 = sb(su, "s2", [128, 16, 128])
                    for j in range(16):
                        S.op("dve", lambda e: e.tensor_scalar(out=ang2[:, j, :], in0=ramp2[:], scalar1=th[:, j:j + 1], scalar2=None, op0=ALU.mult), reads=[ramp2.b, th.b], writes=[ang2.b])
                    sincos("tab2", _V(ang2.t, ang2.b), 2048, c2[:].rearrange("p a b -> p (a b)"), s2[:].rearrange("p a b -> p (a b)"), c2.b, s2.b)
                    for j in range(16):
                        S.op("act", lambda e: e.activation(out=rpow[:, j, :], in_=ramp2[:], func=AF.Exp, scale=ar[:, j:j + 1]), reads=[ramp2.b, ar.b], writes=[rpow.b])
                    S.op("dve", lambda e: e.tensor_tensor(out=c2[:], in0=c2[:], in1=rpow[:], op=ALU.mult), reads=[c2.b, rpow.b], writes=[c2.b])
                    S.op("dve", lambda e: e.tensor_tensor(out=s2[:], in0=s2[:], in1=rpow[:], op=ALU.mult), reads=[s2.b, rpow.b], writes=[s2.b])
                    for j in range(16):
                        for (src_, dst_) in ((c2, Apr16), (s2, Api16)):
                            S.op("pe", lambda e: e.transpose(py[:, 0:128], src_[:, j, :], ident32[:]), reads=[src_.b, ident32.b], writes=[py.b])
                            S.op("dve", lambda e: e.tensor_copy(out=dst_[:, j, :], in_=py[:, 0:128]), reads=[py.b], writes=[dst_.b])
                        jj = j % 4
                        S.op("dve", lambda e: e.tensor_copy(out=Bpre[:, j, :], in_=Mre[:, j, 32 * jj:32 * jj + 32]), reads=[Mre.b], writes=[Bpre.b])
                        S.op("dve", lambda e: e.tensor_copy(out=Bpim[:, j, :], in_=Mim[:, j, 32 * jj:32 * jj + 32]), reads=[Mim.b], writes=[Bpim.b])
                    r128 = small("r128")
                    S.op("act", lambda e: e.activation(out=r128[:], in_=ar[:], func=AF.Exp, scale=128.0), reads=[ar.b], writes=[r128.b])
                    S.op("dve", lambda e: e.tensor_tensor(out=a128[:, 0, :], in0=r128[:], in1=cosT[:, :, 127], op=ALU.mult), reads=[r128.b, cosT.b], writes=[a128.b])
                    S.op("dve", lambda e: e.tensor_tensor(out=a128[:, 1, :], in0=r128[:], in1=sinT[:, :, 127], op=ALU.mult), reads=[r128.b, sinT.b], writes=[a128.b])
                    dump("cosT", cosT[:].rearrange("p a b -> p (a b)"), cosT.b, [128, 2048])
                    dump("Rt", Rt[:].rearrange("p a b -> p (a b)"), Rt.b, [128, 2048])
                    S.barrier()
                pro_a, pro_b_units, _P = make_prologue(es, "act")
                pend = []
                uT16 = [sb(es, f"uT16_{i}", [128, 4, 128], BF16) for i in range(2)]
                utok16 = [sb(es, f"utok16_{i}", [128, 3, 512], BF16) for i in range(2)]
                qri = sb(es, "qri", [128, 2, 16]); m8 = [sb(es, f"m8_{i}", [128, 16]) for i in range(4)]
                u32 = [sb(es, f"u32_{i}", [128, 4, 128]) for i in range(2)]
                tmpA = [sb(es, f"tmpA{i}", [128, 512]) for i in range(4)]
                fS = shared[:, 8192:16384].bitcast(F32)
                tmpB = [View(fS[:, i * 512:(i + 1) * 512], f"tmpB{i}") for i in range(4)]
                bts = [sb(es, "bt0", [128, 2, 16, 128])] * 2; Wt = sb(es, "Wt", [128, 2, 16, 128])
                xri16 = sb(es, "xri16", [128, 2, 16, 128], BF16)
                m6 = [sb(es, f"m6_{i}", [128, 16]) for i in range(4)]
                yv = View(fS[:, 2048:2560], "yv"); g1t = View(fS[:, 2560:3072], "g1t"); g2t = View(fS[:, 3072:3584], "g2t"); gl16 = sb(es, "gl16", [128, 4, 128], BF16)
                ysm = View(fS[:, 3584:4096], "ysm"); sss = sb(es, "sss", [128, 1]); rss = sb(es, "rss", [128, 1])
                junkS = sb(es, "junkS", [128, 512], BF16)
                nq = [0]

                def inproj_units(c, hT):
                    return [lambda i=i: inproj_tok(c, hT, i) for i in range(3)] + [lambda ct=ct: inproj_u1(c, hT, ct) for ct in range(4)]

                def inproj_tok(c, hT, i):
                    pn = pin[i % 2]
                    for dt in range(8):
                        S.op("pe", lambda e: e.matmul(pn[:], lhsT=hT[:, dt, i * 128:(i + 1) * 128], rhs=Wu16[:, dt, :], start=(dt == 0), stop=(dt == 7)), reads=[Wu16.b, hT.b], writes=[pn.b])
                    S.op("act", lambda e: e.activation(out=utok16[c % 2][:, i, :], in_=pn[:], func=AF.Copy), reads=[pn.b], writes=[utok16[c % 2].b])

                def inproj_u1(c, hT, ct):
                    pn = pin[ct % 2]
                    for dt in range(8):
                        S.op("pe", lambda e: e.matmul(pn[:, 0:128], lhsT=Wu16[:, dt, ct * 128:(ct + 1) * 128], rhs=hT[:, dt, 384:512], start=(dt == 0), stop=(dt == 7)), reads=[Wu16.b, hT.b], writes=[pn.b])
                    S.op("act", lambda e: e.activation(out=uT16[c % 2][:, ct, :], in_=pn[:, 0:128], func=AF.Copy), reads=[pn.b], writes=[uT16[c % 2].b])
                    S.op("act", lambda e: e.activation(out=u32[c % 2][:, ct, :], in_=pn[:, 0:128], func=AF.Copy), reads=[pn.b], writes=[u32[c % 2].b])

                def skip_block(c, i):
                    if i == 0:
                        assert not pend
                        if c + 1 < NOWN:
                            pro_a(c + 1)
                            hTn, un = pro_b_units(c + 1)
                            pend.extend(un + inproj_units(c + 1, hTn))
                    sl = nq[0] % 2
                    nq[0] += 1
                    pr, pi_ = pbr[sl], pbi[sl]
                    ut = utok16[c % 2]
                    for j in range(16):
                        S.op("pe", lambda e: e.matmul(pr[:, 32 * j:32 * j + 32], lhsT=Apr16[:, j, :], rhs=ut[:, i, 32 * j:32 * j + 32], start=(j == 0), stop=(j == 15), skip_group_check=True), reads=[Apr16.b, ut.b], writes=[pr.b])
                        S.op("pe", lambda e: e.matmul(pi_[:, 32 * j:32 * j + 32], lhsT=Api16[:, j, :], rhs=ut[:, i, 32 * j:32 * j + 32], start=(j == 0), stop=(j == 15), skip_group_check=True), reads=[Api16.b, ut.b], writes=[pi_.b])
                    for _ in range(3):
                        if pend:
                            pend.pop(0)()
                    ta = tmpA if sl == 0 else tmpB
                    Bre_f = Bpre[:].rearrange("p a b -> p (a b)"); Bim_f = Bpim[:].rearrange("p a b -> p (a b)")
                    S.op("dve", lambda e: e.tensor_tensor(out=ta[0][:], in0=pr[:], in1=Bre_f, op=ALU.mult), reads=[pr.b, Bpre.b], writes=[ta[0].b])
                    S.op("dve", lambda e: e.tensor_tensor(out=ta[1][:], in0=pi_[:], in1=Bim_f, op=ALU.mult), reads=[pi_.b, Bpim.b], writes=[ta[1].b])
                    S.op("dve", lambda e: e.tensor_tensor(out=ta[2][:], in0=pi_[:], in1=Bre_f, op=ALU.mult), reads=[pi_.b, Bpre.b], writes=[ta[2].b])
                    S.op("dve", lambda e: e.tensor_tensor(out=ta[3][:], in0=pr[:], in1=Bim_f, op=ALU.mult), reads=[pr.b, Bpim.b], writes=[ta[3].b])
                    S.op("dve", lambda e: e.tensor_tensor(out=ta[0][:], in0=ta[0][:], in1=ta[1][:], op=ALU.subtract), reads=[ta[0].b, ta[1].b], writes=[ta[0].b])
                    S.op("dve", lambda e: e.tensor_tensor(out=ta[2][:], in0=ta[2][:], in1=ta[3][:], op=ALU.add), reads=[ta[2].b, ta[3].b], writes=[ta[2].b])
                    S.op("dve", lambda e: e.tensor_reduce(out=qri[:, 0, :], in_=ta[0][:].rearrange("p (a b) -> p a b", a=16), axis=AX.X, op=ALU.add), reads=[ta[0].b], writes=[qri.b])
                    S.op("dve", lambda e: e.tensor_reduce(out=qri[:, 1, :], in_=ta[2][:].rearrange("p (a b) -> p a b", a=16), axis=AX.X, op=ALU.add), reads=[ta[2].b], writes=[qri.b])
                    S.op("dve", lambda e: e.tensor_tensor(out=m8[0][:], in0=init[:, 0, :], in1=a128[:, 0, :], op=ALU.mult), reads=[init.b, a128.b], writes=[m8[0].b])
                    S.op("dve", lambda e: e.tensor_tensor(out=m8[1][:], in0=init[:, 1, :], in1=a128[:, 1, :], op=ALU.mult), reads=[init.b, a128.b], writes=[m8[1].b])
                    S.op("dve", lambda e: e.tensor_tensor(out=m8[2][:], in0=init[:, 1, :], in1=a128[:, 0, :], op=ALU.mult), reads=[init.b, a128.b], writes=[m8[2].b])
                    S.op("dve", lambda e: e.tensor_tensor(out=m8[3][:], in0=init[:, 0, :], in1=a128[:, 1, :], op=ALU.mult), reads=[init.b, a128.b], writes=[m8[3].b])
                    S.op("dve", lambda e: e.tensor_tensor(out=m8[0][:], in0=m8[0][:], in1=m8[1][:], op=ALU.subtract), reads=[m8[0].b, m8[1].b], writes=[m8[0].b])
                    S.op("dve", lambda e: e.tensor_tensor(out=m8[2][:], in0=m8[2][:], in1=m8[3][:], op=ALU.add), reads=[m8[2].b, m8[3].b], writes=[m8[2].b])
                    S.op("dve", lambda e: e.tensor_tensor(out=init[:, 0, :], in0=m8[0][:], in1=qri[:, 0, :], op=ALU.add), reads=[m8[0].b, qri.b], writes=[init.b])
                    S.op("dve", lambda e: e.tensor_tensor(out=init[:, 1, :], in0=m8[2][:], in1=qri[:, 1, :], op=ALU.add), reads=[m8[2].b, qri.b], writes=[init.b])

                def demod(g):
                    c, i = g // 4, g % 4
                    bt = bts[g % 2]
                    uT = uT16[c % 2]
                    for qd in range(4):
                        sl = nq[0] % 2
                        nq[0] += 1
                        pr, pi_ = pbr[sl], pbi[sl]
                        for jj in range(4):
                            j = 4 * qd + jj
                            S.op("pe", lambda e, j=j, jj=jj, pr=pr: e.matmul(pr[:, jj * 128:(jj + 1) * 128], lhsT=Bre16[:, j, :], rhs=uT[:, qd, :], start=(jj == 0), stop=(jj == 3), skip_group_check=True),
                                 reads=[Bre16.b, uT.b], writes=[pr.b])
                            S.op("pe", lambda e, j=j, jj=jj, pi_=pi_: e.matmul(pi_[:, jj * 128:(jj + 1) * 128], lhsT=Bim16[:, j, :], rhs=uT[:, qd, :], start=(jj == 0), stop=(jj == 3), skip_group_check=True),
                                 reads=[Bim16.b, uT.b], writes=[pi_.b])
                        for _ in range(2):
                            if pend:
                                pend.pop(0)()
                        cs = cosT[:, 4 * qd:4 * qd + 4, :].rearrange("p a b -> p (a b)")
                        sn = sinT[:, 4 * qd:4 * qd + 4, :].rearrange("p a b -> p (a b)")
                        ta = tmpA if sl == 0 else tmpB
                        S.op("dve", lambda e, pr=pr, cs=cs, ta=ta: e.tensor_tensor(out=ta[0][:], in0=pr[:], in1=cs, op=ALU.mult), reads=[pr.b, cosT.b], writes=[ta[0].b])
                        S.op("dve", lambda e, pi_=pi_, sn=sn, ta=ta: e.tensor_tensor(out=ta[1][:], in0=pi_[:], in1=sn, op=ALU.mult), reads=[pi_.b, sinT.b], writes=[ta[1].b])
                        S.op("dve", lambda e, pi_=pi_, cs=cs, ta=ta: e.tensor_tensor(out=ta[2][:], in0=pi_[:], in1=cs, op=ALU.mult), reads=[pi_.b, cosT.b], writes=[ta[2].b])
                        S.op("dve", lambda e, pr=pr, sn=sn, ta=ta: e.tensor_tensor(out=ta[3][:], in0=pr[:], in1=sn, op=ALU.mult), reads=[pr.b, sinT.b], writes=[ta[3].b])
                        S.op("pool", lambda e, qd=qd, ta=ta: e.tensor_tensor(out=bt[:, 0, 4 * qd:4 * qd + 4, :].rearrange("p a b -> p (a b)"), in0=ta[0][:], in1=ta[1][:], op=ALU.add),
                             reads=[ta[0].b, ta[1].b], writes=[bt.b])
                        S.op("pool", lambda e, qd=qd, ta=ta: e.tensor_tensor(out=bt[:, 1, 4 * qd:4 * qd + 4, :].rearrange("p a b -> p (a b)"), in0=ta[2][:], in1=ta[3][:], op=ALU.subtract),
                             reads=[ta[2].b, ta[3].b], writes=[bt.b])

                def tail(g):
                    bt = bts[g % 2]
                    S.op("dve", lambda e: e.tensor_tensor(out=m6[0][:, 0:16], in0=init[:, 0, :], in1=rr2[:, 0, :], op=ALU.mult), reads=[init.b, rr2.b], writes=[m6[0].b])
                    S.op("dve", lambda e: e.tensor_tensor(out=m6[1][:, 0:16], in0=init[:, 1, :], in1=rr2[:, 1, :], op=ALU.mult), reads=[init.b, rr2.b], writes=[m6[1].b])
                    S.op("dve", lambda e: e.tensor_tensor(out=bt[:, 0, :, 0], in0=bt[:, 0, :, 0], in1=m6[0][:], op=ALU.add), reads=[bt.b, m6[0].b], writes=[bt.b])
                    S.op("dve", lambda e: e.tensor_tensor(out=bt[:, 1, :, 0], in0=bt[:, 1, :, 0], in1=m6[1][:], op=ALU.add), reads=[bt.b, m6[1].b], writes=[bt.b])
                    Rf = Rt[:].rearrange("p a b -> p (a b)")
                    for ri in range(2):
                        S.op("dve", lambda e, ri=ri: e.tensor_tensor_scan(out=Wt[:, ri, :, :].rearrange("p a b -> p (a b)"), data0=Rf, data1=bt[:, ri, :, :].rearrange("p a b -> p (a b)"), initial=0.0, op0=ALU.mult, op1=ALU.add),
                             reads=[Rt.b, bt.b], writes=[Wt.b])
                    wr, wi = Wt[:, 0, :, 127], Wt[:, 1, :, 127]
                    c128, s128 = cosT[:, :, 127], sinT[:, :, 127]
                    S.op("dve", lambda e: e.tensor_tensor(out=m6[0][:], in0=wr, in1=c128, op=ALU.mult), reads=[Wt.b, cosT.b], writes=[m6[0].b])
                    S.op("dve", lambda e: e.tensor_tensor(out=m6[1][:], in0=wi, in1=s128, op=ALU.mult), reads=[Wt.b, sinT.b], writes=[m6[1].b])
                    S.op("dve", lambda e: e.tensor_tensor(out=m6[2][:], in0=wi, in1=c128, op=ALU.mult), reads=[Wt.b, cosT.b], writes=[m6[2].b])
                    S.op("dve", lambda e: e.tensor_tensor(out=m6[3][:], in0=wr, in1=s128, op=ALU.mult), reads=[Wt.b, sinT.b], writes=[m6[3].b])
                    S.op("dve", lambda e: e.tensor_tensor(out=init[:, 0, :], in0=m6[0][:], in1=m6[1][:], op=ALU.subtract), reads=[m6[0].b, m6[1].b], writes=[init.b])
                    S.op("dve", lambda e: e.tensor_tensor(out=init[:, 1, :], in0=m6[2][:], in1=m6[3][:], op=ALU.add), reads=[m6[2].b, m6[3].b], writes=[init.b])

                def own_block(c):
                    ob = c
                    for qd in range(4):
                        cs = cosT[:, 4 * qd:4 * qd + 4, :].rearrange("p a b -> p (a b)")
                        sn = sinT[:, 4 * qd:4 * qd + 4, :].rearrange("p a b -> p (a b)")
                        wrq = Wt[:, 0, 4 * qd:4 * qd + 4, :].rearrange("p a b -> p (a b)")
                        wiq = Wt[:, 1, 4 * qd:4 * qd + 4, :].rearrange("p a b -> p (a b)")
                        ta = tmpA if qd % 2 == 0 else tmpB
                        S.op("dve", lambda e, ta=ta, wrq=wrq, cs=cs: e.tensor_tensor(out=ta[0][:], in0=wrq, in1=cs, op=ALU.mult), reads=[Wt.b, cosT.b], writes=[ta[0].b])
                        S.op("dve", lambda e, ta=ta, wiq=wiq, sn=sn: e.tensor_tensor(out=ta[1][:], in0=wiq, in1=sn, op=ALU.mult), reads=[Wt.b, sinT.b], writes=[ta[1].b])
                        S.op("dve", lambda e, ta=ta, wiq=wiq, cs=cs: e.tensor_tensor(out=ta[2][:], in0=wiq, in1=cs, op=ALU.mult), reads=[Wt.b, cosT.b], writes=[ta[2].b])
                        S.op("dve", lambda e, ta=ta, wrq=wrq, sn=sn: e.tensor_tensor(out=ta[3][:], in0=wrq, in1=sn, op=ALU.mult), reads=[Wt.b, sinT.b], writes=[ta[3].b])
                        S.op("dve", lambda e, ta=ta, qd=qd: e.tensor_tensor(out=xri16[:, 0, 4 * qd:4 * qd + 4, :].rearrange("p a b -> p (a b)"), in0=ta[0][:], in1=ta[1][:], op=ALU.subtract),
                             reads=[ta[0].b, ta[1].b], writes=[xri16.b])
                        S.op("dve", lambda e, ta=ta, qd=qd: e.tensor_tensor(out=xri16[:, 1, 4 * qd:4 * qd + 4, :].rearrange("p a b -> p (a b)"), in0=ta[2][:], in1=ta[3][:], op=ALU.add),
                             reads=[ta[2].b, ta[3].b], writes=[xri16.b])
                    for ct in range(4):
                        n = 0
                        for jj in range(4):
                            j = 4 * ct + jj
                            for (C_, ri) in ((Cre16, 0), (Cim16, 1)):
                                S.op("pe", lambda e, ct=ct, j=j, C_=C_, ri=ri, n=n: e.matmul(py[:, ct * 128:(ct + 1) * 128], lhsT=C_[:, j, :], rhs=xri16[:, ri, j, :], start=(n == 0 and ct == 0), stop=(n == 7 and ct == 3), skip_group_check=True),
                                     reads=[C_.b, xri16.b], writes=[py.b])
                                n += 1
                    for ct in range(4):
                        S.op("dve", lambda e, ct=ct: e.scalar_tensor_tensor(out=yv[:, ct * 128:(ct + 1) * 128], in0=u32[c % 2][:, ct, :], scalar=dsk[:, ct:ct + 1], in1=py[:, ct * 128:(ct + 1) * 128], op0=ALU.mult, op1=ALU.add),
                             reads=[u32[c % 2].b, dsk.b, py.b], writes=[yv.b])
                    dump(f"ypre{ob}", yv[:], yv.b, [128, 512])

                def own_B(c):
                    ob = c
                    S.op("dve", lambda e: e.tensor_tensor(out=g1t[:], in0=yv[:], in1=yv[:], op=ALU.mult), reads=[yv.b], writes=[g1t.b])
                    S.op("dve", lambda e: e.tensor_scalar(out=g1t[:], in0=g1t[:], scalar1=0.044715, scalar2=1.0, op0=ALU.mult, op1=ALU.add), reads=[g1t.b], writes=[g1t.b])
                    S.op("dve", lambda e: e.tensor_tensor(out=g1t[:], in0=g1t[:], in1=yv[:], op=ALU.mult), reads=[g1t.b, yv.b], writes=[g1t.b])
                    S.op("act", lambda e: e.activation(out=g2t[:], in_=g1t[:], func=AF.Sigmoid, scale=2.0 * GELU_C), reads=[g1t.b], writes=[g2t.b])
                    S.op("dve", lambda e: e.tensor_tensor(out=gl16[:].rearrange("p a b -> p (a b)"), in0=yv[:], in1=g2t[:], op=ALU.mult), reads=[yv.b, g2t.b], writes=[gl16.b])
                    for half in range(2):
                        for ct in range(4):
                            S.op("pe", lambda e, half=half, ct=ct: e.matmul(pin[half][:], lhsT=gl16[:, ct, :], rhs=Wglu16[:, ct, half * 512:(half + 1) * 512], start=(ct == 0), stop=(ct == 3)),
                                 reads=[gl16.b, Wglu16.b], writes=[pin[half].b])

                def own_B2(c):
                    ob = c
                    S.op("act", lambda e: e.activation(out=g1t[:], in_=pin[1][:], func=AF.Sigmoid), reads=[pin[1].b], writes=[g1t.b])
                    S.op("dve", lambda e: e.tensor_tensor(out=ysm[:], in0=pin[0][:], in1=g1t[:], op=ALU.mult), reads=[pin[0].b, g1t.b], writes=[ysm.b])
                    dump(f"yssm{ob}", ysm[:], ysm.b, [128, 512])
                    S.op("act", lambda e: e.activation(out=junkS[:], in_=ysm[:], func=AF.Square, accum_out=sss[:, 0:1]), reads=[ysm.b], writes=[junkS.b, sss.b])
                    S.op("act", lambda e: e.activation(out=rss[:], in_=sss[:], func=AF.Ln, scale=1.0 / 512, bias=EPS), reads=[sss.b], writes=[rss.b])
                    S.op("act", lambda e: e.activation(out=rss[:], in_=rss[:], func=AF.Exp, scale=-0.5), reads=[rss.b], writes=[rss.b])
                    S.op("act", lambda e: e.activation(out=mssm16[:, ob, :], in_=ysm[:], func=AF.Copy, scale=rss[:, 0:1]), reads=[ysm.b, rss.b], writes=[mssm16.b])

                if upto == "setup":
                    return
                pro_a(0)
                hT0, un = pro_b_units(0)
                for u_ in un + inproj_units(0, hT0):
                    u_()
                nch_ = NOWN if upto is None else 2
                done_ = set()

                def do_skip(c, i):
                    if (c, i) not in done_:
                        done_.add((c, i))
                        skip_block(c, i)
                for c in range(nch_):
                    for i in range(3):
                        do_skip(c, i)
                    demod(4 * c + 3)
                    tail(4 * c + 3)
                    while pend:
                        pend.pop(0)()
                    own_block(c)
                    if c + 1 < nch_:
                        do_skip(c + 1, 0)
                    own_B(c)
                    if c + 1 < nch_:
                        do_skip(c + 1, 1)
                    own_B2(c)
                S.barrier()

        def phase_A(hs, pidx):
            es = ExitStack()
            with es:
                stgs = [sb(es, f"stgA{pidx}_{i}", [128, 256]) for i in range(3)]
                W16 = load_win(es, f"Wqkv16_{pidx}", [(64 * hs, 256, 0.125), (512 + 64 * hs, 256, 1.0), (1024 + 64 * hs, 256, 1.0)], stgs)
                kT = sb(es, f"kT{pidx}", [128, 2, NTOK], BF16)
                vS = sb(es, f"vS{pidx}", [128, NBLK, 256], BF16)
                kb_ = [Buf(f"kT_c{c}") for c in range(NOWN)]
                vb_ = [Buf(f"vS_c{c}") for c in range(NOWN)]
                qT = [sb(es, f"qT{pidx}_{i}", [128, 2, 128], BF16) for i in range(2)]
                pin = [ps(es, f"pinA{pidx}_{i}") for i in range(2)]
                pA = [ps(es, f"pA{pidx}_{i}") for i in range(4)]
                pO = ps(es, f"pO{pidx}")
                E32 = [sb(es, f"E32_{pidx}_{i}", [128, 512]) for i in range(4)]
                Lp = [sb(es, f"Lp{pidx}_{i}", [128, 512], BF16) for i in range(4)]
                AT = [sb(es, f"AT{pidx}_{i}", [128, 512], BF16) for i in range(4)]
                Gs = [sb(es, f"Gs{pidx}_{i}", [128, 128], BF16) for i in range(4)]
                Ls = [sb(es, f"Ls{pidx}_{i}", [128, 512], BF16) for i in range(4)]
                gt1 = sb(es, f"gt1_{pidx}", [128, 256], BF16); gt2 = sb(es, f"gt2_{pidx}", [128, 128], BF16)
                ysraw = sb(es, f"ysraw{pidx}", [128, 256]); junkA = sb(es, f"junkA{pidx}", [128, 256])
                pro_a, pro_b_units, PP = make_prologue(es, "dve", "dve", extra_pT=pin)
                deferred = []
                PP["defer"] = deferred
                pend = []

                def inproj_units(c, hT):
                    return [lambda ft=ft, h=h: inproj_k(c, hT, ft, h) for ft in range(2) for h in range(2)] + [lambda i=i, h=h: inproj_v(c, hT, i, h) for i in range(4) for h in range(2)] + [lambda: inproj_q(c, hT, 0), lambda: inproj_q(c, hT, 1)]

                def inproj_k(c, hT, ft, h):
                    if True:
                        pn = pin[ft]
                        for dt in range(4 * h, 4 * h + 4):
                            S.op("pe", lambda e: e.matmul(pn[:], lhsT=W16[:, dt, 256 + ft * 128:256 + (ft + 1) * 128], rhs=hT[:, dt, :], start=(dt == 0), stop=(dt == 7)),
                                 reads=[W16.b, hT.b], writes=[pn.b])
                        if h == 1:
                            deferred.append(lambda: S.op("dve", lambda e: e.tensor_copy(out=kT[:, ft, c * 512:(c + 1) * 512], in_=pn[:]), reads=[pn.b], writes=[kb_[c]]))

                def inproj_v(c, hT, i, h):
                    if True:
                        pn = pin[i % 2]
                        for dt in range(4 * h, 4 * h + 4):
                            S.op("pe", lambda e: e.matmul(pn[:, 0:256], lhsT=hT[:, dt, i * 128:(i + 1) * 128], rhs=W16[:, dt, 512:768], start=(dt == 0), stop=(dt == 7)),
                                 reads=[W16.b, hT.b], writes=[pn.b])
                        if h == 1:
                            deferred.append(lambda: S.op("dve", lambda e: e.tensor_copy(out=vS[:, 4 * c + i, :], in_=pn[:, 0:256]), reads=[pn.b], writes=[vb_[c]]))

                def inproj_q(c, hT, ft):
                    q = qT[c % 2]
                    if True:
                        pn = pin[ft]
                        for dt in range(8):
                            S.op("pe", lambda e: e.matmul(pn[:, 0:128], lhsT=W16[:, dt, ft * 128:(ft + 1) * 128], rhs=hT[:, dt, 384:512], start=(dt == 0), stop=(dt == 7)),
                                 reads=[W16.b, hT.b], writes=[pn.b])
                        deferred.append(lambda: S.op("dve", lambda e: e.tensor_copy(out=q[:, ft, :], in_=pn[:, 0:128]), reads=[pn.b], writes=[q.b]))

                def attention(c):
                    q = qT[c % 2]
                    tasks = [(hl, m) for hl in range(4) for m in range(c, -1, -1)]
                    st = {"n": 0}

                    def stage1(n):
                        hl, m = tasks[n]
                        sl = n % 4
                        p0 = 64 * (hl % 2)
                        ft = hl // 2
                        first = (m == c)
                        for i in range(4):
                            kb = 4 * m + i
                            S.op("pe", lambda e: e.matmul(pA[sl][:, i * 128:(i + 1) * 128], lhsT=kT[p0:p0 + 64, ft, kb * 128:(kb + 1) * 128], rhs=q[p0:p0 + 64, ft, :], start=(i == 0), stop=False, skip_group_check=True),
                                 reads=[kb_[m], q.b], writes=[pA[sl].b])
                        S.op("act", lambda e: e.activation(out=E32[sl][:], in_=pA[sl][:], func=AF.Exp), reads=[pA[sl].b], writes=[E32[sl].b])

                    def stage1b(n):
                        hl, m = tasks[n]
                        sl = n % 4
                        first = (m == c)
                        S.op("act", lambda e: e.activation(out=Lp[sl][:], in_=E32[sl][:], func=AF.Ln, bias=1.0, scale=1.0), reads=[E32[sl].b], writes=[Lp[sl].b])
                        if first:
                            S.op("dve", lambda e: e.tensor_tensor(out=Lp[sl][:, 384:512], in0=Lp[sl][:, 384:512], in1=strict16[:], op=ALU.mult), reads=[Lp[sl].b, strict16.b], writes=[Lp[sl].b])
                        L_ = Ls[sl]
                        Ln_ = Ls[(n + 1) % 4]
                        if first:
                            S.op("dve", lambda e: e.tensor_copy(out=L_[:, 256:384], in_=Lp[sl][:, 384:512]), reads=[Lp[sl].b], writes=[L_.b])
                        else:
                            S.op("dve", lambda e: e.tensor_tensor(out=L_[:, 256:384], in0=Lp[sl][:, 384:512], in1=L_[:, 384:512], op=ALU.add), reads=[Lp[sl].b, L_.b], writes=[L_.b])
                        S.op("dve", lambda e: e.tensor_tensor(out=L_[:, 128:256], in0=L_[:, 256:384], in1=Lp[sl][:, 256:384], op=ALU.add), reads=[Lp[sl].b, L_.b], writes=[L_.b])
                        S.op("dve", lambda e: e.tensor_tensor(out=L_[:, 0:128], in0=L_[:, 128:256], in1=Lp[sl][:, 128:256], op=ALU.add), reads=[Lp[sl].b, L_.b], writes=[L_.b])
                        if m > 0:
                            S.op("dve", lambda e: e.tensor_tensor(out=Ln_[:, 384:512], in0=L_[:, 0:128], in1=Lp[sl][:, 0:128], op=ALU.add), reads=[Lp[sl].b, L_.b], writes=[Ln_.b])

                    def stage2(n):
                        hl, m = tasks[n]
                        sl = n % 4
                        first = (m == c)
                        nmm = 2
                        cnt = [0]

                        def mm(out_ap, lhsT_ap, rhs_ap, rbufs):
                            cnt[0] += 1
                            S.op("pe", lambda e: e.matmul(out_ap, lhsT=lhsT_ap, rhs=rhs_ap, start=False, stop=(cnt[0] == nmm), skip_group_check=True), reads=rbufs, writes=[pA[sl].b])
                        mm(pA[sl][:], negtri16[:], Lp[sl][:], [negtri16.b, Lp[sl].b])
                        if first:
                            mm(pA[sl][:, 0:384], negones16[:], Ls[sl][:, 0:384], [negones16.b, Ls[sl].b])
                        else:
                            mm(pA[sl][:], negones16[:], Ls[sl][:], [negones16.b, Ls[sl].b])
                        S.op("act", lambda e: e.activation(out=AT[sl][:], in_=pA[sl][:], func=AF.Exp), reads=[pA[sl].b], writes=[AT[sl].b])
                        if first:
                            S.op("dve", lambda e: e.tensor_tensor(out=AT[sl][:, 384:512], in0=AT[sl][:, 384:512], in1=strict16[:], op=ALU.mult), reads=[AT[sl].b, strict16.b], writes=[AT[sl].b])

                    def stage3(n):
                        hl, m = tasks[n]
                        sl = n % 4
                        first = (m == c)
                        for i in range(4):
                            kb = 4 * m + i
                            S.op("pe", lambda e: e.matmul(pO[:, hl * 64:(hl + 1) * 64], lhsT=AT[sl][:, i * 128:(i + 1) * 128], rhs=vS[:, kb, hl * 64:(hl + 1) * 64], start=(first and i == 0 and hl == 0), stop=(m == 0 and i == 3 and hl == 3), skip_group_check=True),
                                 reads=[AT[sl].b, vb_[m]], writes=[pO.b])

                    NT = len(tasks)
                    for it in range(NT + 3):
                        while deferred:
                            deferred.pop(0)()
                        if it < NT:
                            stage1(it)
                        for _ in range(2 if len(pend) > (NT + 3 - it) else 1):
                            if pend:
                                pend.pop(0)()
                        if 0 <= it - 2 < NT:
                            stage2(it - 2)
                        if it < NT:
                            stage1b(it)
                        if 0 <= it - 3 < NT:
                            stage3(it - 3)
                    S.op("dve", lambda e: e.tensor_copy(out=ysraw[:], in_=pO[:, 0:256]), reads=[pO.b], writes=[ysraw.b])
                    dump(f"ysb{pidx}_{c}", ysraw[:], ysraw.b, [128, 256])
                    S.op("dve", lambda e: e.scalar_tensor_tensor(out=junkA[:], in0=ysraw[:], scalar=1.0, in1=ysraw[:], op0=ALU.mult, op1=ALU.mult, accum_out=ss_sb[:, c, pidx:pidx + 1]),
                         reads=[ysraw.b], writes=[junkA.b, ss_sb.b])
                    S.op("pool", lambda e: e.tensor_copy(out=ysb16[:, c, 64 * hs:64 * hs + 256], in_=ysraw[:]), reads=[ysraw.b], writes=[ysb16.b])

                nch = NOWN if upto is None else int(upto)
                pro_a(0)
                hT0, un = pro_b_units(0)
                for u_ in un + inproj_units(0, hT0):
                    u_()
                    while deferred:
                        deferred.pop(0)()
                for c in range(nch):
                    if c + 1 < nch:
                        pro_a(c + 1)
                        hTn, un = pro_b_units(c + 1)
                        pend.extend(un + inproj_units(c + 1, hTn))
                    attention(c)
                    while pend:
                        pend.pop(0)()
                        while deferred:
                            deferred.pop(0)()
                    while deferred:
                        deferred.pop(0)()
                S.barrier()

        def phase_F(R):
            es = ExitStack()
            with es:
                acc, xnT16, Wgt = R["acc"], R["xnT16"], R["Wgt"]
                stgF = [sb(es, f"stgF{i}", [128, 1024]) for i in range(2)]
                gcat = sb(es, "gcat", [128, 8]); load(gcat, gcat[:], gcat_d)
                g2 = R["g2"]
                Wout16 = sb(es, "Wout16", [128, 8, 1024], BF16)
                for k in range(8):
                    stg = stgF[k % 2]
                    load(stg, stg[:], w_out[k * 128:(k + 1) * 128, :])
                    S.op("act", lambda e: e.activation(out=Wout16[:, k, :], in_=stg[:], func=AF.Copy, scale=gcat[:, k:k + 1]), reads=[stg.b, gcat.b], writes=[Wout16.b])
                Wr16 = sb(es, "Wr16", [128, 8, 32], BF16)
                for k in range(8):
                    stg = stgF[k % 2]
                    load(stg, stg[:, 0:32], w_router[k * 128:(k + 1) * 128, :])
                    S.op("act", lambda e: e.activation(out=Wr16[:, k, :], in_=stg[:, 0:32], func=AF.Copy, scale=g2[:, k:k + 1]), reads=[stg.b, g2.b], writes=[Wr16.b])
                browb = sb(es, "browb", [128, 32]); load(browb, browb[:], brow_d.partition_broadcast(128))
                bd32 = sb(es, "bd32", [32, 1024]); load(bd32, bd32[:], bd_d)
                def two(name, shape, dt=F32):
                    return [sb(es, f"{name}{i}", shape, dt) for i in range(2)]
                xb_ = two("xbF", [128, 1024]); mix16_ = two("mix16", [128, 512], BF16); mixT_ = two("mixT", [128, 8, 128], BF16)
                sst_ = two("sstF", [128, 1]); rst_ = two("rstF", [128, 1]); junk = sb(es, "junkF", [128, 1024], BF16)
                xn16_ = two("xn16", [128, 1024], BF16)
                lg_ = two("lg", [128, 32]); mx8_ = two("mx8", [128, 8]); msk_ = two("msk", [128, 32]); nmx_ = two("nmx", [128, 1])
                ex_ = two("exF", [128, 32]); den_ = two("denF", [128, 1]); WgtT_ = two("WgtT", [32, 128])
                pT_ = [ps(es, f"pTF{i}", [128, 1024], BF16) for i in range(2)]
                pd = [ps(es, f"pdF{i}") for i in range(2)]
                pl = ps(es, "plF"); pw = ps(es, "pwF")
                pbs = [ps(es, f"pbF{i}") for i in range(2)]
                for ob in range(NOWN):
                    z_ = ob % 2
                    xb, mix16, mixT, sst, rst, xn16 = xb_[z_], mix16_[z_], mixT_[z_], sst_[z_], rst_[z_], xn16_[z_]
                    lg, mx8, msk, nmx, ex, den, WgtT, pT = lg_[z_], mx8_[z_], msk_[z_], nmx_[z_], ex_[z_], den_[z_], WgtT_[z_], pT_[z_]
                    load(xb, xb[:], xv[(4 * ob + 3) * 128:(4 * ob + 4) * 128, :])
                    S.op("dve", lambda e: e.tensor_tensor(out=sst[:], in0=ss_sb[:, ob, 0:1], in1=ss_sb[:, ob, 1:2], op=ALU.add), reads=[ss_sb.b], writes=[sst.b])
                    S.op("act", lambda e: e.activation(out=rst[:], in_=sst[:], func=AF.Ln, scale=1.0 / 512, bias=EPS), reads=[sst.b], writes=[rst.b])
                    S.op("act", lambda e: e.activation(out=rst[:], in_=rst[:], func=AF.Exp, scale=-0.5), reads=[rst.b], writes=[rst.b])
                    S.op("act", lambda e: e.activation(out=mix16[:], in_=ysb16[:, ob, :], func=AF.Copy, scale=rst[:, 0:1]), reads=[ysb16.b, rst.b], writes=[mix16.b])
                    for k in range(8):
                        src = mix16[:, k * 128:(k + 1) * 128] if k < 4 else mssm16[:, ob, (k - 4) * 128:(k - 3) * 128]
                        S.op("pe", lambda e: e.transpose(pT[:, k * 128:(k + 1) * 128], src, ident16[:]), reads=[mix16.b, mssm16.b, ident16.b], writes=[pT.b])
                    S.op("dve", lambda e: e.tensor_copy(out=mixT[:].rearrange("p a b -> p (a b)"), in_=pT[:]), reads=[pT.b], writes=[mixT.b])
                    for half in range(2):
                        for k in range(8):
                            S.op("pe", lambda e: e.matmul(pd[half][:], lhsT=mixT[:, k, :], rhs=Wout16[:, k, half * 512:(half + 1) * 512], start=(k == 0), stop=(k == 7)),
                                 reads=[mixT.b, Wout16.b], writes=[pd[half].b])
                        S.op("dve", lambda e: e.tensor_tensor(out=acc[:, ob, half * 512:(half + 1) * 512], in0=pd[half][:], in1=xb[:, half * 512:(half + 1) * 512], op=ALU.add),
                             reads=[pd[half].b, xb.b], writes=[acc.b])
                    dump(f"x1_{ob}", acc[:, ob, :], acc.b, [128, 1024])
                    S.op("act", lambda e: e.activation(out=junk[:], in_=acc[:, ob, :], func=AF.Square, accum_out=sst[:, 0:1]), reads=[acc.b], writes=[junk.b, sst.b])
                    S.op("act", lambda e: e.activation(out=rst[:], in_=sst[:], func=AF.Ln, scale=1.0 / 1024, bias=EPS), reads=[sst.b], writes=[rst.b])
                    S.op("act", lambda e: e.activation(out=rst[:], in_=rst[:], func=AF.Exp, scale=-0.5), reads=[rst.b], writes=[rst.b])
                    S.op("act", lambda e: e.activation(out=xn16[:], in_=acc[:, ob, :], func=AF.Copy, scale=rst[:, 0:1]), reads=[acc.b, rst.b], writes=[xn16.b])
                    for k in range(8):
                        S.op("pe", lambda e: e.transpose(pT[:, k * 128:(k + 1) * 128], xn16[:, k * 128:(k + 1) * 128], ident16[:]), reads=[xn16.b, ident16.b], writes=[pT.b])
                    S.op("dve", lambda e: e.tensor_copy(out=xnT16[:, :, ob * 128:(ob + 1) * 128], in_=pT[:].rearrange("p (a b) -> p a b", a=8)), reads=[pT.b], writes=[xnT16.b])
                    for k in range(8):
                        S.op("pe", lambda e: e.matmul(pl[:, 0:32], lhsT=xnT16[:, k, ob * 128:(ob + 1) * 128], rhs=Wr16[:, k, :], start=(k == 0), stop=(k == 7)), reads=[xnT16.b, Wr16.b], writes=[pl.b])
                    S.op("dve", lambda e: e.tensor_tensor(out=lg[:], in0=pl[:, 0:32], in1=browb[:], op=ALU.add), reads=[pl.b, browb.b], writes=[lg.b])
                    dump(f"lg_{ob}", lg[:], lg.b, [128, 32])
                    S.op("dve", lambda e: e.max(out=mx8[:], in_=lg[:]), reads=[lg.b], writes=[mx8.b])
                    S.op("dve", lambda e: e.tensor_scalar(out=msk[:], in0=lg[:], scalar1=mx8[:, 3:4], scalar2=None, op0=ALU.is_ge), reads=[lg.b, mx8.b], writes=[msk.b])
                    S.op("dve", lambda e: e.tensor_scalar(out=nmx[:], in0=mx8[:, 0:1], scalar1=-1.0, scalar2=None, op0=ALU.mult), reads=[mx8.b], writes=[nmx.b])
                    S.op("act", lambda e: e.activation(out=ex[:], in_=lg[:], func=AF.Exp, bias=nmx[:, 0:1], scale=1.0), reads=[lg.b, nmx.b], writes=[ex.b])
                    S.op("dve", lambda e: e.scalar_tensor_tensor(out=ex[:], in0=ex[:], scalar=1.0, in1=msk[:], op0=ALU.mult, op1=ALU.mult, accum_out=den[:, 0:1]), reads=[ex.b, msk.b], writes=[ex.b, den.b])
                    S.op("dve", lambda e: e.reciprocal(out=den[:], in_=den[:]), reads=[den.b], writes=[den.b])
                    S.op("dve", lambda e: e.tensor_scalar(out=Wgt[:, ob, :], in0=ex[:], scalar1=den[:, 0:1], scalar2=None, op0=ALU.mult), reads=[ex.b, den.b], writes=[Wgt.b])
                    S.op("pe", lambda e: e.transpose(pw[0:32, 0:128], Wgt[:, ob, :], ident32[:]), reads=[Wgt.b, ident32.b], writes=[pw.b])
                    S.op("dve", lambda e: e.tensor_copy(out=WgtT[:], in_=pw[0:32, 0:128]), reads=[pw.b], writes=[WgtT.b])
                    for half in range(2):
                        S.op("pe", lambda e: e.matmul(pbs[half][:], lhsT=WgtT[:], rhs=bd32[:, half * 512:(half + 1) * 512], start=True, stop=True), reads=[WgtT.b, bd32.b], writes=[pbs[half].b])
                        S.op("dve", lambda e: e.tensor_tensor(out=acc[:, ob, half * 512:(half + 1) * 512], in0=pbs[half][:], in1=acc[:, ob, half * 512:(half + 1) * 512], op=ALU.add),
                             reads=[pbs[half].b, acc.b], writes=[acc.b])
                S.barrier()

        def phase_M(R):
            es = ExitStack()
            with es:
                acc, xnT16, Wgt, g2 = R["acc"], R["xnT16"], R["Wgt"], R["g2"]
                bgT = sb(es, "bgT", [128, 32, 8]); load(bgT, bgT[:], bgT_d)
                buT = sb(es, "buT", [128, 32, 8]); load(buT, buT[:], buT_d)
                Wg16 = sb(es, "Wg16", [128, 8, 1024], BF16); Wu16 = sb(es, "Wu16m", [128, 8, 1024], BF16); Wd16 = sb(es, "Wd16", [128, 8, 1024], BF16)
                stg = [sb(es, f"stgM{i}", [128, 1024]) for i in range(2)]
                hT = View(shared[:, 0:16384].rearrange("p (a b) -> p a b", a=8), "hTm")
                hb = [Buf(f"hTm_t{i}") for i in range(4)]
                g32 = [sb(es, f"g32_{i}", [128, 512]) for i in range(2)]; s32 = [sb(es, f"s32_{i}", [128, 512]) for i in range(2)]
                u32 = [sb(es, f"u32m_{i}", [128, 512]) for i in range(2)]
                pg = [ps(es, f"pg{i}") for i in range(3)]; pu = [ps(es, f"pu{i}") for i in range(3)]; pdn = [ps(es, f"pdn{i}") for i in range(2)]
                nst = [0]
                K17 = 1.0 / 1.702
                g2s = sb(es, "g2s", [128, 8])
                S.op("dve", lambda e: e.tensor_scalar(out=g2s[:], in0=g2[:], scalar1=K17, scalar2=None, op0=ALU.mult), reads=[g2.b], writes=[g2s.b])
                S.op("dve", lambda e: e.tensor_scalar(out=buT[:], in0=buT[:], scalar1=K17, scalar2=None, op0=ALU.mult), reads=[buT.b], writes=[buT.b])

                def w_pieces(dst, src_e, scaled):
                    def piece(dt):
                        st = stg[nst[0] % 2]
                        nst[0] += 1
                        load(st, st[:], src_e[dt * 128:(dt + 1) * 128, :])
                        if scaled is not None:
                            S.op("act", lambda e: e.activation(out=dst[:, dt, :], in_=st[:], func=AF.Copy, scale=scaled[:, dt:dt + 1]), reads=[st.b, scaled.b], writes=[dst.b])
                        else:
                            S.op("act", lambda e: e.activation(out=dst[:, dt, :], in_=st[:], func=AF.Copy), reads=[st.b], writes=[dst.b])
                    return [lambda dt=dt: piece(dt) for dt in range(8)]

                ngu = [0]
                ndn = [0]
                pendw = []

                def gate_up(e_):
                    for tt in range(4):
                        t0 = tt * 512
                        for ft in range(8):
                            sl = ngu[0] % 3
                            s2 = ngu[0] % 2
                            ngu[0] += 1
                            for dt in range(8):
                                S.op("pe", lambda e: e.matmul(pg[sl][:], lhsT=Wg16[:, dt, ft * 128:(ft + 1) * 128], rhs=xnT16[:, dt, t0:t0 + 512], start=(dt == 0), stop=(dt == 7)), reads=[Wg16.b, xnT16.b], writes=[pg[sl].b])
                            for dt in range(8):
                                S.op("pe", lambda e: e.matmul(pu[sl][:], lhsT=Wu16[:, dt, ft * 128:(ft + 1) * 128], rhs=xnT16[:, dt, t0:t0 + 512], start=(dt == 0), stop=(dt == 7)), reads=[Wu16.b, xnT16.b], writes=[pu[sl].b])
                            S.op("dve", lambda e: e.tensor_scalar(out=g32[s2][:], in0=pg[sl][:], scalar1=bgT[:, e_, ft:ft + 1], scalar2=7.0, op0=ALU.add, op1=ALU.min), reads=[pg[sl].b, bgT.b], writes=[g32[s2].b])
                            S.op("act", lambda e: e.activation(out=s32[s2][:], in_=g32[s2][:], func=AF.Silu, scale=1.702), reads=[g32[s2].b], writes=[s32[s2].b])
                            S.op("dve", lambda e: e.tensor_scalar(out=u32[s2][:], in0=pu[sl][:], scalar1=buT[:, e_, ft:ft + 1], scalar2=7.0 * K17, op0=ALU.add, op1=ALU.min), reads=[pu[sl].b, buT.b], writes=[u32[s2].b])
                            S.op("dve", lambda e: e.tensor_scalar(out=u32[s2][:], in0=u32[s2][:], scalar1=-7.0 * K17, scalar2=K17, op0=ALU.max, op1=ALU.add), reads=[u32[s2].b], writes=[u32[s2].b])
                            S.op("dve", lambda e: e.tensor_tensor(out=hT[:, ft, t0:t0 + 512], in0=s32[s2][:], in1=u32[s2][:], op=ALU.mult), reads=[s32[s2].b, u32[s2].b], writes=[hb[tt]])
                            if pendw and (ngu[0] % 2 == 0):
                                pendw.pop(0)()

                def down(e_):
                    for ob in range(NOWN):
                        for hf in range(2):
                            sl = ndn[0] % 2
                            ndn[0] += 1
                            for ft in range(8):
                                S.op("pe", lambda e: e.matmul(pdn[sl][:], lhsT=hT[:, ft, ob * 128:(ob + 1) * 128], rhs=Wd16[:, ft, hf * 512:(hf + 1) * 512], start=(ft == 0), stop=(ft == 7)), reads=[hb[ob // 4], Wd16.b], writes=[pdn[sl].b])
                            S.op("dve", lambda e: e.scalar_tensor_tensor(out=acc[:, ob, hf * 512:(hf + 1) * 512], in0=pdn[sl][:], scalar=Wgt[:, ob, e_:e_ + 1], in1=acc[:, ob, hf * 512:(hf + 1) * 512], op0=ALU.mult, op1=ALU.add),
                                 reads=[pdn[sl].b, Wgt.b, acc.b], writes=[acc.b])

                for p_ in w_pieces(Wg16, w_gate[0], g2) + w_pieces(Wu16, w_up[0], g2s):
                    p_()
                for e_ in range(n_experts):
                    pendw.extend(w_pieces(Wd16, w_down[e_], None))
                    gate_up(e_)
                    while pendw:
                        pendw.pop(0)()
                    if e_ + 1 < n_experts:
                        for p_ in w_pieces(Wg16, w_gate[e_ + 1], g2) + w_pieces(Wu16, w_up[e_ + 1], g2s):
                            p_()
                    down(e_)
                S.barrier()

        def phase_out(R):
            es = ExitStack()
            with es:
                acc = R["acc"]
                gfb = sb(es, "gfb", [128, 1024]); load(gfb, gfb[:], gf_d.partition_broadcast(128))
                junk = sb(es, "junkO", [128, 1024], BF16); sst = sb(es, "sstO", [128, 1]); rst = sb(es, "rstO", [128, 1])
                ob_t = [sb(es, f"obuf{i}", [128, 1024]) for i in range(2)]
                for ob in range(NOWN):
                    o = ob_t[ob % 2]
                    dump(f"x2_{ob}", acc[:, ob, :], acc.b, [128, 1024])
                    S.op("act", lambda e: e.activation(out=junk[:], in_=acc[:, ob, :], func=AF.Square, accum_out=sst[:, 0:1]), reads=[acc.b], writes=[junk.b, sst.b])
                    S.op("act", lambda e: e.activation(out=rst[:], in_=sst[:], func=AF.Ln, scale=1.0 / 1024, bias=EPS), reads=[sst.b], writes=[rst.b])
                    S.op("act", lambda e: e.activation(out=rst[:], in_=rst[:], func=AF.Exp, scale=-0.5), reads=[rst.b], writes=[rst.b])
                    S.op("dve", lambda e: e.scalar_tensor_tensor(out=o[:], in0=acc[:, ob, :], scalar=rst[:, 0:1], in1=gfb[:], op0=ALU.mult, op1=ALU.mult), reads=[acc.b, rst.b, gfb.b], writes=[o.b])
                    S.dma(lambda e: e.dma_start(out=out_d[ob * 128:(ob + 1) * 128, :], in_=o[:]), reads=[o.b])

        def finish():
            S.finish()
            with nc.Block() as block:
                S.emit(block)
            return nc, dbg_d

        phases = stop_after or "SABFMO"
        if "S" in phases:
            phase_S()
        if "A" in phases:
            phase_A(0, 0)
        if "B" in phases:
            phase_A(4, 1)
        R = {}
        R["g2"] = sb(top, "g2", [128, 8]); load(R["g2"], R["g2"][:], g2_d)
        R["acc"] = sb(top, "acc", [128, NOWN, 1024])
        R["xnT16"] = sb(top, "xnT16", [128, 8, NOWN * 128], BF16)
        R["Wgt"] = sb(top, "Wgt", [128, NOWN, 32])
        if "F" in phases:
            phase_F(R)
        if "M" in phases:
            phase_M(R)
        if "O" in phases:
            phase_out(R)
        return finish()


def host_inputs(x, ln1_g, w_in, lam_re, lam_im, log_dt, ssm_b_re, ssm_b_im, ssm_c_re, ssm_c_im,
                ssm_d, w_glu, g_sb, g_ssm, w_out, ln2_g, w_router, b_router, w_gate, b_gate,
                w_up, b_up, w_down, b_down, ln_f_g, cores=range(8)):
    f = np.float32
    c_ = np.ascontiguousarray
    x = np.asarray(x, f)
    shared = {
        "w_in": c_(np.asarray(w_in, f)[0]), "w_glu": c_(np.asarray(w_glu, f)[0]), "w_out": c_(np.asarray(w_out, f)[0]),
        "w_router": c_(np.asarray(w_router, f)[0]),
        "w_gate": c_(np.asarray(w_gate, f)[0]), "w_up": c_(np.asarray(w_up, f)[0]), "w_down": c_(np.asarray(w_down, f)[0]),
        "g1": c_(np.asarray(ln1_g, f)[0].reshape(8, 128).T),
        "gcat": c_(np.concatenate([np.asarray(g_sb, f)[0], np.asarray(g_ssm, f)[0]]).reshape(8, 128).T),
        "g2": c_(np.asarray(ln2_g, f)[0].reshape(8, 128).T),
        "gf": c_(np.asarray(ln_f_g, f).reshape(1, 1024)), "brow": c_(np.asarray(b_router, f)[0].reshape(1, 32)),
        "bgT": c_(np.asarray(b_gate, f)[0].reshape(32, 8, 128).transpose(2, 0, 1)),
        "buT": c_(np.asarray(b_up, f)[0].reshape(32, 8, 128).transpose(2, 0, 1)),
        "bd": c_(np.asarray(b_down, f)[0]),
        "lamre": c_(np.asarray(lam_re, f)[0].reshape(16, 128).T), "lamim": c_(np.asarray(lam_im, f)[0].reshape(16, 128).T),
        "logdt": c_(np.repeat(np.asarray(log_dt, f)[0].reshape(16, 2, 1), 64, axis=2).reshape(16, 128).T),
        "bre": c_(np.asarray(ssm_b_re, f)[0].reshape(16, 128, 16).transpose(1, 0, 2)),
        "bim": c_(np.asarray(ssm_b_im, f)[0].reshape(16, 128, 16).transpose(1, 0, 2)),
        "creT": c_(np.asarray(ssm_c_re, f)[0].reshape(16, 2, 16, 64).transpose(1, 3, 0, 2).reshape(128, 16, 16)),
        "cimT": c_(np.asarray(ssm_c_im, f)[0].reshape(16, 2, 16, 64).transpose(1, 3, 0, 2).reshape(128, 16, 16)),
        "dsk": c_(np.asarray(ssm_d, f)[0].reshape(4, 128).T),
        "ident": np.eye(128, dtype=f), "negtri": c_(-np.tril(np.ones((128, 128), f))), "negones": -np.ones((128, 128), f),
        "strict": c_(np.triu(np.ones((128, 128), f), 1)),
        "ramp": c_(np.broadcast_to(np.arange(1, 129, dtype=f)[None, :], (128, 128))),
        "ramp2": c_(np.broadcast_to(np.arange(127, -1, -1, dtype=f)[None, :], (128, 128))),
    }
    maps = []
    for c in cores:
        b, qt = c // 4, c % 4
        xp = np.concatenate([np.zeros((384, 1024), f), x[b]], axis=0)
        m = dict(shared)
        m["xv"] = c_(xp[qt * 128: qt * 128 + NTOK])
        maps.append(m)
    return maps


_NC_CACHE = {}


def kernel(**inputs):
    if "nc" not in _NC_CACHE:
        _NC_CACHE["nc"] = build_program()[0]
    nc = _NC_CACHE["nc"]
    maps = host_inputs(**inputs)
    res = run_bass_kernel_spmd(nc, maps, core_ids=list(range(8)))
    out = np.empty((2, 8192, 1024), np.float32)
    for c in range(8):
        b, qt = c // 4, c % 4
        o = np.asarray(res.results[c]["out"]).reshape(NOWN, 128, 1024)
        out[b].reshape(NBLK, 128, 1024)[qt::4] = o
    return out
```

```python
import numpy as np
import ml_dtypes
import concourse.bass as bass
import concourse.mybir as mybir
from concourse.bass_utils import run_bass_kernel_spmd

F32 = mybir.dt.float32
BF16 = mybir.dt.bfloat16
AF = mybir.ActivationFunctionType
ALU = mybir.AluOpType
AX = mybir.AxisListType


class Buf:
    __slots__ = ("name", "w", "r")

    def __init__(self, name=""):
        self.name = name
        self.w = None
        self.r = {}


class _Rec:
    def __init__(self):
        self.call = None

    def __getattr__(self, name):
        def f(*a, **kw):
            self.call = (name, a, kw)
            return self
        return f


class Sched:
    ENGS = ("pe", "act", "dve", "pool", "sp")

    def __init__(self, nc, sems, dma_sems):
        self.nc = nc
        self.sem = dict(zip(self.ENGS, sems))
        self.cnt = {e: 0 for e in self.ENGS}
        self.ops = {e: [] for e in self.ENGS}
        self.waited = {e: {} for e in self.ENGS}
        self.dma_sems = dma_sems
        self.dma_cnt = [0] * len(dma_sems)
        self.dma_next = 0
        self.n_inst = 0

    def _semh(self, key):
        return self.sem[key] if isinstance(key, str) else self.dma_sems[key]

    def _need(self, eng, key, val):
        if key == eng and eng == "pe":
            return
        if self.waited[eng].get(key, 0) >= val:
            return
        self.waited[eng][key] = val
        h = self._semh(key)
        self.ops[eng].append(lambda e, h=h, val=val: e.wait_ge(h, val))

    def _deps(self, eng, reads, writes):
        for b in reads:
            if b.w is not None:
                self._need(eng, *b.w)
        for b in writes:
            if b.w is not None:
                self._need(eng, *b.w)
            for k, v in b.r.items():
                self._need(eng, k, v)

    def op(self, eng, fn, reads=(), writes=()):
        self._deps(eng, reads, writes)
        self.cnt[eng] += 1
        c = self.cnt[eng]
        h = self.sem[eng]
        rec = _Rec()
        fn(rec)
        m, a, kw = rec.call
        self.ops[eng].append(lambda e, m=m, a=a, kw=kw, h=h: getattr(e, m)(*a, **kw).then_inc(h, 1))
        for b in writes:
            b.w = (eng, c)
            b.r = {}
        for b in reads:
            if b.w is None or b.w != (eng, c):
                b.r[eng] = c
        self.n_inst += 1

    def dma(self, fn, reads=(), writes=(), eng="sp"):
        i = self.dma_next
        self.dma_next = (self.dma_next + 1) % len(self.dma_sems)
        if self.dma_cnt[i] > 0:
            self._need(eng, i, self.dma_cnt[i])
        self._deps(eng, reads, writes)
        self.dma_cnt[i] += 16
        v = self.dma_cnt[i]
        h = self.dma_sems[i]
        rec = _Rec()
        fn(rec)
        m, a, kw = rec.call
        self.ops[eng].append(lambda e, m=m, a=a, kw=kw, h=h: getattr(e, m)(*a, **kw).then_inc(h, 16))
        for b in writes:
            b.w = (i, v)
            b.r = {}
        for b in reads:
            b.r[i] = v
        self.n_inst += 1
        return (i, v)

    def finish(self, eng="sp"):
        for i, v in enumerate(self.dma_cnt):
            if v > 0:
                self._need(eng, i, v)

    def emit(self, block):
        ops = self.ops

        @block.tensor
        def _(e):
            for f in ops["pe"]:
                f(e)

        @block.scalar
        def _(e):
            for f in ops["act"]:
                f(e)

        @block.vector
        def _(e):
            for f in ops["dve"]:
                f(e)

        @block.gpsimd
        def _(e):
            for f in ops["pool"]:
                f(e)

        @block.sync
        def _(e):
            for f in ops["sp"]:
                f(e)

    def barrier(self):
        for eng in self.ENGS:
            for k in self.ENGS:
                if k != eng and self.cnt[k] > 0:
                    self._need(eng, k, self.cnt[k])
            for i, v in enumerate(self.dma_cnt):
                if v > 0:
                    self._need(eng, i, v)


class TT:
    def __init__(self, t, name, nbuf=1):
        self.t = t
        self.b = Buf(name)
        self.bs = [Buf(f"{name}{i}") for i in range(nbuf)] if nbuf > 1 else None

    def __getitem__(self, k):
        return self.t[k]


class View:
    def __init__(self, ap, name):
        self.ap = ap
        self.b = Buf(name)

    def __getitem__(self, k):
        return self.ap[k]


NTOK = 8192
NBLK = 64
NOWN = 16
EPS = 1e-5
GELU_C = 0.7978845608028654
EXPERTS = 32


def build_program(dbg=(), stop_after=None, n_experts=EXPERTS, upto=None):
    from contextlib import ExitStack
    nc = bass.Bass("TRN2", target_bir_lowering=False)

    def din(name, shape, dt=F32):
        return nc.dram_tensor(name, list(shape), dt, kind="ExternalInput").ap()

    xv = din("xv", [NTOK, 1024])
    w_in = din("w_in", [1024, 2048]); w_glu = din("w_glu", [512, 1024]); w_out = din("w_out", [1024, 1024])
    w_router = din("w_router", [1024, 32])
    w_gate = din("w_gate", [32, 1024, 1024]); w_up = din("w_up", [32, 1024, 1024]); w_down = din("w_down", [32, 1024, 1024])
    g1_d = din("g1", [128, 8]); gcat_d = din("gcat", [128, 8]); g2_d = din("g2", [128, 8])
    gf_d = din("gf", [1, 1024]); brow_d = din("brow", [1, 32])
    bgT_d = din("bgT", [128, 32, 8]); buT_d = din("buT", [128, 32, 8]); bd_d = din("bd", [32, 1024])
    lamre_d = din("lamre", [128, 16]); lamim_d = din("lamim", [128, 16]); logdt_d = din("logdt", [128, 16])
    bre_d = din("bre", [128, 16, 16]); bim_d = din("bim", [128, 16, 16])
    cre_d = din("creT", [128, 16, 16]); cim_d = din("cimT", [128, 16, 16]); dsk_d = din("dsk", [128, 4])
    ident_d = din("ident", [128, 128]); negtri_d = din("negtri", [128, 128]); negones_d = din("negones", [128, 128])
    strict_d = din("strict", [128, 128]); ramp_d = din("ramp", [128, 128]); ramp2_d = din("ramp2", [128, 128])
    out_d = nc.dram_tensor("out", [NOWN * 128, 1024], F32, kind="ExternalOutput").ap()
    dbg_d = {}

    top = ExitStack()
    with top:
        sems = [top.enter_context(nc.semaphore(f"s_{e}")) for e in Sched.ENGS]
        dsems = [top.enter_context(nc.semaphore(f"d_{i}")) for i in range(24)]
        S = Sched(nc, sems, dsems)

        used = {}

        def uniq(n):
            used[n] = used.get(n, 0) + 1
            return n if used[n] == 1 else f"{n}_v{used[n]}"

        def sb(es, name, shape, dt=F32, nbuf=1):
            return TT(es.enter_context(nc.sbuf_tensor(uniq("sb_" + name), list(shape), dt)), name, nbuf)

        def ps(es, name, shape=(128, 512), dt=F32):
            return TT(es.enter_context(nc.psum_tensor(uniq("ps_" + name), list(shape), dt)), name)

        def dump(name, ap, buf, shape):
            if name not in dbg:
                return
            d = nc.dram_tensor("dbg_" + name, list(shape), F32, kind="ExternalOutput").ap()
            dbg_d[name] = d
            S.dma(lambda e: e.dma_start(out=d, in_=ap), reads=[buf])

        def load(dst, dst_ap, src_ap, eng="sp"):
            S.dma(lambda e: e.dma_start(out=dst_ap, in_=src_ap), writes=[dst.b], eng=eng)

        ident32 = sb(top, "ident32", [128, 128]); ident16 = sb(top, "ident16", [128, 128], BF16)
        negtri16 = sb(top, "negtri16", [128, 128], BF16); negones16 = sb(top, "negones16", [128, 128], BF16)
        strict16 = sb(top, "strict16", [128, 128], BF16)
        shared = sb(top, "shared", [128, 16384], BF16)
        mssm16 = View(shared[:, 0:8192].rearrange("p (a b) -> p a b", a=NOWN), "mssm16")
        ysb16 = View(shared[:, 8192:16384].rearrange("p (a b) -> p a b", a=NOWN), "ysb16")
        ss_sb = sb(top, "ss_sb", [128, NOWN, 2])
        rstd_all = sb(top, "rstd_all", [128, NBLK])
        cstg = sb(top, "cstg", [128, 128])
        load(ident32, ident32[:], ident_d)
        S.op("dve", lambda e: e.tensor_copy(out=ident16[:], in_=ident32[:]), reads=[ident32.b], writes=[ident16.b])
        for dst, src in ((negtri16, negtri_d), (negones16, negones_d), (strict16, strict_d)):
            load(cstg, cstg[:], src)
            S.op("dve", lambda e, dst=dst: e.tensor_copy(out=dst[:], in_=cstg[:]), reads=[cstg.b], writes=[dst.b])

        def make_prologue(es, evac_eng, stat_eng="act", extra_pT=(), reuse_rstd=False):
            P = {}
            P["xc"] = sb(es, "xc", [128, 2, 1024]); P["xs16"] = sb(es, "xs16", [128, 4, 1024], BF16)
            P["junk"] = sb(es, "junk", [128, 1024], BF16); P["ss4"] = sb(es, "ss4", [128, 4]); P["rstd4"] = sb(es, "rstd4", [128, 4])
            P["hT"] = [sb(es, "hT0", [128, 8, 512], BF16)] * 2
            pTt = ps(es, "pTt", [128, 1024], BF16)

            class _PV:
                def __init__(s_): s_.b = pTt.b
                def __getitem__(s_, k): return pTt.t[:, 0:512][k]
            class _PVx:
                def __init__(s_, tt): s_.tt = tt; s_.b = tt.b
                def __getitem__(s_, k): return s_.tt.t[:].bitcast(BF16)[:, 0:512][k]
            P["pT"] = [_PV()] + [_PVx(t_) for t_ in extra_pT]
            P["n"] = 0
            P["defer"] = None

            def pro_a(c):
                xc, xs16, junk, ss4 = P["xc"], P["xs16"], P["junk"], P["ss4"]
                for hf in range(2):
                    load(xc, xc[:], xv[c * 512 + hf * 256:c * 512 + (hf + 1) * 256, :].rearrange("(b p) d -> p b d", p=128))
                    b0 = 4 * c + 2 * hf
                    if not reuse_rstd:
                        for i2 in range(2):
                            i = 2 * hf + i2
                            S.op("act", lambda e: e.activation(out=junk[:], in_=xc[:, i2, :], func=AF.Square, accum_out=ss4[:, i:i + 1]),
                                 reads=[xc.b], writes=[junk.b, ss4.b])
                        S.op("act", lambda e: e.activation(out=rstd_all[:, b0:b0 + 2], in_=ss4[:, 2 * hf:2 * hf + 2], func=AF.Ln, scale=1.0 / 1024, bias=EPS), reads=[ss4.b], writes=[rstd_all.b])
                        S.op("act", lambda e: e.activation(out=rstd_all[:, b0:b0 + 2], in_=rstd_all[:, b0:b0 + 2], func=AF.Exp, scale=-0.5), reads=[rstd_all.b], writes=[rstd_all.b])
                    for i2 in range(2):
                        i = 2 * hf + i2
                        if stat_eng == "act":
                            S.op("act", lambda e: e.activation(out=xs16[:, i, :], in_=xc[:, i2, :], func=AF.Copy, scale=rstd_all[:, b0 + i2:b0 + i2 + 1]),
                                 reads=[xc.b, rstd_all.b], writes=[xs16.b])
                        else:
                            S.op("dve", lambda e: e.tensor_scalar(out=xs16[:, i, :], in0=xc[:, i2, :], scalar1=rstd_all[:, b0 + i2:b0 + i2 + 1], scalar2=None, op0=ALU.mult),
                                 reads=[xc.b, rstd_all.b], writes=[xs16.b])

            def pro_b_units(c):
                xs16 = P["xs16"]
                hT = P["hT"][c % 2]
                units = []
                for dt in range(8):
                    def unit(dt=dt):
                        pT = P["pT"][dt % len(P["pT"])]
                        for i in range(4):
                            S.op("pe", lambda e: e.transpose(pT[:, i * 128:(i + 1) * 128], xs16[:, i, dt * 128:(dt + 1) * 128], ident16[:]),
                                 reads=[xs16.b, ident16.b], writes=[pT.b])
                        eng = evac_eng if isinstance(evac_eng, str) else evac_eng[dt % len(evac_eng)]

                        def evac(pT=pT, dt=dt, eng=eng):
                            if eng == "act":
                                S.op("act", lambda e: e.activation(out=hT[:, dt, :], in_=pT[:], func=AF.Copy), reads=[pT.b], writes=[hT.b])
                            else:
                                S.op(eng, lambda e: e.tensor_copy(out=hT[:, dt, :], in_=pT[:]), reads=[pT.b], writes=[hT.b])
                        if P.get("defer") is not None:
                            P["defer"].append(evac)
                        else:
                            evac()
                    units.append(unit)
                return hT, units
            return pro_a, pro_b_units, P

        def load_win(es, name, colspecs, stgs):
            tot = sum(n for _, n, _ in colspecs)
            W = sb(es, name, [128, 8, tot], BF16)
            k = 0
            for dt in range(8):
                o = 0
                for (c0, n, sc) in colspecs:
                    stg = stgs[k % len(stgs)]
                    k += 1
                    load(stg, stg[:, 0:n], w_in[dt * 128:(dt + 1) * 128, c0:c0 + n])
                    gx = g1 if sc == 1.0 else g1q
                    S.op("act", lambda e: e.activation(out=W[:, dt, o:o + n], in_=stg[:, 0:n], func=AF.Copy, scale=gx[:, dt:dt + 1]), reads=[stg.b, gx.b], writes=[W.b])
                    o += n
            return W

        g1 = sb(top, "g1", [128, 8]); load(g1, g1[:], g1_d)
        g1q = sb(top, "g1q", [128, 8])
        S.op("dve", lambda e: e.tensor_scalar(out=g1q[:], in0=g1[:], scalar1=0.125, scalar2=None, op0=ALU.mult), reads=[g1.b], writes=[g1q.b])

        def phase_S():
            es = ExitStack()
            with es:
                stgs = [sb(es, f"stgS{i}", [128, 1024]) for i in range(2)]
                stg = stgs[0]
                Wu16 = load_win(es, "Wu16", [(1536, 512, 1.0)], stgs)
                Wglu16 = sb(es, "Wglu16", [128, 4, 1024], BF16)
                for ct in range(4):
                    stg = stgs[ct % 2]
                    load(stg, stg[:], w_glu[ct * 128:(ct + 1) * 128, :])
                    S.op("act", lambda e: e.activation(out=Wglu16[:, ct, :], in_=stg[:], func=AF.Copy), reads=[stg.b], writes=[Wglu16.b])
                dsk = sb(es, "dsk", [128, 4]); load(dsk, dsk[:], dsk_d)
                cosT = sb(es, "cosT", [128, 16, 128]); sinT = sb(es, "sinT", [128, 16, 128]); Rt = sb(es, "Rt", [128, 16, 128])
                Bre16 = sb(es, "Bre16", [128, 16, 128], BF16); Bim16 = sb(es, "Bim16", [128, 16, 128], BF16)
                Cre16 = sb(es, "Cre16", [128, 16, 128], BF16); Cim16 = sb(es, "Cim16", [128, 16, 128], BF16)
                rr2 = sb(es, "rr2", [128, 2, 16]); init = sb(es, "init", [128, 2, 16])
                Apr16 = sb(es, "Apr16", [128, 16, 128], BF16); Api16 = sb(es, "Api16", [128, 16, 128], BF16)
                Bpre = sb(es, "Bpre", [128, 16, 32]); Bpim = sb(es, "Bpim", [128, 16, 32]); a128 = sb(es, "a128", [128, 2, 16])
                pbr = [ps(es, f"pbr{i}") for i in range(2)]; pbi = [ps(es, f"pbi{i}") for i in range(2)]
                py = ps(es, "py")
                pin = [ps(es, f"pinS{i}") for i in range(2)]

                su = ExitStack()
                with su:
                    def small(name, shape=(128, 16)):
                        return sb(su, name, shape)
                    lamre = small("lamre"); lamim = small("lamim"); logdt = small("logdt")
                    bre = small("bre", [128, 16, 16]); bim = small("bim", [128, 16, 16])
                    creT = small("creTs", [128, 16, 16]); cimT = small("cimTs", [128, 16, 16])
                    ramp = small("ramp", [128, 128])
                    for t_, d_ in ((lamre, lamre_d), (lamim, lamim_d), (logdt, logdt_d), (bre, bre_d), (bim, bim_d), (creT, cre_d), (cimT, cim_d), (ramp, ramp_d)):
                        load(t_, t_[:], d_)
                    dtv = small("dtv"); ar = small("ar"); th = small("th"); rr = small("rr")
                    S.op("act", lambda e: e.activation(out=dtv[:], in_=logdt[:], func=AF.Exp), reads=[logdt.b], writes=[dtv.b])
                    S.op("dve", lambda e: e.tensor_tensor(out=ar[:], in0=lamre[:], in1=dtv[:], op=ALU.mult), reads=[lamre.b, dtv.b], writes=[ar.b])
                    S.op("dve", lambda e: e.tensor_tensor(out=th[:], in0=lamim[:], in1=dtv[:], op=ALU.mult), reads=[lamim.b, dtv.b], writes=[th.b])
                    S.op("act", lambda e: e.activation(out=rr[:], in_=ar[:], func=AF.Exp), reads=[ar.b], writes=[rr.b])

                    sc_tmp = {}

                    def sincos(name, ang, n, cos_out, sin_out, cb, sbuf_):
                        if n not in sc_tmp:
                            sc_tmp[n] = (sb(su, name + "_tq", [128, n]), sb(su, name + "_ti", [128, n], mybir.dt.int32), sb(su, name + "_tf", [128, n]))
                        tq, ti, tf = sc_tmp[n]
                        for off, outap, ob in ((0.0, sin_out, sbuf_), (0.25, cos_out, cb)):
                            S.op("dve", lambda e, off=off: e.tensor_scalar(out=tq[:], in0=ang[:], scalar1=1.0 / (2 * np.pi), scalar2=off, op0=ALU.mult, op1=ALU.add),
                                 reads=[ang.b], writes=[tq.b])
                            S.op("dve", lambda e: e.tensor_copy(out=ti[:], in_=tq[:]), reads=[tq.b], writes=[ti.b])
                            S.op("dve", lambda e: e.tensor_copy(out=tf[:], in_=ti[:]), reads=[ti.b], writes=[tf.b])
                            S.op("dve", lambda e: e.tensor_tensor(out=tq[:], in0=tq[:], in1=tf[:], op=ALU.subtract), reads=[tq.b, tf.b], writes=[tq.b])
                            S.op("act", lambda e, outap=outap: e.activation(out=outap, in_=tq[:], func=AF.Sin, scale=6.28318), reads=[tq.b], writes=[ob])

                    cth = small("cth"); sth = small("sth")
                    sincos("a", th, 16, cth[:], sth[:], cth.b, sth.b)
                    a_re = small("a_re"); a_im = small("a_im")
                    S.op("dve", lambda e: e.tensor_tensor(out=a_re[:], in0=rr[:], in1=cth[:], op=ALU.mult), reads=[rr.b, cth.b], writes=[a_re.b])
                    S.op("dve", lambda e: e.tensor_tensor(out=a_im[:], in0=rr[:], in1=sth[:], op=ALU.mult), reads=[rr.b, sth.b], writes=[a_im.b])
                    nre = small("nre"); den = small("den"); t0 = small("t0"); t1 = small("t1"); cf_re = small("cf_re"); cf_im = small("cf_im"); ncf_im = small("ncf_im")
                    S.op("dve", lambda e: e.tensor_scalar(out=nre[:], in0=a_re[:], scalar1=-1.0, scalar2=None, op0=ALU.add), reads=[a_re.b], writes=[nre.b])
                    S.op("dve", lambda e: e.tensor_tensor(out=t0[:], in0=lamre[:], in1=lamre[:], op=ALU.mult), reads=[lamre.b], writes=[t0.b])
                    S.op("dve", lambda e: e.tensor_tensor(out=t1[:], in0=lamim[:], in1=lamim[:], op=ALU.mult), reads=[lamim.b], writes=[t1.b])
                    S.op("dve", lambda e: e.tensor_tensor(out=den[:], in0=t0[:], in1=t1[:], op=ALU.add), reads=[t0.b, t1.b], writes=[den.b])
                    S.op("dve", lambda e: e.reciprocal(out=den[:], in_=den[:]), reads=[den.b], writes=[den.b])
                    S.op("dve", lambda e: e.tensor_tensor(out=t0[:], in0=nre[:], in1=lamre[:], op=ALU.mult), reads=[nre.b, lamre.b], writes=[t0.b])
                    S.op("dve", lambda e: e.tensor_tensor(out=t1[:], in0=a_im[:], in1=lamim[:], op=ALU.mult), reads=[a_im.b, lamim.b], writes=[t1.b])
                    S.op("dve", lambda e: e.tensor_tensor(out=t0[:], in0=t0[:], in1=t1[:], op=ALU.add), reads=[t0.b, t1.b], writes=[t0.b])
                    S.op("dve", lambda e: e.tensor_tensor(out=cf_re[:], in0=t0[:], in1=den[:], op=ALU.mult), reads=[t0.b, den.b], writes=[cf_re.b])
                    S.op("dve", lambda e: e.tensor_tensor(out=t0[:], in0=a_im[:], in1=lamre[:], op=ALU.mult), reads=[a_im.b, lamre.b], writes=[t0.b])
                    S.op("dve", lambda e: e.tensor_tensor(out=t1[:], in0=nre[:], in1=lamim[:], op=ALU.mult), reads=[nre.b, lamim.b], writes=[t1.b])
                    S.op("dve", lambda e: e.tensor_tensor(out=t0[:], in0=t0[:], in1=t1[:], op=ALU.subtract), reads=[t0.b, t1.b], writes=[t0.b])
                    S.op("dve", lambda e: e.tensor_tensor(out=cf_im[:], in0=t0[:], in1=den[:], op=ALU.mult), reads=[t0.b, den.b], writes=[cf_im.b])
                    S.op("dve", lambda e: e.tensor_scalar(out=ncf_im[:], in0=cf_im[:], scalar1=-1.0, scalar2=None, op0=ALU.mult), reads=[cf_im.b], writes=[ncf_im.b])
                    Mre = sb(su, "Mre", [128, 16, 128]); Mim = sb(su, "Mim", [128, 16, 128]); tb = sb(su, "tb", [128, 16])
                    S.op("pool", lambda e: e.memset(Mre[:], 0.0), writes=[Mre.b])
                    S.op("pool", lambda e: e.memset(Mim[:], 0.0), writes=[Mim.b])
                    S.op("pool", lambda e: e.memset(Cre16[:], 0.0), writes=[Cre16.b])
                    S.op("pool", lambda e: e.memset(Cim16[:], 0.0), writes=[Cim16.b])
                    for j in range(16):
                        jj = j % 4
                        for g2 in range(2):
                            p0, p1 = 64 * g2, 64 * g2 + 64
                            c0 = 32 * jj + 16 * g2
                            S.op("dve", lambda e, j=j, p0=p0, p1=p1: e.tensor_scalar(out=tb[p0:p1, :], in0=bre[p0:p1, j, :], scalar1=cf_re[p0:p1, j:j + 1], scalar2=None, op0=ALU.mult),
                                 reads=[bre.b, cf_re.b], writes=[tb.b])
                            S.op("dve", lambda e, j=j, p0=p0, p1=p1, c0=c0: e.scalar_tensor_tensor(out=Mre[p0:p1, j, c0:c0 + 16], in0=bim[p0:p1, j, :], scalar=ncf_im[p0:p1, j:j + 1], in1=tb[p0:p1, :], op0=ALU.mult, op1=ALU.add),
                                 reads=[bim.b, ncf_im.b, tb.b], writes=[Mre.b])
                            S.op("dve", lambda e, j=j, p0=p0, p1=p1: e.tensor_scalar(out=tb[p0:p1, :], in0=bim[p0:p1, j, :], scalar1=cf_re[p0:p1, j:j + 1], scalar2=None, op0=ALU.mult),
                                 reads=[bim.b, cf_re.b], writes=[tb.b])
                            S.op("dve", lambda e, j=j, p0=p0, p1=p1, c0=c0: e.scalar_tensor_tensor(out=Mim[p0:p1, j, c0:c0 + 16], in0=bre[p0:p1, j, :], scalar=cf_im[p0:p1, j:j + 1], in1=tb[p0:p1, :], op0=ALU.mult, op1=ALU.add),
                                 reads=[bre.b, cf_im.b, tb.b], writes=[Mim.b])
                            S.op("pool", lambda e, j=j, p0=p0, p1=p1, c0=c0: e.tensor_copy(out=Cre16[p0:p1, j, c0:c0 + 16], in_=creT[p0:p1, j, :]), reads=[creT.b], writes=[Cre16.b])
                            S.op("pool", lambda e, j=j, p0=p0, p1=p1, c0=c0: e.tensor_scalar(out=Cim16[p0:p1, j, c0:c0 + 16], in0=cimT[p0:p1, j, :], scalar1=-1.0, scalar2=None, op0=ALU.mult), reads=[cimT.b], writes=[Cim16.b])
                    for j in range(16):
                        for (M_, B_) in ((Mre, Bre16), (Mim, Bim16)):
                            S.op("pe", lambda e, j=j, M_=M_: e.transpose(py[:, 0:128], M_[:, j, :], ident32[:]), reads=[M_.b, ident32.b], writes=[py.b])
                            S.op("dve", lambda e, j=j, B_=B_: e.tensor_copy(out=B_[:, j, :], in_=py[:, 0:128]), reads=[py.b], writes=[B_.b])
                    ang = sb(su, "ang", [128, 16, 128])
                    for j in range(16):
                        S.op("dve", lambda e, j=j: e.tensor_scalar(out=ang[:, j, :], in0=ramp[:], scalar1=th[:, j:j + 1], scalar2=None, op0=ALU.mult), reads=[ramp.b, th.b], writes=[ang.b])
                        S.op("pool", lambda e, j=j: e.tensor_scalar(out=Rt[:, j, :], in0=ramp[:], scalar1=0.0, scalar2=rr[:, j:j + 1], op0=ALU.mult, op1=ALU.add), reads=[ramp.b, rr.b], writes=[Rt.b])
                    angf = TT(ang.t, "angf"); angf.b = ang.b
                    class _V:
                        def __init__(s_, t, b): s_.t = t; s_.b = b
                        def __getitem__(s_, k): return s_.t[:].rearrange("p a b -> p (a b)")
                    sincos("tab", _V(ang.t, ang.b), 2048, cosT[:].rearrange("p a b -> p (a b)"), sinT[:].rearrange("p a b -> p (a b)"), cosT.b, sinT.b)
                    S.op("pool", lambda e: e.memset(Rt[:, :, 0:1], 0.0), writes=[Rt.b])
                    S.op("dve", lambda e: e.tensor_copy(out=rr2[:, 0, :], in_=rr[:]), reads=[rr.b], writes=[rr2.b])
                    S.op("dve", lambda e: e.tensor_copy(out=rr2[:, 1, :], in_=rr[:]), reads=[rr.b], writes=[rr2.b])
                    S.op("dve", lambda e: e.memset(init[:], 0.0), writes=[init.b])
                    ramp2 = small("ramp2", [128, 128]); load(ramp2, ramp2[:], ramp2_d)
                    ang2 = ang; rpow = sb(su, "rpow", [128, 16, 128])
                    c2 = sb(su, "c2", [128, 16, 128]); s2 = sb(su, "s2", [128, 16, 128])
                    for j in range(16):
                        S.op("dve", lambda e: e.tensor_scalar(out=ang2[:, j, :], in0=ramp2[:], scalar1=th[:, j:j + 1], scalar2=None, op0=ALU.mult), reads=[ramp2.b, th.b], writes=[ang2.b])
                    sincos("tab2", _V(ang2.t, ang2.b), 2048, c2[:].rearrange("p a b -> p (a b)"), s2[:].rearrange("p a b -> p (a b)"), c2.b, s2.b)
                    for j in range(16):
                        S.op("act", lambda e: e.activation(out=rpow[:, j, :], in_=ramp2[:], func=AF.Exp, scale=ar[:, j:j + 1]), reads=[ramp2.b, ar.b], writes=[rpow.b])
                    S.op("dve", lambda e: e.tensor_tensor(out=c2[:], in0=c2[:], in1=rpow[:], op=ALU.mult), reads=[c2.b, rpow.b], writes=[c2.b])
                    S.op("dve", lambda e: e.tensor_tensor(out=s2[:], in0=s2[:], in1=rpow[:], op=ALU.mult), reads=[s2.b, rpow.b], writes=[s2.b])
                    for j in range(16):
                        for (src_, dst_) in ((c2, Apr16), (s2, Api16)):
                            S.op("pe", lambda e: e.transpose(py[:, 0:128], src_[:, j, :], ident32[:]), reads=[src_.b, ident32.b], writes=[py.b])
                            S.op("dve", lambda e: e.tensor_copy(out=dst_[:, j, :], in_=py[:, 0:128]), reads=[py.b], writes=[dst_.b])
                        jj = j % 4
                        S.op("dve", lambda e: e.tensor_copy(out=Bpre[:, j, :], in_=Mre[:, j, 32 * jj:32 * jj + 32]), reads=[Mre.b], writes=[Bpre.b])
                        S.op("dve", lambda e: e.tensor_copy(out=Bpim[:, j, :], in_=Mim[:, j, 32 * jj:32 * jj + 32]), reads=[Mim.b], writes=[Bpim.b])
                    r128 = small("r128")
                    S.op("act", lambda e: e.activation(out=r128[:], in_=ar[:], func=AF.Exp, scale=128.0), reads=[ar.b], writes=[r128.b])
                    S.op("dve", lambda e: e.tensor_tensor(out=a128[:, 0, :], in0=r128[:], in1=cosT[:, :, 127], op=ALU.mult), reads=[r128.b, cosT.b], writes=[a128.b])
                    S.op("dve", lambda e: e.tensor_tensor(out=a128[:, 1, :], in0=r128[:], in1=sinT[:, :, 127], op=ALU.mult), reads=[r128.b, sinT.b], writes=[a128.b])
                    dump("cosT", cosT[:].rearrange("p a b -> p (a b)"), cosT.b, [128, 2048])
                    dump("Rt", Rt[:].rearrange("p a b -> p (a b)"), Rt.b, [128, 2048])
                    S.barrier()
                pro_a, pro_b_units, _P = make_prologue(es, "act")
                pend = []
                uT16 = [sb(es, f"uT16_{i}", [128, 4, 128], BF16) for i in range(2)]
                utok16 = [sb(es, f"utok16_{i}", [128, 3, 512], BF16) for i in range(2)]
                qri = sb(es, "qri", [128, 2, 16]); m8 = [sb(es, f"m8_{i}", [128, 16]) for i in range(4)]
                u32 = [sb(es, f"u32_{i}", [128, 4, 128]) for i in range(2)]
                tmpA = [sb(es, f"tmpA{i}", [128, 512]) for i in range(4)]
                fS = shared[:, 8192:16384].bitcast(F32)
                tmpB = [View(fS[:, i * 512:(i + 1) * 512], f"tmpB{i}") for i in range(4)]
                bts = [sb(es, "bt0", [128, 2, 16, 128])] * 2; Wt = sb(es, "Wt", [128, 2, 16, 128])
                xri16 = sb(es, "xri16", [128, 2, 16, 128], BF16)
                m6 = [sb(es, f"m6_{i}", [128, 16]) for i in range(4)]
                yv = View(fS[:, 2048:2560], "yv"); g1t = View(fS[:, 2560:3072], "g1t"); g2t = View(fS[:, 3072:3584], "g2t"); gl16 = sb(es, "gl16", [128, 4, 128], BF16)
                ysm = View(fS[:, 3584:4096], "ysm"); sss = sb(es, "sss", [128, 1]); rss = sb(es, "rss", [128, 1])
                junkS = sb(es, "junkS", [128, 512], BF16)
                nq = [0]

                def inproj_units(c, hT):
                    return [lambda i=i: inproj_tok(c, hT, i) for i in range(3)] + [lambda ct=ct: inproj_u1(c, hT, ct) for ct in range(4)]

                def inproj_tok(c, hT, i):
                    pn = pin[i % 2]
                    for dt in range(8):
                        S.op("pe", lambda e: e.matmul(pn[:], lhsT=hT[:, dt, i * 128:(i + 1) * 128], rhs=Wu16[:, dt, :], start=(dt == 0), stop=(dt == 7)), reads=[Wu16.b, hT.b], writes=[pn.b])
                    S.op("act", lambda e: e.activation(out=utok16[c % 2][:, i, :], in_=pn[:], func=AF.Copy), reads=[pn.b], writes=[utok16[c % 2].b])

                def inproj_u1(c, hT, ct):
                    pn = pin[ct % 2]
                    for dt in range(8):
                        S.op("pe", lambda e: e.matmul(pn[:, 0:128], lhsT=Wu16[:, dt, ct * 128:(ct + 1) * 128], rhs=hT[:, dt, 384:512], start=(dt == 0), stop=(dt == 7)), reads=[Wu16.b, hT.b], writes=[pn.b])
                    S.op("act", lambda e: e.activation(out=uT16[c % 2][:, ct, :], in_=pn[:, 0:128], func=AF.Copy), reads=[pn.b], writes=[uT16[c % 2].b])
                    S.op("act", lambda e: e.activation(out=u32[c % 2][:, ct, :], in_=pn[:, 0:128], func=AF.Copy), reads=[pn.b], writes=[u32[c % 2].b])

                def skip_block(c, i):
                    if i == 0:
                        assert not pend
                        if c + 1 < NOWN:
                            pro_a(c + 1)
                            hTn, un = pro_b_units(c + 1)
                            pend.extend(un + inproj_units(c + 1, hTn))
                    sl = nq[0] % 2
                    nq[0] += 1
                    pr, pi_ = pbr[sl], pbi[sl]
                    ut = utok16[c % 2]
                    for j in range(16):
                        S.op("pe", lambda e: e.matmul(pr[:, 32 * j:32 * j + 32], lhsT=Apr16[:, j, :], rhs=ut[:, i, 32 * j:32 * j + 32], start=(j == 0), stop=(j == 15), skip_group_check=True), reads=[Apr16.b, ut.b], writes=[pr.b])
                        S.op("pe", lambda e: e.matmul(pi_[:, 32 * j:32 * j + 32], lhsT=Api16[:, j, :], rhs=ut[:, i, 32 * j:32 * j + 32], start=(j == 0), stop=(j == 15), skip_group_check=True), reads=[Api16.b, ut.b], writes=[pi_.b])
                    for _ in range(3):
                        if pend:
                            pend.pop(0)()
                    ta = tmpA if sl == 0 else tmpB
                    Bre_f = Bpre[:].rearrange("p a b -> p (a b)"); Bim_f = Bpim[:].rearrange("p a b -> p (a b)")
                    S.op("dve", lambda e: e.tensor_tensor(out=ta[0][:], in0=pr[:], in1=Bre_f, op=ALU.mult), reads=[pr.b, Bpre.b], writes=[ta[0].b])
                    S.op("dve", lambda e: e.tensor_tensor(out=ta[1][:], in0=pi_[:], in1=Bim_f, op=ALU.mult), reads=[pi_.b, Bpim.b], writes=[ta[1].b])
                    S.op("dve", lambda e: e.tensor_tensor(out=ta[2][:], in0=pi_[:], in1=Bre_f, op=ALU.mult), reads=[pi_.b, Bpre.b], writes=[ta[2].b])
                    S.op("dve", lambda e: e.tensor_tensor(out=ta[3][:], in0=pr[:], in1=Bim_f, op=ALU.mult), reads=[pr.b, Bpim.b], writes=[ta[3].b])
                    S.op("dve", lambda e: e.tensor_tensor(out=ta[0][:], in0=ta[0][:], in1=ta[1][:], op=ALU.subtract), reads=[ta[0].b, ta[1].b], writes=[ta[0].b])
                    S.op("dve", lambda e: e.tensor_tensor(out=ta[2][:], in0=ta[2][:], in1=ta[3][:], op=ALU.add), reads=[ta[2].b, ta[3].b], writes=[ta[2].b])
                    S.op("dve", lambda e: e.tensor_reduce(out=qri[:, 0, :], in_=ta[0][:].rearrange("p (a b) -> p a b", a=16), axis=AX.X, op=ALU.add), reads=[ta[0].b], writes=[qri.b])
                    S.op("dve", lambda e: e.tensor_reduce(out=qri[:, 1, :], in_=ta[2][:].rearrange("p (a b) -> p a b", a=16), axis=AX.X, op=ALU.add), reads=[ta[2].b], writes=[qri.b])
                    S.op("dve", lambda e: e.tensor_tensor(out=m8[0][:], in0=init[:, 0, :], in1=a128[:, 0, :], op=ALU.mult), reads=[init.b, a128.b], writes=[m8[0].b])
                    S.op("dve", lambda e: e.tensor_tensor(out=m8[1][:], in0=init[:, 1, :], in1=a128[:, 1, :], op=ALU.mult), reads=[init.b, a128.b], writes=[m8[1].b])
                    S.op("dve", lambda e: e.tensor_tensor(out=m8[2][:], in0=init[:, 1, :], in1=a128[:, 0, :], op=ALU.mult), reads=[init.b, a128.b], writes=[m8[2].b])
                    S.op("dve", lambda e: e.tensor_tensor(out=m8[3][:], in0=init[:, 0, :], in1=a128[:, 1, :], op=ALU.mult), reads=[init.b, a128.b], writes=[m8[3].b])
                    S.op("dve", lambda e: e.tensor_tensor(out=m8[0][:], in0=m8[0][:], in1=m8[1][:], op=ALU.subtract), reads=[m8[0].b, m8[1].b], writes=[m8[0].b])
                    S.op("dve", lambda e: e.tensor_tensor(out=m8[2][:], in0=m8[2][:], in1=m8[3][:], op=ALU.add), reads=[m8[2].b, m8[3].b], writes=[m8[2].b])
                    S.op("dve", lambda e: e.tensor_tensor(out=init[:, 0, :], in0=m8[0][:], in1=qri[:, 0, :], op=ALU.add), reads=[m8[0].b, qri.b], writes=[init.b])
                    S.op("dve", lambda e: e.tensor_tensor(out=init[:, 1, :], in0=m8[2][:], in1=qri[:, 1, :], op=ALU.add), reads=[m8[2].b, qri.b], writes=[init.b])

                def demod(g):
                    c, i = g // 4, g % 4
                    bt = bts[g % 2]
                    uT = uT16[c % 2]
                    for qd in range(4):
                        sl = nq[0] % 2
                        nq[0] += 1
                        pr, pi_ = pbr[sl], pbi[sl]
                        for jj in range(4):
                            j = 4 * qd + jj
                            S.op("pe", lambda e, j=j, jj=jj, pr=pr: e.matmul(pr[:, jj * 128:(jj + 1) * 128], lhsT=Bre16[:, j, :], rhs=uT[:, qd, :], start=(jj == 0), stop=(jj == 3), skip_group_check=True),
                                 reads=[Bre16.b, uT.b], writes=[pr.b])
                            S.op("pe", lambda e, j=j, jj=jj, pi_=pi_: e.matmul(pi_[:, jj * 128:(jj + 1) * 128], lhsT=Bim16[:, j, :], rhs=uT[:, qd, :], start=(jj == 0), stop=(jj == 3), skip_group_check=True),
                                 reads=[Bim16.b, uT.b], writes=[pi_.b])
                        for _ in range(2):
                            if pend:
                                pend.pop(0)()
                        cs = cosT[:, 4 * qd:4 * qd + 4, :].rearrange("p a b -> p (a b)")
                        sn = sinT[:, 4 * qd:4 * qd + 4, :].rearrange("p a b -> p (a b)")
                        ta = tmpA if sl == 0 else tmpB
                        S.op("dve", lambda e, pr=pr, cs=cs, ta=ta: e.tensor_tensor(out=ta[0][:], in0=pr[:], in1=cs, op=ALU.mult), reads=[pr.b, cosT.b], writes=[ta[0].b])
                        S.op("dve", lambda e, pi_=pi_, sn=sn, ta=ta: e.tensor_tensor(out=ta[1][:], in0=pi_[:], in1=sn, op=ALU.mult), reads=[pi_.b, sinT.b], writes=[ta[1].b])
                        S.op("dve", lambda e, pi_=pi_, cs=cs, ta=ta: e.tensor_tensor(out=ta[2][:], in0=pi_[:], in1=cs, op=ALU.mult), reads=[pi_.b, cosT.b], writes=[ta[2].b])
                        S.op("dve", lambda e, pr=pr, sn=sn, ta=ta: e.tensor_tensor(out=ta[3][:], in0=pr[:], in1=sn, op=ALU.mult), reads=[pr.b, sinT.b], writes=[ta[3].b])
                        S.op("pool", lambda e, qd=qd, ta=ta: e.tensor_tensor(out=bt[:, 0, 4 * qd:4 * qd + 4, :].rearrange("p a b -> p (a b)"), in0=ta[0][:], in1=ta[1][:], op=ALU.add),
                             reads=[ta[0].b, ta[1].b], writes=[bt.b])
                        S.op("pool", lambda e, qd=qd, ta=ta: e.tensor_tensor(out=bt[:, 1, 4 * qd:4 * qd + 4, :].rearrange("p a b -> p (a b)"), in0=ta[2][:], in1=ta[3][:], op=ALU.subtract),
                             reads=[ta[2].b, ta[3].b], writes=[bt.b])

                def tail(g):
                    bt = bts[g % 2]
                    S.op("dve", lambda e: e.tensor_tensor(out=m6[0][:, 0:16], in0=init[:, 0, :], in1=rr2[:, 0, :], op=ALU.mult), reads=[init.b, rr2.b], writes=[m6[0].b])
                    S.op("dve", lambda e: e.tensor_tensor(out=m6[1][:, 0:16], in0=init[:, 1, :], in1=rr2[:, 1, :], op=ALU.mult), reads=[init.b, rr2.b], writes=[m6[1].b])
                    S.op("dve", lambda e: e.tensor_tensor(out=bt[:, 0, :, 0], in0=bt[:, 0, :, 0], in1=m6[0][:], op=ALU.add), reads=[bt.b, m6[0].b], writes=[bt.b])
                    S.op("dve", lambda e: e.tensor_tensor(out=bt[:, 1, :, 0], in0=bt[:, 1, :, 0], in1=m6[1][:], op=ALU.add), reads=[bt.b, m6[1].b], writes=[bt.b])
                    Rf = Rt[:].rearrange("p a b -> p (a b)")
                    for ri in range(2):
                        S.op("dve", lambda e, ri=ri: e.tensor_tensor_scan(out=Wt[:, ri, :, :].rearrange("p a b -> p (a b)"), data0=Rf, data1=bt[:, ri, :, :].rearrange("p a b -> p (a b)"), initial=0.0, op0=ALU.mult, op1=ALU.add),
                             reads=[Rt.b, bt.b], writes=[Wt.b])
                    wr, wi = Wt[:, 0, :, 127], Wt[:, 1, :, 127]
                    c128, s128 = cosT[:, :, 127], sinT[:, :, 127]
                    S.op("dve", lambda e: e.tensor_tensor(out=m6[0][:], in0=wr, in1=c128, op=ALU.mult), reads=[Wt.b, cosT.b], writes=[m6[0].b])
                    S.op("dve", lambda e: e.tensor_tensor(out=m6[1][:], in0=wi, in1=s128, op=ALU.mult), reads=[Wt.b, sinT.b], writes=[m6[1].b])
                    S.op("dve", lambda e: e.tensor_tensor(out=m6[2][:], in0=wi, in1=c128, op=ALU.mult), reads=[Wt.b, cosT.b], writes=[m6[2].b])
                    S.op("dve", lambda e: e.tensor_tensor(out=m6[3][:], in0=wr, in1=s128, op=ALU.mult), reads=[Wt.b, sinT.b], writes=[m6[3].b])
                    S.op("dve", lambda e: e.tensor_tensor(out=init[:, 0, :], in0=m6[0][:], in1=m6[1][:], op=ALU.subtract), reads=[m6[0].b, m6[1].b], writes=[init.b])
                    S.op("dve", lambda e: e.tensor_tensor(out=init[:, 1, :], in0=m6[2][:], in1=m6[3][:], op=ALU.add), reads=[m6[2].b, m6[3].b], writes=[init.b])

                def own_block(c):
                    ob = c
                    for qd in range(4):
                        cs = cosT[:, 4 * qd:4 * qd + 4, :].rearrange("p a b -> p (a b)")
                        sn = sinT[:, 4 * qd:4 * qd + 4, :].rearrange("p a b -> p (a b)")
                        wrq = Wt[:, 0, 4 * qd:4 * qd + 4, :].rearrange("p a b -> p (a b)")
                        wiq = Wt[:, 1, 4 * qd:4 * qd + 4, :].rearrange("p a b -> p (a b)")
                        ta = tmpA if qd % 2 == 0 else tmpB
                        S.op("dve", lambda e, ta=ta, wrq=wrq, cs=cs: e.tensor_tensor(out=ta[0][:], in0=wrq, in1=cs, op=ALU.mult), reads=[Wt.b, cosT.b], writes=[ta[0].b])
                        S.op("dve", lambda e, ta=ta, wiq=wiq, sn=sn: e.tensor_tensor(out=ta[1][:], in0=wiq, in1=sn, op=ALU.mult), reads=[Wt.b, sinT.b], writes=[ta[1].b])
                        S.op("dve", lambda e, ta=ta, wiq=wiq, cs=cs: e.tensor_tensor(out=ta[2][:], in0=wiq, in1=cs, op=ALU.mult), reads=[Wt.b, cosT.b], writes=[ta[2].b])
                        S.op("dve", lambda e, ta=ta, wrq=wrq, sn=sn: e.tensor_tensor(out=ta[3][:], in0=wrq, in1=sn, op=ALU.mult), reads=[Wt.b, sinT.b], writes=[ta[3].b])
                        S.op("dve", lambda e, ta=ta, qd=qd: e.tensor_tensor(out=xri16[:, 0, 4 * qd:4 * qd + 4, :].rearrange("p a b -> p (a b)"), in0=ta[0][:], in1=ta[1][:], op=ALU.subtract),
                             reads=[ta[0].b, ta[1].b], writes=[xri16.b])
                        S.op("dve", lambda e, ta=ta, qd=qd: e.tensor_tensor(out=xri16[:, 1, 4 * qd:4 * qd + 4, :].rearrange("p a b -> p (a b)"), in0=ta[2][:], in1=ta[3][:], op=ALU.add),
                             reads=[ta[2].b, ta[3].b], writes=[xri16.b])
                    for ct in range(4):
                        n = 0
                        for jj in range(4):
                            j = 4 * ct + jj
                            for (C_, ri) in ((Cre16, 0), (Cim16, 1)):
                                S.op("pe", lambda e, ct=ct, j=j, C_=C_, ri=ri, n=n: e.matmul(py[:, ct * 128:(ct + 1) * 128], lhsT=C_[:, j, :], rhs=xri16[:, ri, j, :], start=(n == 0 and ct == 0), stop=(n == 7 and ct == 3), skip_group_check=True),
                                     reads=[C_.b, xri16.b], writes=[py.b])
                                n += 1
                    for ct in range(4):
                        S.op("dve", lambda e, ct=ct: e.scalar_tensor_tensor(out=yv[:, ct * 128:(ct + 1) * 128], in0=u32[c % 2][:, ct, :], scalar=dsk[:, ct:ct + 1], in1=py[:, ct * 128:(ct + 1) * 128], op0=ALU.mult, op1=ALU.add),
                             reads=[u32[c % 2].b, dsk.b, py.b], writes=[yv.b])
                    dump(f"ypre{ob}", yv[:], yv.b, [128, 512])

                def own_B(c):
                    ob = c
                    S.op("dve", lambda e: e.tensor_tensor(out=g1t[:], in0=yv[:], in1=yv[:], op=ALU.mult), reads=[yv.b], writes=[g1t.b])
                    S.op("dve", lambda e: e.tensor_scalar(out=g1t[:], in0=g1t[:], scalar1=0.044715, scalar2=1.0, op0=ALU.mult, op1=ALU.add), reads=[g1t.b], writes=[g1t.b])
                    S.op("dve", lambda e: e.tensor_tensor(out=g1t[:], in0=g1t[:], in1=yv[:], op=ALU.mult), reads=[g1t.b, yv.b], writes=[g1t.b])
                    S.op("act", lambda e: e.activation(out=g2t[:], in_=g1t[:], func=AF.Sigmoid, scale=2.0 * GELU_C), reads=[g1t.b], writes=[g2t.b])
                    S.op("dve", lambda e: e.tensor_tensor(out=gl16[:].rearrange("p a b -> p (a b)"), in0=yv[:], in1=g2t[:], op=ALU.mult), reads=[yv.b, g2t.b], writes=[gl16.b])
                    for half in range(2):
                        for ct in range(4):
                            S.op("pe", lambda e, half=half, ct=ct: e.matmul(pin[half][:], lhsT=gl16[:, ct, :], rhs=Wglu16[:, ct, half * 512:(half + 1) * 512], start=(ct == 0), stop=(ct == 3)),
                                 reads=[gl16.b, Wglu16.b], writes=[pin[half].b])

                def own_B2(c):
                    ob = c
                    S.op("act", lambda e: e.activation(out=g1t[:], in_=pin[1][:], func=AF.Sigmoid), reads=[pin[1].b], writes=[g1t.b])
                    S.op("dve", lambda e: e.tensor_tensor(out=ysm[:], in0=pin[0][:], in1=g1t[:], op=ALU.mult), reads=[pin[0].b, g1t.b], writes=[ysm.b])
                    dump(f"yssm{ob}", ysm[:], ysm.b, [128, 512])
                    S.op("act", lambda e: e.activation(out=junkS[:], in_=ysm[:], func=AF.Square, accum_out=sss[:, 0:1]), reads=[ysm.b], writes=[junkS.b, sss.b])
                    S.op("act", lambda e: e.activation(out=rss[:], in_=sss[:], func=AF.Ln, scale=1.0 / 512, bias=EPS), reads=[sss.b], writes=[rss.b])
                    S.op("act", lambda e: e.activation(out=rss[:], in_=rss[:], func=AF.Exp, scale=-0.5), reads=[rss.b], writes=[rss.b])
                    S.op("act", lambda e: e.activation(out=mssm16[:, ob, :], in_=ysm[:], func=AF.Copy, scale=rss[:, 0:1]), reads=[ysm.b, rss.b], writes=[mssm16.b])

                if upto == "setup":
                    return
                pro_a(0)
                hT0, un = pro_b_units(0)
                for u_ in un + inproj_units(0, hT0):
                    u_()
                nch_ = NOWN if upto is None else 2
                done_ = set()

                def do_skip(c, i):
                    if (c, i) not in done_:
                        done_.add((c, i))
                        skip_block(c, i)
                for c in range(nch_):
                    for i in range(3):
                        do_skip(c, i)
                    demod(4 * c + 3)
                    tail(4 * c + 3)
                    while pend:
                        pend.pop(0)()
                    own_block(c)
                    if c + 1 < nch_:
                        do_skip(c + 1, 0)
                    own_B(c)
                    if c + 1 < nch_:
                        do_skip(c + 1, 1)
                    own_B2(c)
                S.barrier()

        def phase_A(hs, pidx):
            es = ExitStack()
            with es:
                stgs = [sb(es, f"stgA{pidx}_{i}", [128, 256]) for i in range(3)]
                W16 = load_win(es, f"Wqkv16_{pidx}", [(64 * hs, 256, 0.125), (512 + 64 * hs, 256, 1.0), (1024 + 64 * hs, 256, 1.0)], stgs)
                kT = sb(es, f"kT{pidx}", [128, 2, NTOK], BF16)
                vS = sb(es, f"vS{pidx}", [128, NBLK, 256], BF16)
                kb_ = [Buf(f"kT_c{c}") for c in range(NOWN)]
                vb_ = [Buf(f"vS_c{c}") for c in range(NOWN)]
                qT = [sb(es, f"qT{pidx}_{i}", [128, 2, 128], BF16) for i in range(2)]
                pin = [ps(es, f"pinA{pidx}_{i}") for i in range(2)]
                pA = [ps(es, f"pA{pidx}_{i}") for i in range(4)]
                pO = ps(es, f"pO{pidx}")
                E32 = [sb(es, f"E32_{pidx}_{i}", [128, 512]) for i in range(4)]
                Lp = [sb(es, f"Lp{pidx}_{i}", [128, 512], BF16) for i in range(4)]
                AT = [sb(es, f"AT{pidx}_{i}", [128, 512], BF16) for i in range(4)]
                Gs = [sb(es, f"Gs{pidx}_{i}", [128, 128], BF16) for i in range(4)]
                Ls = [sb(es, f"Ls{pidx}_{i}", [128, 512], BF16) for i in range(4)]
                gt1 = sb(es, f"gt1_{pidx}", [128, 256], BF16); gt2 = sb(es, f"gt2_{pidx}", [128, 128], BF16)
                ysraw = sb(es, f"ysraw{pidx}", [128, 256]); junkA = sb(es, f"junkA{pidx}", [128, 256])
                pro_a, pro_b_units, PP = make_prologue(es, "dve", "dve", extra_pT=pin, reuse_rstd=True)
                deferred = []
                PP["defer"] = deferred
                pend = []

                def inproj_units(c, hT):
                    return [lambda ft=ft, h=h: inproj_k(c, hT, ft, h) for ft in range(2) for h in range(2)] + [lambda i=i, h=h: inproj_v(c, hT, i, h) for i in range(4) for h in range(2)] + [lambda: inproj_q(c, hT, 0), lambda: inproj_q(c, hT, 1)]

                def inproj_k(c, hT, ft, h):
                    if True:
                        pn = pin[ft]
                        for dt in range(4 * h, 4 * h + 4):
                            S.op("pe", lambda e: e.matmul(pn[:], lhsT=W16[:, dt, 256 + ft * 128:256 + (ft + 1) * 128], rhs=hT[:, dt, :], start=(dt == 0), stop=(dt == 7)),
                                 reads=[W16.b, hT.b], writes=[pn.b])
                        if h == 1:
                            deferred.append(lambda: S.op("dve", lambda e: e.tensor_copy(out=kT[:, ft, c * 512:(c + 1) * 512], in_=pn[:]), reads=[pn.b], writes=[kb_[c]]))

                def inproj_v(c, hT, i, h):
                    if True:
                        pn = pin[i % 2]
                        for dt in range(4 * h, 4 * h + 4):
                            S.op("pe", lambda e: e.matmul(pn[:, 0:256], lhsT=hT[:, dt, i * 128:(i + 1) * 128], rhs=W16[:, dt, 512:768], start=(dt == 0), stop=(dt == 7)),
                                 reads=[W16.b, hT.b], writes=[pn.b])
                        if h == 1:
                            deferred.append(lambda: S.op("dve", lambda e: e.tensor_copy(out=vS[:, 4 * c + i, :], in_=pn[:, 0:256]), reads=[pn.b], writes=[vb_[c]]))

                def inproj_q(c, hT, ft):
                    q = qT[c % 2]
                    if True:
                        pn = pin[ft]
                        for dt in range(8):
                            S.op("pe", lambda e: e.matmul(pn[:, 0:128], lhsT=W16[:, dt, ft * 128:(ft + 1) * 128], rhs=hT[:, dt, 384:512], start=(dt == 0), stop=(dt == 7)),
                                 reads=[W16.b, hT.b], writes=[pn.b])
                        deferred.append(lambda: S.op("dve", lambda e: e.tensor_copy(out=q[:, ft, :], in_=pn[:, 0:128]), reads=[pn.b], writes=[q.b]))

                def attention(c):
                    q = qT[c % 2]
                    tasks = [(hl, m) for hl in range(4) for m in range(c, -1, -1)]
                    st = {"n": 0}

                    def stage1(n):
                        hl, m = tasks[n]
                        sl = n % 4
                        p0 = 64 * (hl % 2)
                        ft = hl // 2
                        first = (m == c)
                        for i in range(4):
                            kb = 4 * m + i
                            S.op("pe", lambda e: e.matmul(pA[sl][:, i * 128:(i + 1) * 128], lhsT=kT[p0:p0 + 64, ft, kb * 128:(kb + 1) * 128], rhs=q[p0:p0 + 64, ft, :], start=(i == 0), stop=False, skip_group_check=True),
                                 reads=[kb_[m], q.b], writes=[pA[sl].b])
                        S.op("act", lambda e: e.activation(out=E32[sl][:], in_=pA[sl][:], func=AF.Exp), reads=[pA[sl].b], writes=[E32[sl].b])

                    def stage1b(n):
                        hl, m = tasks[n]
                        sl = n % 4
                        first = (m == c)
                        S.op("act", lambda e: e.activation(out=Lp[sl][:], in_=E32[sl][:], func=AF.Ln, bias=1.0, scale=1.0), reads=[E32[sl].b], writes=[Lp[sl].b])
                        if first:
                            S.op("dve", lambda e: e.tensor_tensor(out=Lp[sl][:, 384:512], in0=Lp[sl][:, 384:512], in1=strict16[:], op=ALU.mult), reads=[Lp[sl].b, strict16.b], writes=[Lp[sl].b])
                        L_ = Ls[sl]
                        Ln_ = Ls[(n + 1) % 4]
                        if first:
                            S.op("dve", lambda e: e.tensor_copy(out=L_[:, 256:384], in_=Lp[sl][:, 384:512]), reads=[Lp[sl].b], writes=[L_.b])
                        else:
                            S.op("dve", lambda e: e.tensor_tensor(out=L_[:, 256:384], in0=Lp[sl][:, 384:512], in1=L_[:, 384:512], op=ALU.add), reads=[Lp[sl].b, L_.b], writes=[L_.b])
                        S.op("dve", lambda e: e.tensor_tensor(out=L_[:, 128:256], in0=L_[:, 256:384], in1=Lp[sl][:, 256:384], op=ALU.add), reads=[Lp[sl].b, L_.b], writes=[L_.b])
                        S.op("dve", lambda e: e.tensor_tensor(out=L_[:, 0:128], in0=L_[:, 128:256], in1=Lp[sl][:, 128:256], op=ALU.add), reads=[Lp[sl].b, L_.b], writes=[L_.b])
                        if m > 0:
                            S.op("dve", lambda e: e.tensor_tensor(out=Ln_[:, 384:512], in0=L_[:, 0:128], in1=Lp[sl][:, 0:128], op=ALU.add), reads=[Lp[sl].b, L_.b], writes=[Ln_.b])

                    def stage2(n):
                        hl, m = tasks[n]
                        sl = n % 4
                        first = (m == c)
                        nmm = 2
                        cnt = [0]

                        def mm(out_ap, lhsT_ap, rhs_ap, rbufs):
                            cnt[0] += 1
                            S.op("pe", lambda e: e.matmul(out_ap, lhsT=lhsT_ap, rhs=rhs_ap, start=False, stop=(cnt[0] == nmm), skip_group_check=True), reads=rbufs, writes=[pA[sl].b])
                        mm(pA[sl][:], negtri16[:], Lp[sl][:], [negtri16.b, Lp[sl].b])
                        if first:
                            mm(pA[sl][:, 0:384], negones16[:], Ls[sl][:, 0:384], [negones16.b, Ls[sl].b])
                        else:
                            mm(pA[sl][:], negones16[:], Ls[sl][:], [negones16.b, Ls[sl].b])
                        S.op("act", lambda e: e.activation(out=AT[sl][:], in_=pA[sl][:], func=AF.Exp), reads=[pA[sl].b], writes=[AT[sl].b])
                        if first:
                            S.op("dve", lambda e: e.tensor_tensor(out=AT[sl][:, 384:512], in0=AT[sl][:, 384:512], in1=strict16[:], op=ALU.mult), reads=[AT[sl].b, strict16.b], writes=[AT[sl].b])

                    def stage3(n):
                        hl, m = tasks[n]
                        sl = n % 4
                        first = (m == c)
                        for i in range(4):
                            kb = 4 * m + i
                            S.op("pe", lambda e: e.matmul(pO[:, hl * 64:(hl + 1) * 64], lhsT=AT[sl][:, i * 128:(i + 1) * 128], rhs=vS[:, kb, hl * 64:(hl + 1) * 64], start=(first and i == 0 and hl == 0), stop=(m == 0 and i == 3 and hl == 3), skip_group_check=True),
                                 reads=[AT[sl].b, vb_[m]], writes=[pO.b])

                    NT = len(tasks)
                    for it in range(NT + 3):
                        while deferred:
                            deferred.pop(0)()
                        if it < NT:
                            stage1(it)
                        if pend:
                            pend.pop(0)()
                        if 0 <= it - 2 < NT:
                            stage2(it - 2)
                        if it < NT:
                            stage1b(it)
                        if 0 <= it - 3 < NT:
                            stage3(it - 3)
                    S.op("dve", lambda e: e.tensor_copy(out=ysraw[:], in_=pO[:, 0:256]), reads=[pO.b], writes=[ysraw.b])
                    dump(f"ysb{pidx}_{c}", ysraw[:], ysraw.b, [128, 256])
                    S.op("dve", lambda e: e.scalar_tensor_tensor(out=junkA[:], in0=ysraw[:], scalar=1.0, in1=ysraw[:], op0=ALU.mult, op1=ALU.mult, accum_out=ss_sb[:, c, pidx:pidx + 1]),
                         reads=[ysraw.b], writes=[junkA.b, ss_sb.b])
                    S.op("pool", lambda e: e.tensor_copy(out=ysb16[:, c, 64 * hs:64 * hs + 256], in_=ysraw[:]), reads=[ysraw.b], writes=[ysb16.b])

                nch = NOWN if upto is None else int(upto)
                pro_a(0)
                hT0, un = pro_b_units(0)
                for u_ in un + inproj_units(0, hT0):
                    u_()
                    while deferred:
                        deferred.pop(0)()
                for c in range(nch):
                    if c + 1 < nch:
                        pro_a(c + 1)
                        hTn, un = pro_b_units(c + 1)
                        pend.extend(un + inproj_units(c + 1, hTn))
                    attention(c)
                    while pend:
                        pend.pop(0)()
                        while deferred:
                            deferred.pop(0)()
                    while deferred:
                        deferred.pop(0)()
                S.barrier()

        def phase_F(R):
            es = ExitStack()
            with es:
                acc, xnT16, Wgt = R["acc"], R["xnT16"], R["Wgt"]
                stgF = [sb(es, f"stgF{i}", [128, 1024]) for i in range(2)]
                gcat = sb(es, "gcat", [128, 8]); load(gcat, gcat[:], gcat_d)
                g2 = R["g2"]
                Wout16 = sb(es, "Wout16", [128, 8, 1024], BF16)
                for k in range(8):
                    stg = stgF[k % 2]
                    load(stg, stg[:], w_out[k * 128:(k + 1) * 128, :])
                    S.op("act", lambda e: e.activation(out=Wout16[:, k, :], in_=stg[:], func=AF.Copy, scale=gcat[:, k:k + 1]), reads=[stg.b, gcat.b], writes=[Wout16.b])
                Wr16 = sb(es, "Wr16", [128, 8, 32], BF16)
                for k in range(8):
                    stg = stgF[k % 2]
                    load(stg, stg[:, 0:32], w_router[k * 128:(k + 1) * 128, :])
                    S.op("act", lambda e: e.activation(out=Wr16[:, k, :], in_=stg[:, 0:32], func=AF.Copy, scale=g2[:, k:k + 1]), reads=[stg.b, g2.b], writes=[Wr16.b])
                browb = sb(es, "browb", [128, 32]); load(browb, browb[:], brow_d.partition_broadcast(128))
                bd32 = sb(es, "bd32", [32, 1024]); load(bd32, bd32[:], bd_d)
                def two(name, shape, dt=F32):
                    return [sb(es, f"{name}{i}", shape, dt) for i in range(2)]
                xb_ = two("xbF", [128, 1024]); mix16_ = two("mix16", [128, 512], BF16); mixT_ = two("mixT", [128, 8, 128], BF16)
                sst_ = two("sstF", [128, 1]); rst_ = two("rstF", [128, 1]); junk = sb(es, "junkF", [128, 1024], BF16)
                xn16_ = two("xn16", [128, 1024], BF16)
                lg_ = two("lg", [128, 32]); mx8_ = two("mx8", [128, 8]); msk_ = two("msk", [128, 32]); nmx_ = two("nmx", [128, 1])
                ex_ = two("exF", [128, 32]); den_ = two("denF", [128, 1]); WgtT_ = two("WgtT", [32, 128])
                pT_ = [ps(es, f"pTF{i}", [128, 1024], BF16) for i in range(2)]
                pd = [ps(es, f"pdF{i}") for i in range(2)]
                pl = ps(es, "plF"); pw = ps(es, "pwF")
                pbs = [ps(es, f"pbF{i}") for i in range(2)]
                for ob in range(NOWN):
                    z_ = ob % 2
                    xb, mix16, mixT, sst, rst, xn16 = xb_[z_], mix16_[z_], mixT_[z_], sst_[z_], rst_[z_], xn16_[z_]
                    lg, mx8, msk, nmx, ex, den, WgtT, pT = lg_[z_], mx8_[z_], msk_[z_], nmx_[z_], ex_[z_], den_[z_], WgtT_[z_], pT_[z_]
                    load(xb, xb[:], xv[(4 * ob + 3) * 128:(4 * ob + 4) * 128, :])
                    S.op("dve", lambda e: e.tensor_tensor(out=sst[:], in0=ss_sb[:, ob, 0:1], in1=ss_sb[:, ob, 1:2], op=ALU.add), reads=[ss_sb.b], writes=[sst.b])
                    S.op("act", lambda e: e.activation(out=rst[:], in_=sst[:], func=AF.Ln, scale=1.0 / 512, bias=EPS), reads=[sst.b], writes=[rst.b])
                    S.op("act", lambda e: e.activation(out=rst[:], in_=rst[:], func=AF.Exp, scale=-0.5), reads=[rst.b], writes=[rst.b])
                    S.op("act", lambda e: e.activation(out=mix16[:], in_=ysb16[:, ob, :], func=AF.Copy, scale=rst[:, 0:1]), reads=[ysb16.b, rst.b], writes=[mix16.b])
                    for k in range(8):
                        src = mix16[:, k * 128:(k + 1) * 128] if k < 4 else mssm16[:, ob, (k - 4) * 128:(k - 3) * 128]
                        S.op("pe", lambda e: e.transpose(pT[:, k * 128:(k + 1) * 128], src, ident16[:]), reads=[mix16.b, mssm16.b, ident16.b], writes=[pT.b])
                    S.op("dve", lambda e: e.tensor_copy(out=mixT[:].rearrange("p a b -> p (a b)"), in_=pT[:]), reads=[pT.b], writes=[mixT.b])
                    for half in range(2):
                        for k in range(8):
                            S.op("pe", lambda e: e.matmul(pd[half][:], lhsT=mixT[:, k, :], rhs=Wout16[:, k, half * 512:(half + 1) * 512], start=(k == 0), stop=(k == 7)),
                                 reads=[mixT.b, Wout16.b], writes=[pd[half].b])
                        S.op("dve", lambda e: e.tensor_tensor(out=acc[:, ob, half * 512:(half + 1) * 512], in0=pd[half][:], in1=xb[:, half * 512:(half + 1) * 512], op=ALU.add),
                             reads=[pd[half].b, xb.b], writes=[acc.b])
                    dump(f"x1_{ob}", acc[:, ob, :], acc.b, [128, 1024])
                    S.op("act", lambda e: e.activation(out=junk[:], in_=acc[:, ob, :], func=AF.Square, accum_out=sst[:, 0:1]), reads=[acc.b], writes=[junk.b, sst.b])
                    S.op("act", lambda e: e.activation(out=rst[:], in_=sst[:], func=AF.Ln, scale=1.0 / 1024, bias=EPS), reads=[sst.b], writes=[rst.b])
                    S.op("act", lambda e: e.activation(out=rst[:], in_=rst[:], func=AF.Exp, scale=-0.5), reads=[rst.b], writes=[rst.b])
                    S.op("act", lambda e: e.activation(out=xn16[:], in_=acc[:, ob, :], func=AF.Copy, scale=rst[:, 0:1]), reads=[acc.b, rst.b], writes=[xn16.b])
                    for k in range(8):
                        S.op("pe", lambda e: e.transpose(pT[:, k * 128:(k + 1) * 128], xn16[:, k * 128:(k + 1) * 128], ident16[:]), reads=[xn16.b, ident16.b], writes=[pT.b])
                    S.op("dve", lambda e: e.tensor_copy(out=xnT16[:, :, ob * 128:(ob + 1) * 128], in_=pT[:].rearrange("p (a b) -> p a b", a=8)), reads=[pT.b], writes=[xnT16.b])
                    for k in range(8):
                        S.op("pe", lambda e: e.matmul(pl[:, 0:32], lhsT=xnT16[:, k, ob * 128:(ob + 1) * 128], rhs=Wr16[:, k, :], start=(k == 0), stop=(k == 7)), reads=[xnT16.b, Wr16.b], writes=[pl.b])
                    S.op("dve", lambda e: e.tensor_tensor(out=lg[:], in0=pl[:, 0:32], in1=browb[:], op=ALU.add), reads=[pl.b, browb.b], writes=[lg.b])
                    dump(f"lg_{ob}", lg[:], lg.b, [128, 32])
                    S.op("dve", lambda e: e.max(out=mx8[:], in_=lg[:]), reads=[lg.b], writes=[mx8.b])
                    S.op("dve", lambda e: e.tensor_scalar(out=msk[:], in0=lg[:], scalar1=mx8[:, 3:4], scalar2=None, op0=ALU.is_ge), reads=[lg.b, mx8.b], writes=[msk.b])
                    S.op("dve", lambda e: e.tensor_scalar(out=nmx[:], in0=mx8[:, 0:1], scalar1=-1.0, scalar2=None, op0=ALU.mult), reads=[mx8.b], writes=[nmx.b])
                    S.op("act", lambda e: e.activation(out=ex[:], in_=lg[:], func=AF.Exp, bias=nmx[:, 0:1], scale=1.0), reads=[lg.b, nmx.b], writes=[ex.b])
                    S.op("dve", lambda e: e.scalar_tensor_tensor(out=ex[:], in0=ex[:], scalar=1.0, in1=msk[:], op0=ALU.mult, op1=ALU.mult, accum_out=den[:, 0:1]), reads=[ex.b, msk.b], writes=[ex.b, den.b])
                    S.op("dve", lambda e: e.reciprocal(out=den[:], in_=den[:]), reads=[den.b], writes=[den.b])
                    S.op("dve", lambda e: e.tensor_scalar(out=Wgt[:, ob, :], in0=ex[:], scalar1=den[:, 0:1], scalar2=None, op0=ALU.mult), reads=[ex.b, den.b], writes=[Wgt.b])
                    S.op("pe", lambda e: e.transpose(pw[0:32, 0:128], Wgt[:, ob, :], ident32[:]), reads=[Wgt.b, ident32.b], writes=[pw.b])
                    S.op("dve", lambda e: e.tensor_copy(out=WgtT[:], in_=pw[0:32, 0:128]), reads=[pw.b], writes=[WgtT.b])
                    for half in range(2):
                        S.op("pe", lambda e: e.matmul(pbs[half][:], lhsT=WgtT[:], rhs=bd32[:, half * 512:(half + 1) * 512], start=True, stop=True), reads=[WgtT.b, bd32.b], writes=[pbs[half].b])
                        S.op("dve", lambda e: e.tensor_tensor(out=acc[:, ob, half * 512:(half + 1) * 512], in0=pbs[half][:], in1=acc[:, ob, half * 512:(half + 1) * 512], op=ALU.add),
                             reads=[pbs[half].b, acc.b], writes=[acc.b])
                S.barrier()

        def phase_M(R):
            es = ExitStack()
            with es:
                acc, xnT16, Wgt, g2 = R["acc"], R["xnT16"], R["Wgt"], R["g2"]
                bgT = sb(es, "bgT", [128, 32, 8]); load(bgT, bgT[:], bgT_d)
                buT = sb(es, "buT", [128, 32, 8]); load(buT, buT[:], buT_d)
                Wg16 = sb(es, "Wg16", [128, 8, 1024], BF16); Wu16 = sb(es, "Wu16m", [128, 8, 1024], BF16); Wd16 = sb(es, "Wd16", [128, 8, 1024], BF16)
                stg = [sb(es, f"stgM{i}", [128, 1024]) for i in range(2)]
                hT = View(shared[:, 0:16384].rearrange("p (a b) -> p a b", a=8), "hTm")
                hb = [Buf(f"hTm_t{i}") for i in range(4)]
                g32 = [sb(es, f"g32_{i}", [128, 512]) for i in range(2)]; s32 = [sb(es, f"s32_{i}", [128, 512]) for i in range(2)]
                u32 = [sb(es, f"u32m_{i}", [128, 512]) for i in range(2)]
                pg = [ps(es, f"pg{i}") for i in range(3)]; pu = [ps(es, f"pu{i}") for i in range(3)]; pdn = [ps(es, f"pdn{i}") for i in range(2)]
                nst = [0]
                K17 = 1.0 / 1.702
                g2s = sb(es, "g2s", [128, 8])
                S.op("dve", lambda e: e.tensor_scalar(out=g2s[:], in0=g2[:], scalar1=K17, scalar2=None, op0=ALU.mult), reads=[g2.b], writes=[g2s.b])
                S.op("dve", lambda e: e.tensor_scalar(out=buT[:], in0=buT[:], scalar1=K17, scalar2=None, op0=ALU.mult), reads=[buT.b], writes=[buT.b])

                def w_pieces(dst, src_e, scaled):
                    def piece(dt):
                        st = stg[nst[0] % 2]
                        nst[0] += 1
                        load(st, st[:], src_e[dt * 128:(dt + 1) * 128, :])
                        if scaled is not None:
                            S.op("act", lambda e: e.activation(out=dst[:, dt, :], in_=st[:], func=AF.Copy, scale=scaled[:, dt:dt + 1]), reads=[st.b, scaled.b], writes=[dst.b])
                        else:
                            S.op("act", lambda e: e.activation(out=dst[:, dt, :], in_=st[:], func=AF.Copy), reads=[st.b], writes=[dst.b])
                    return [lambda dt=dt: piece(dt) for dt in range(8)]

                ngu = [0]
                ndn = [0]
                pendw = []

                def gate_up(e_):
                    for tt in range(4):
                        t0 = tt * 512
                        for ft in range(8):
                            sl = ngu[0] % 3
                            s2 = ngu[0] % 2
                            ngu[0] += 1
                            for dt in range(8):
                                S.op("pe", lambda e: e.matmul(pg[sl][:], lhsT=Wg16[:, dt, ft * 128:(ft + 1) * 128], rhs=xnT16[:, dt, t0:t0 + 512], start=(dt == 0), stop=(dt == 7)), reads=[Wg16.b, xnT16.b], writes=[pg[sl].b])
                            for dt in range(8):
                                S.op("pe", lambda e: e.matmul(pu[sl][:], lhsT=Wu16[:, dt, ft * 128:(ft + 1) * 128], rhs=xnT16[:, dt, t0:t0 + 512], start=(dt == 0), stop=(dt == 7)), reads=[Wu16.b, xnT16.b], writes=[pu[sl].b])
                            S.op("dve", lambda e: e.tensor_scalar(out=g32[s2][:], in0=pg[sl][:], scalar1=bgT[:, e_, ft:ft + 1], scalar2=7.0, op0=ALU.add, op1=ALU.min), reads=[pg[sl].b, bgT.b], writes=[g32[s2].b])
                            S.op("act", lambda e: e.activation(out=s32[s2][:], in_=g32[s2][:], func=AF.Silu, scale=1.702), reads=[g32[s2].b], writes=[s32[s2].b])
                            S.op("dve", lambda e: e.tensor_scalar(out=u32[s2][:], in0=pu[sl][:], scalar1=buT[:, e_, ft:ft + 1], scalar2=7.0 * K17, op0=ALU.add, op1=ALU.min), reads=[pu[sl].b, buT.b], writes=[u32[s2].b])
                            S.op("dve", lambda e: e.tensor_scalar(out=u32[s2][:], in0=u32[s2][:], scalar1=-7.0 * K17, scalar2=K17, op0=ALU.max, op1=ALU.add), reads=[u32[s2].b], writes=[u32[s2].b])
                            S.op("dve", lambda e: e.tensor_tensor(out=hT[:, ft, t0:t0 + 512], in0=s32[s2][:], in1=u32[s2][:], op=ALU.mult), reads=[s32[s2].b, u32[s2].b], writes=[hb[tt]])
                            if pendw and (ngu[0] % 2 == 0):
                                pendw.pop(0)()

                def down(e_):
                    for ob in range(NOWN):
                        for hf in range(2):
                            sl = ndn[0] % 2
                            ndn[0] += 1
                            for ft in range(8):
                                S.op("pe", lambda e: e.matmul(pdn[sl][:], lhsT=hT[:, ft, ob * 128:(ob + 1) * 128], rhs=Wd16[:, ft, hf * 512:(hf + 1) * 512], start=(ft == 0), stop=(ft == 7)), reads=[hb[ob // 4], Wd16.b], writes=[pdn[sl].b])
                            S.op("dve", lambda e: e.scalar_tensor_tensor(out=acc[:, ob, hf * 512:(hf + 1) * 512], in0=pdn[sl][:], scalar=Wgt[:, ob, e_:e_ + 1], in1=acc[:, ob, hf * 512:(hf + 1) * 512], op0=ALU.mult, op1=ALU.add),
                                 reads=[pdn[sl].b, Wgt.b, acc.b], writes=[acc.b])

                for p_ in w_pieces(Wg16, w_gate[0], g2) + w_pieces(Wu16, w_up[0], g2s):
                    p_()
                for e_ in range(n_experts):
                    pendw.extend(w_pieces(Wd16, w_down[e_], None))
                    gate_up(e_)
                    while pendw:
                        pendw.pop(0)()
                    if e_ + 1 < n_experts:
                        for p_ in w_pieces(Wg16, w_gate[e_ + 1], g2) + w_pieces(Wu16, w_up[e_ + 1], g2s):
                            p_()
                    down(e_)
                S.barrier()

        def phase_out(R):
            es = ExitStack()
            with es:
                acc = R["acc"]
                gfb = sb(es, "gfb", [128, 1024]); load(gfb, gfb[:], gf_d.partition_broadcast(128))
                junk = sb(es, "junkO", [128, 1024], BF16); sst = sb(es, "sstO", [128, 1]); rst = sb(es, "rstO", [128, 1])
                ob_t = [sb(es, f"obuf{i}", [128, 1024]) for i in range(2)]
                for ob in range(NOWN):
                    o = ob_t[ob % 2]
                    dump(f"x2_{ob}", acc[:, ob, :], acc.b, [128, 1024])
                    S.op("act", lambda e: e.activation(out=junk[:], in_=acc[:, ob, :], func=AF.Square, accum_out=sst[:, 0:1]), reads=[acc.b], writes=[junk.b, sst.b])
                    S.op("act", lambda e: e.activation(out=rst[:], in_=sst[:], func=AF.Ln, scale=1.0 / 1024, bias=EPS), reads=[sst.b], writes=[rst.b])
                    S.op("act", lambda e: e.activation(out=rst[:], in_=rst[:], func=AF.Exp, scale=-0.5), reads=[rst.b], writes=[rst.b])
                    S.op("dve", lambda e: e.scalar_tensor_tensor(out=o[:], in0=acc[:, ob, :], scalar=rst[:, 0:1], in1=gfb[:], op0=ALU.mult, op1=ALU.mult), reads=[acc.b, rst.b, gfb.b], writes=[o.b])
                    S.dma(lambda e: e.dma_start(out=out_d[ob * 128:(ob + 1) * 128, :], in_=o[:]), reads=[o.b])

        def finish():
            S.finish()
            with nc.Block() as block:
                S.emit(block)
            return nc, dbg_d

        phases = stop_after or "SABFMO"
        if "S" in phases:
            phase_S()
        if "A" in phases:
            phase_A(0, 0)
        if "B" in phases:
            phase_A(4, 1)
        R = {}
        R["g2"] = sb(top, "g2", [128, 8]); load(R["g2"], R["g2"][:], g2_d)
        R["acc"] = sb(top, "acc", [128, NOWN, 1024])
        R["xnT16"] = sb(top, "xnT16", [128, 8, NOWN * 128], BF16)
        R["Wgt"] = sb(top, "Wgt", [128, NOWN, 32])
        if "F" in phases:
            phase_F(R)
        if "M" in phases:
            phase_M(R)
        if "O" in phases:
            phase_out(R)
        return finish()


def host_inputs(x, ln1_g, w_in, lam_re, lam_im, log_dt, ssm_b_re, ssm_b_im, ssm_c_re, ssm_c_im,
                ssm_d, w_glu, g_sb, g_ssm, w_out, ln2_g, w_router, b_router, w_gate, b_gate,
                w_up, b_up, w_down, b_down, ln_f_g, cores=range(8)):
    f = np.float32
    c_ = np.ascontiguousarray
    x = np.asarray(x, f)
    shared = {
        "w_in": c_(np.asarray(w_in, f)[0]), "w_glu": c_(np.asarray(w_glu, f)[0]), "w_out": c_(np.asarray(w_out, f)[0]),
        "w_router": c_(np.asarray(w_router, f)[0]),
        "w_gate": c_(np.asarray(w_gate, f)[0]), "w_up": c_(np.asarray(w_up, f)[0]), "w_down": c_(np.asarray(w_down, f)[0]),
        "g1": c_(np.asarray(ln1_g, f)[0].reshape(8, 128).T),
        "gcat": c_(np.concatenate([np.asarray(g_sb, f)[0], np.asarray(g_ssm, f)[0]]).reshape(8, 128).T),
        "g2": c_(np.asarray(ln2_g, f)[0].reshape(8, 128).T),
        "gf": c_(np.asarray(ln_f_g, f).reshape(1, 1024)), "brow": c_(np.asarray(b_router, f)[0].reshape(1, 32)),
        "bgT": c_(np.asarray(b_gate, f)[0].reshape(32, 8, 128).transpose(2, 0, 1)),
        "buT": c_(np.asarray(b_up, f)[0].reshape(32, 8, 128).transpose(2, 0, 1)),
        "bd": c_(np.asarray(b_down, f)[0]),
        "lamre": c_(np.asarray(lam_re, f)[0].reshape(16, 128).T), "lamim": c_(np.asarray(lam_im, f)[0].reshape(16, 128).T),
        "logdt": c_(np.repeat(np.asarray(log_dt, f)[0].reshape(16, 2, 1), 64, axis=2).reshape(16, 128).T),
        "bre": c_(np.asarray(ssm_b_re, f)[0].reshape(16, 128, 16).transpose(1, 0, 2)),
        "bim": c_(np.asarray(ssm_b_im, f)[0].reshape(16, 128, 16).transpose(1, 0, 2)),
        "creT": c_(np.asarray(ssm_c_re, f)[0].reshape(16, 2, 16, 64).transpose(1, 3, 0, 2).reshape(128, 16, 16)),
        "cimT": c_(np.asarray(ssm_c_im, f)[0].reshape(16, 2, 16, 64).transpose(1, 3, 0, 2).reshape(128, 16, 16)),
        "dsk": c_(np.asarray(ssm_d, f)[0].reshape(4, 128).T),
        "ident": np.eye(128, dtype=f), "negtri": c_(-np.tril(np.ones((128, 128), f))), "negones": -np.ones((128, 128), f),
        "strict": c_(np.triu(np.ones((128, 128), f), 1)),
        "ramp": c_(np.broadcast_to(np.arange(1, 129, dtype=f)[None, :], (128, 128))),
        "ramp2": c_(np.broadcast_to(np.arange(127, -1, -1, dtype=f)[None, :], (128, 128))),
    }
    maps = []
    for c in cores:
        b, qt = c // 4, c % 4
        xp = np.concatenate([np.zeros((384, 1024), f), x[b]], axis=0)
        m = dict(shared)
        m["xv"] = c_(xp[qt * 128: qt * 128 + NTOK])
        maps.append(m)
    return maps


_NC_CACHE = {}


def kernel(**inputs):
    if "nc" not in _NC_CACHE:
        _NC_CACHE["nc"] = build_program()[0]
    nc = _NC_CACHE["nc"]
    maps = host_inputs(**inputs)
    res = run_bass_kernel_spmd(nc, maps, core_ids=list(range(8)))
    out = np.empty((2, 8192, 1024), np.float32)
    for c in range(8):
        b, qt = c // 4, c % 4
        o = np.asarray(res.results[c]["out"]).reshape(NOWN, 128, 1024)
        out[b].reshape(NBLK, 128, 1024)[qt::4] = o
    return out
```
